# Optimizing a Trainium2 kernel written in Bass

```python
import math
import jax, jax.numpy as jnp
from jax import lax
import numpy as np

D_MODEL = 1024
BATCH = 8
SEQ = 4096
DEPTH = 1

CHUNK = 64
Q_BLOCK = 128
MEM_LEN = 256
EPS = 1e-6

DA_HEADS = 4
DA_QK_DIM = D_MODEL // 16
DA_V_DIM = 2 * DA_QK_DIM
DA_QK_COLS = DA_HEADS * 2 * DA_QK_DIM
DA_WIDTH = DA_HEADS * DA_V_DIM

GLA_HEADS = 4
GLA_K_DIM = D_MODEL // 16
GLA_V_DIM = D_MODEL // 8
GLA_QK_COLS = GLA_HEADS * GLA_K_DIM
GLA_WIDTH = GLA_HEADS * GLA_V_DIM
GLA_GATE_RANK = 16
GLA_TAU = 16.0

MIX_WIDTH = DA_WIDTH + GLA_WIDTH
IN_WIDTH = 2 * DA_QK_COLS + DA_WIDTH + 2 * GLA_QK_COLS + 2 * GLA_WIDTH + GLA_GATE_RANK

CROSS_HEADS = 4
CROSS_DIM = D_MODEL // CROSS_HEADS

N_GROUPS = 4
EXPERTS_PER_GROUP = 8
N_EXPERTS = N_GROUPS * EXPERTS_PER_GROUP
TOP_K = 2
D_EXPERT = D_MODEL // 2
EXPERT_BLOCK = 128

kernel_name = 'hybrid_diffattn_gla_hiermoe_layer'


def _rmsnorm(t, g):
    tf = t.astype(jnp.float32)
    tf = tf * lax.rsqrt(jnp.mean(tf * tf, axis=-1, keepdims=True) + EPS)
    return (tf * g.astype(jnp.float32)).astype(t.dtype)


def _split_heads(t, n):
    b, s, _ = t.shape
    return t.reshape(b, s, n, -1).transpose(0, 2, 1, 3)


def _merge_heads(t):
    b, h, s, d = t.shape
    return t.transpose(0, 2, 1, 3).reshape(b, s, h * d)


def _diff_attention(q1, q2, k1, k2, v, lam):
    b, h, s, d = q1.shape
    n_blocks = s // Q_BLOCK
    scale = d ** -0.5
    key_chunk = jnp.arange(s) // CHUNK

    def to_blocks(t):
        return t.reshape(b, h, n_blocks, Q_BLOCK, d).transpose(2, 0, 1, 3, 4)

    def one_block(args):
        qb1, qb2, bi = args
        q_chunk = (bi * Q_BLOCK + jnp.arange(Q_BLOCK)) // CHUNK
        mask = key_chunk[None, :] <= q_chunk[:, None]

        def probs(qb, kk):
            sc = jnp.einsum('bhqd,bhkd->bhqk', qb, kk).astype(jnp.float32) * scale
            return jax.nn.softmax(jnp.where(mask, sc, -jnp.inf), axis=-1)

        p = probs(qb1, k1) - lam * probs(qb2, k2)
        return jnp.einsum('bhqk,bhkv->bhqv', p.astype(v.dtype), v)

    o = lax.map(one_block, (to_blocks(q1), to_blocks(q2), jnp.arange(n_blocks)))
    return o.transpose(1, 2, 0, 3, 4).reshape(b, h, s, v.shape[-1])


def _gla_chunked(q, k, v, log_a):
    b, h, s, dk = q.shape
    dv = v.shape[-1]
    nc = s // CHUNK
    q = q.reshape(b, h, nc, CHUNK, dk)
    k = k.reshape(b, h, nc, CHUNK, dk)
    v = v.reshape(b, h, nc, CHUNK, dv)
    L = jnp.cumsum(log_a.reshape(b, h, nc, CHUNK, dk), axis=3)
    L_end = L[:, :, :, -1, :]
    Lc = L - L[:, :, :, CHUNK // 2 - 1:CHUNK // 2, :]
    e_pos, e_neg = jnp.exp(Lc), jnp.exp(-Lc)
    a_past = jnp.einsum('bhnid,bhnjd->bhnij', q * e_pos, k * e_neg)
    a_fut = jnp.einsum('bhnid,bhnjd->bhnij', q * e_neg, k * e_pos)
    lower = jnp.tril(jnp.ones((CHUNK, CHUNK), dtype=bool))
    a = jnp.where(lower, a_past, a_fut)
    o_intra = jnp.einsum('bhnij,bhnjv->bhniv', a, v)
    u = jnp.einsum('bhnjd,bhnjv->bhndv', k * jnp.exp(L_end[:, :, :, None, :] - L), v)
    decay = jnp.exp(L_end)

    def step(state, inp):
        d_c, u_c = inp
        return state * d_c[..., None] + u_c, state

    _, s_prev = lax.scan(step, jnp.zeros((b, h, dk, dv), jnp.float32),
                         (jnp.moveaxis(decay, 2, 0), jnp.moveaxis(u, 2, 0)))
    s_prev = jnp.moveaxis(s_prev, 0, 2)
    o_inter = jnp.einsum('bhnid,bhndv->bhniv', q * jnp.exp(L), s_prev)
    return (o_intra + o_inter).reshape(b, h, s, dv)


def _mixer(h, norm_g, w_in, da_qn, da_kn, lq1, lk1, lq2, lk2, da_on, gate_w, gate_b, gla_on, w_o, lam_init):
    b, s, _ = h.shape
    f32 = jnp.float32
    u = _rmsnorm(h, norm_g)
    p = u @ w_in
    widths = (DA_QK_COLS, DA_QK_COLS, DA_WIDTH, GLA_QK_COLS, GLA_QK_COLS, GLA_WIDTH, GLA_WIDTH, GLA_GATE_RANK)
    parts = []
    off = 0
    for w in widths:
        parts.append(p[..., off:off + w])
        off += w
    dq, dk, dv, gq, gk, gv, gg, gr = parts

    dq = dq.reshape(b, s, DA_HEADS, 2, DA_QK_DIM).transpose(0, 2, 3, 1, 4)
    dk = dk.reshape(b, s, DA_HEADS, 2, DA_QK_DIM).transpose(0, 2, 3, 1, 4)
    dq = _rmsnorm(dq, da_qn)
    dk = _rmsnorm(dk, da_kn)
    dvh = _split_heads(dv, DA_HEADS)
    lam = (jnp.exp(jnp.sum(lq1.astype(f32) * lk1.astype(f32)))
           - jnp.exp(jnp.sum(lq2.astype(f32) * lk2.astype(f32))) + lam_init)
    da = _diff_attention(dq[:, :, 0], dq[:, :, 1], dk[:, :, 0], dk[:, :, 1], dvh, lam)
    da = _rmsnorm(da, da_on) * (1.0 - lam_init)
    da_out = _merge_heads(da)

    qg = _split_heads(gq, GLA_HEADS).astype(f32) * (GLA_K_DIM ** -0.5)
    kg = _split_heads(gk, GLA_HEADS).astype(f32)
    vg = _split_heads(gv, GLA_HEADS).astype(f32)
    z = gr.astype(f32) @ gate_w.astype(f32) + gate_b.astype(f32)
    log_a = _split_heads(jax.nn.log_sigmoid(z) / GLA_TAU, GLA_HEADS)
    og = _gla_chunked(qg, kg, vg, log_a)
    og = _rmsnorm(og, gla_on)
    gla_out = (_merge_heads(og) * jax.nn.silu(gg.astype(f32))).astype(h.dtype)

    return jnp.concatenate([da_out.astype(h.dtype), gla_out], axis=-1) @ w_o


def _cross_attention(h, mem, norm_g, norm_m, w_q, w_kv, qn, kn, w_out):
    b, s, d = h.shape
    m = mem.shape[1]
    u = _rmsnorm(h, norm_g)
    mn = _rmsnorm(mem, norm_m)
    q = _rmsnorm((u @ w_q).reshape(b, s, CROSS_HEADS, CROSS_DIM), qn)
    kv = mn @ w_kv
    k = _rmsnorm(kv[..., :d].reshape(b, m, CROSS_HEADS, CROSS_DIM), kn)
    v = kv[..., d:].reshape(b, m, CROSS_HEADS, CROSS_DIM)
    sc = jnp.einsum('bshd,bmhd->bhsm', q, k).astype(jnp.float32) * (CROSS_DIM ** -0.5)
    pr = jax.nn.softmax(sc, axis=-1).astype(v.dtype)
    o = jnp.einsum('bhsm,bmhd->bshd', pr, v).reshape(b, s, d)
    return o @ w_out


def _hier_moe(h, norm_g, w_group, b_group, w_expert, b_expert, w_gate, w_up, w_down):
    b, s, d = h.shape
    t = b * s
    f32 = jnp.float32
    xf = _rmsnorm(h, norm_g).reshape(t, d)
    xr = xf.astype(f32)
    p_group = jax.nn.softmax(xr @ w_group.astype(f32) + b_group.astype(f32), axis=-1)
    g_sel = jnp.argmax(p_group, axis=-1)
    p_g = jnp.take_along_axis(p_group, g_sel[:, None], axis=-1)
    e_logits = (xr @ w_expert.astype(f32) + b_expert.astype(f32)).reshape(t, N_GROUPS, EXPERTS_PER_GROUP)
    e_logits = jnp.take_along_axis(e_logits, g_sel[:, None, None], axis=1)[:, 0]
    top_p, top_i = lax.top_k(jax.nn.softmax(e_logits, axis=-1), TOP_K)
    gate = p_g * top_p / jnp.sum(top_p, axis=-1, keepdims=True)
    flat_id = (g_sel[:, None] * EXPERTS_PER_GROUP + top_i).reshape(-1).astype(jnp.int32)
    flat_gate = gate.reshape(-1)
    n_assign = t * TOP_K
    order = jnp.argsort(flat_id)
    sorted_id = flat_id[order]
    token_of = order // TOP_K
    sizes = jnp.bincount(flat_id, length=N_EXPERTS)
    padded = (sizes + EXPERT_BLOCK - 1) // EXPERT_BLOCK * EXPERT_BLOCK
    start = jnp.cumsum(sizes) - sizes
    padded_end = jnp.cumsum(padded)
    padded_start = padded_end - padded
    dest = padded_start[sorted_id] + jnp.arange(n_assign) - start[sorted_id]
    n_rows = n_assign + N_EXPERTS * EXPERT_BLOCK
    n_blocks = n_rows // EXPERT_BLOCK
    x_pad = jnp.zeros((n_rows, d), xf.dtype).at[dest].set(xf[token_of])
    block_expert = jnp.minimum(
        jnp.searchsorted(padded_end, jnp.arange(n_blocks) * EXPERT_BLOCK, side='right'), N_EXPERTS - 1)

    def expert_block(args):
        xb, e = args
        hid = jax.nn.silu(xb @ w_gate[e]) * (xb @ w_up[e])
        return hid @ w_down[e]

    out_pad = lax.map(expert_block, (x_pad.reshape(n_blocks, EXPERT_BLOCK, d), block_expert)).reshape(n_rows, d)
    out = out_pad[dest] * flat_gate[order][:, None].astype(out_pad.dtype)
    y = jnp.zeros((t, d), out.dtype).at[token_of].add(out)
    return y.reshape(b, s, d).astype(h.dtype)


def setup_inputs(seed: int = 0) -> dict:
    key = jax.random.key(seed)
    ks = iter(jax.random.split(key, 40))
    f32 = jnp.float32
    D = D_MODEL

    def nrm(shape, scale):
        return scale * jax.random.normal(next(ks), shape, f32)

    def gain(n):
        return 1.0 + 0.02 * jax.random.normal(next(ks), (DEPTH, n), f32)

    return {
        'x': nrm((BATCH, SEQ, D), 1.0),
        'mem': nrm((BATCH, MEM_LEN, D), 1.0),
        'norm_mix': gain(D),
        'w_in': nrm((DEPTH, D, IN_WIDTH), D ** -0.5),
        'da_q_norm': gain(DA_QK_DIM),
        'da_k_norm': gain(DA_QK_DIM),
        'lambda_q1': nrm((DEPTH, DA_QK_DIM), 0.1),
        'lambda_k1': nrm((DEPTH, DA_QK_DIM), 0.1),
        'lambda_q2': nrm((DEPTH, DA_QK_DIM), 0.1),
        'lambda_k2': nrm((DEPTH, DA_QK_DIM), 0.1),
        'da_out_norm': gain(DA_V_DIM),
        'gla_gate_w': nrm((DEPTH, GLA_GATE_RANK, GLA_QK_COLS), GLA_GATE_RANK ** -0.5),
        'gla_gate_b': nrm((DEPTH, GLA_QK_COLS), 0.1),
        'gla_out_norm': gain(GLA_V_DIM),
        'w_o': nrm((DEPTH, MIX_WIDTH, D), MIX_WIDTH ** -0.5),
        'norm_cross': gain(D),
        'norm_mem': gain(D),
        'w_cq': nrm((DEPTH, D, D), D ** -0.5),
        'w_ckv': nrm((DEPTH, D, 2 * D), D ** -0.5),
        'cross_q_norm': gain(CROSS_DIM),
        'cross_k_norm': gain(CROSS_DIM),
        'w_co': nrm((DEPTH, D, D), D ** -0.5),
        'norm_ffn': gain(D),
        'w_group': nrm((DEPTH, D, N_GROUPS), D ** -0.5),
        'b_group': nrm((DEPTH, N_GROUPS), 0.01),
        'w_expert': nrm((DEPTH, D, N_EXPERTS), D ** -0.5),
        'b_expert': nrm((DEPTH, N_EXPERTS), 0.01),
        'w_e_gate': nrm((DEPTH, N_EXPERTS, D, D_EXPERT), D ** -0.5),
        'w_e_up': nrm((DEPTH, N_EXPERTS, D, D_EXPERT), D ** -0.5),
        'w_e_down': nrm((DEPTH, N_EXPERTS, D_EXPERT, D), D_EXPERT ** -0.5),
    }


def reference(x, mem, norm_mix, w_in, da_q_norm, da_k_norm, lambda_q1, lambda_k1, lambda_q2, lambda_k2,
              da_out_norm, gla_gate_w, gla_gate_b, gla_out_norm, w_o, norm_cross, norm_mem, w_cq, w_ckv,
              cross_q_norm, cross_k_norm, w_co, norm_ffn, w_group, b_group, w_expert, b_expert,
              w_e_gate, w_e_up, w_e_down):
    h = x
    for l in range(DEPTH):
        lam_init = 0.8 - 0.6 * math.exp(-0.3 * l)
        h = h + _mixer(h, norm_mix[l], w_in[l], da_q_norm[l], da_k_norm[l], lambda_q1[l], lambda_k1[l],
                       lambda_q2[l], lambda_k2[l], da_out_norm[l], gla_gate_w[l], gla_gate_b[l],
                       gla_out_norm[l], w_o[l], lam_init)
        h = h + _cross_attention(h, mem, norm_cross[l], norm_mem[l], w_cq[l], w_ckv[l],
                                 cross_q_norm[l], cross_k_norm[l], w_co[l])
        h = h + _hier_moe(h, norm_ffn[l], w_group[l], b_group[l], w_expert[l], b_expert[l],
                          w_e_gate[l], w_e_up[l], w_e_down[l])
    return h
```

```python
import contextlib
import numpy as np
import concourse.bass as bass
import concourse.mybir as mybir
from concourse.bass_utils import run_bass_kernel_spmd

F32 = mybir.dt.float32
BF16 = mybir.dt.bfloat16
I32 = mybir.dt.int32
AF = mybir.ActivationFunctionType
ALU = mybir.AluOpType
AX = mybir.AxisListType

T = 4096
NT = 32
D = 1024
KC = 8
EPS = 1e-6
IN_W = 3088
LAM_INIT = 0.2
SEM_CHUNK = 16000
SAME_ENGINE_SYNC = True


class Prog:
    def __init__(self, nc):
        self.nc = nc
        self.es = contextlib.ExitStack()
        self.insts = []
        self.engs = {"pe": nc.tensor, "act": nc.scalar, "dve": nc.vector, "pool": nc.gpsimd, "sp": nc.sync}
        self.slot_sems = {}
        self.slot_vals = {}

    def sb(self, name, shape, dtype):
        return self.es.enter_context(self.nc.sbuf_tensor(name, list(shape), dtype))

    def ps(self, name, shape, dtype):
        return self.es.enter_context(self.nc.psum_tensor(name, list(shape), dtype))

    LOOPVARS = {'t', 'tt', 'g', 'h', 'p', 'hh', 'c', 'cs', 'rows', 'kt', 'qc', 'm', 'qs', 's', 'r', 'q0', 'bk', 'ba', 'bb', 'bv', 'bo',
                'b1', 'b2', 'o1', 'o2', 'tok0', 'ws', 'xs', 's2', 'i', 'j', 'a', 'ch', 'bu', 'snap', 'par', 'Sba', 'Sbb', 'els', 'dcol',
                'oc', 'lc', 'lt', 'o4', 'dst', 'src', 'ee', 'sc', 'nm', 'col0', 'c0', 'last', 'ps_', 'hf', 'e', 'blk', 'half', 'hc',
                'k', 'kc', 'mt', 'dc', 'eb', 'wsl', 'xb', 'hb'}

    def _chk(self, fn):
        bad = set(fn.__code__.co_freevars) & self.LOOPVARS
        assert not bad, ("late-bound loop variable in lambda", bad, fn.__code__.co_firstlineno)

    def op(self, eng, fn, reads=(), writes=()):
        self._chk(fn)
        self.insts.append(dict(kind="op", eng=eng, fn=fn, reads=tuple(reads), writes=tuple(writes)))

    def dma(self, eng, fn, reads=(), writes=(), slot=None, n=1):
        assert slot is not None
        self._chk(fn)
        self.insts.append(dict(kind="dma", eng=eng, fn=fn, reads=tuple(reads), writes=tuple(writes), slot=slot, n=n))

    def rename(self, old_keys, new_keys):
        self.insts.append(dict(kind="rename", old=tuple(old_keys), new=tuple(new_keys)))

    def fence(self):
        self.insts.append(dict(kind="fence"))

    def emit(self):
        nc = self.nc
        last_w = {}
        readers = {}
        eng_count = {e: 0 for e in self.engs}
        marked = {e: set() for e in self.engs}
        slot_val = {}
        fence_toks = []
        seen_since_fence = set()
        for ins in self.insts:
            if ins["kind"] == "fence":
                toks = list(fence_toks)
                for k, v in last_w.items():
                    if v is not None:
                        toks.append(v)
                for k, v in readers.items():
                    toks.extend(v)
                best = {}
                for tk in toks:
                    kk = (tk[0], tk[1])
                    if kk not in best or tk[2] > best[kk][2]:
                        best[kk] = tk
                fence_toks = list(best.values())
                seen_since_fence = set(last_w.keys()) | set(readers.keys())
                continue
            if ins["kind"] == "rename":
                toks = []
                for k in ins["old"]:
                    if last_w.get(k) is not None:
                        toks.append(last_w[k])
                    toks.extend(readers.get(k, []))
                    last_w.pop(k, None)
                    readers.pop(k, None)
                for k in ins["new"]:
                    readers.setdefault(k, []).extend(toks)
                continue
            e = ins["eng"]
            idx = eng_count[e]
            eng_count[e] += 1
            deps = set()
            for k in ins["reads"] + ins["writes"]:
                if k not in seen_since_fence:
                    seen_since_fence.add(k)
                    deps.update(fence_toks)
            for k in ins["reads"]:
                if last_w.get(k) is not None:
                    deps.add(last_w[k])
            for k in ins["writes"]:
                if last_w.get(k) is not None:
                    deps.add(last_w[k])
                deps.update(readers.get(k, []))
            if ins["kind"] == "dma":
                s = ins["slot"]
                slot_val[s] = slot_val.get(s, 0) + 16 * ins["n"]
                tok = ("d", s, slot_val[s])
            else:
                tok = ("e", e, idx)
            best = {}
            for tk in deps:
                kk = (tk[0], tk[1])
                if kk not in best or tk[2] > best[kk][2]:
                    best[kk] = tk
            deps = set(best.values())
            ins["idx"] = idx
            ins["tok"] = tok
            ins["deps"] = deps
            for dp in deps:
                if dp[0] == "e":
                    marked[dp[1]].add(dp[2])
            for k in ins["reads"]:
                readers.setdefault(k, []).append(tok)
            for k in ins["writes"]:
                last_w[k] = tok
                readers[k] = []
        for e in self.engs:
            if eng_count[e] > 0:
                marked[e].add(eng_count[e] - 1)
        self._last_idx = {e: eng_count[e] - 1 for e in self.engs if eng_count[e] > 0}
        rank = {}
        for e in self.engs:
            for r, idx in enumerate(sorted(marked[e])):
                rank[(e, idx)] = r
        nchunks = {e: (len(marked[e]) + SEM_CHUNK - 1) // SEM_CHUNK for e in self.engs}
        esems = {e: [self.es.enter_context(nc.semaphore(f"q_{e}_{i}")) for i in range(max(1, nchunks[e]))] for e in self.engs}
        dsems = {}
        for s in slot_val:
            dsems[s] = self.es.enter_context(nc.semaphore(f"d_{len(dsems)}"))
        waited_e = {e: {b: -1 for b in self.engs} for e in self.engs}
        waited_d = {e: {} for e in self.engs}
        nwaits = 0
        ins_kind_last = {}
        for ins in self.insts:
            if ins["kind"] in ("op", "dma"):
                ins_kind_last[ins["eng"]] = ins["kind"]
        trace = {e: [] for e in self.engs}
        for ins in self.insts:
            if ins["kind"] in ("rename", "fence"):
                continue
            e = ins["eng"]
            h = self.engs[e]
            tw = []
            trace[e].append((tw, ins))
            for dp in sorted(ins["deps"], key=str):
                if dp[0] == "e":
                    b, j = dp[1], dp[2]
                    if b == e and (e == "pe" or not SAME_ENGINE_SYNC):
                        continue
                    r = rank[(b, j)]
                    if waited_e[e][b] >= r:
                        continue
                    waited_e[e][b] = r
                    h.wait_ge(esems[b][r // SEM_CHUNK], (r % SEM_CHUNK) + 1)
                    tw.append(("e", b, r + 1))
                    nwaits += 1
                else:
                    s, v = dp[1], dp[2]
                    if waited_d[e].get(s, 0) >= v:
                        continue
                    waited_d[e][s] = v
                    h.wait_ge(dsems[s], v)
                    tw.append(("d", s, v))
                    nwaits += 1
            bi = ins["fn"]()
            if ins["kind"] == "dma":
                bi.then_inc(dsems[ins["slot"]], 16)
            elif (e, ins["idx"]) in rank:
                r = rank[(e, ins["idx"])]
                bi.then_inc(esems[e][r // SEM_CHUNK], 1)
        self.final_slots = {s: (dsems[s], v) for s, v in slot_val.items()}
        for e, li in self._last_idx.items():
            if e == "sp" or ins_kind_last.get(e) == "dma":
                continue
            r = rank[(e, li)]
            nc.sync.wait_ge(esems[e][r // SEM_CHUNK], (r % SEM_CHUNK) + 1)
        for s_, v in slot_val.items():
            nc.sync.wait_ge(dsems[s_], v)
        semv = {}
        pos = {e: 0 for e in self.engs}
        progress = True
        while progress:
            progress = False
            for e in self.engs:
                while pos[e] < len(trace[e]):
                    tw, ins = trace[e][pos[e]]
                    if all(semv.get((w[0], w[1]), 0) >= w[2] for w in tw):
                        if ins["kind"] == "dma":
                            semv[("d", ins["slot"])] = semv.get(("d", ins["slot"]), 0) + 16
                        elif (e, ins["idx"]) in rank:
                            semv[("e", e)] = semv.get(("e", e), 0) + 1
                        pos[e] += 1
                        progress = True
                    else:
                        break
        stuck = {e: (pos[e], len(trace[e])) for e in self.engs if pos[e] < len(trace[e])}
        if stuck:
            for e in stuck:
                tw, ins = trace[e][pos[e]]
                print("DEADLOCK", e, pos[e], tw, ins["reads"], ins["writes"], {k: v for k, v in semv.items()})
            raise RuntimeError("deadlock in generated program")
        self.stats = dict(n_inst=len(self.insts), n_waits=nwaits, marked={e: len(marked[e]) for e in self.engs})


class Arena:
    def __init__(self, buf, nelem):
        self.buf = buf
        self.n = nelem
        self.off = 0

    def reset(self):
        self.off = 0

    def take(self, shape, dtype):
        free = 1
        for d_ in shape[1:]:
            free *= d_
        ne = free * (2 if dtype == F32 else 1)
        ne = (ne + 15) // 16 * 16
        assert self.off + ne <= self.n, ("arena overflow", self.off, ne, self.n)
        ap = self.buf[0:shape[0], self.off:self.off + (free * (2 if dtype == F32 else 1))]
        self.off += ne
        if dtype == F32:
            ap = ap.bitcast(F32)
        if len(shape) == 3:
            ap = ap.rearrange("p (a b) -> p a b", a=shape[1])
        elif len(shape) == 4:
            ap = ap.rearrange("p (a b c) -> p a b c", a=shape[1], b=shape[2])
        return ap


NCONST = 3236
CAP = 512
NSLOT = 32 * CAP


def build(stage="full"):
    nc = bass.Bass("TRN2", target_bir_lowering=False)
    P = Prog(nc)

    def din(name, shape):
        return nc.dram_tensor(name, list(shape), F32, kind="ExternalInput").ap()

    x = din("x", [T, D])
    mem = din("mem", [256, D])
    norm_mix = din("norm_mix", [1, D])
    w_in = din("w_in", [D, IN_W])
    da_q_norm = din("da_q_norm", [1, 64])
    da_k_norm = din("da_k_norm", [1, 64])
    lq1 = din("lambda_q1", [1, 64])
    lk1 = din("lambda_k1", [1, 64])
    lq2 = din("lambda_q2", [1, 64])
    lk2 = din("lambda_k2", [1, 64])
    da_out_norm = din("da_out_norm", [1, 128])
    gla_gate_w = din("gla_gate_w", [16, 256])
    gla_gate_b = din("gla_gate_b", [1, 256])
    gla_out_norm = din("gla_out_norm", [1, 128])
    w_o = din("w_o", [D, D])
    norm_cross = din("norm_cross", [1, D])
    norm_mem = din("norm_mem", [1, D])
    w_cq = din("w_cq", [D, D])
    w_ckv = din("w_ckv", [D, 2 * D])
    cross_q_norm = din("cross_q_norm", [1, 256])
    cross_k_norm = din("cross_k_norm", [1, 256])
    w_co = din("w_co", [D, D])
    norm_ffn = din("norm_ffn", [1, D])
    w_group = din("w_group", [D, 4])
    b_group = din("b_group", [1, 4])
    w_expert = din("w_expert", [D, 32])
    b_expert = din("b_expert", [1, 32])
    w_e_gate = din("w_e_gate", [32, D, 512])
    w_e_up = din("w_e_up", [32, D, 512])
    w_e_down = din("w_e_down", [32, 512, D])
    consts = din("consts", [128, NCONST])
    out = nc.dram_tensor("out", [T, D], F32, kind="ExternalOutput").ap()
    x_pad = nc.dram_tensor("x_pad", [NSLOT, D], BF16, kind="Internal").ap()
    o_pad = nc.dram_tensor("o_pad", [NSLOT, D], F32, kind="Internal").ap()
    if stage in ("da", "gla"):
        dbg = nc.dram_tensor("dbg", [128, 4, T], BF16, kind="ExternalOutput").ap()

    UT = P.sb("UT", [128, KC, T], BF16)
    mixT = P.sb("mixT", [128, KC, T], BF16)
    ARENA_N = 35 * 1024
    arena_t = P.sb("arena", [128, ARENA_N], BF16)
    A = Arena(arena_t, ARENA_N)
    cbf = P.sb("cbf", [128, 256 + 5 * 128], BF16)
    cf32 = P.sb("cf32", [128, 292], F32)
    TriS = cbf[:, 640:768]
    ones_bf = cbf[:, 768:896]
    iota32 = cf32[:, 256:288]
    ident = cbf[:, 0:128]
    bones = cbf[:, 128:256]
    Trin = cbf[:, 256:384]
    TriCn = cbf[:, 384:512]
    TriEn = cbf[:, 512:640]
    maskP = cf32[:, 0:128]
    maskF = cf32[:, 128:256]
    P.dma("pool", lambda: nc.gpsimd.dma_start(out=cbf[:, 0:256], in_=consts[:, 0:256]), writes=["cbf"], slot="c0")
    P.dma("pool", lambda: nc.gpsimd.dma_start(out=cbf[:, 256:640], in_=consts[:, 2304:2304 + 384]), writes=["cbf"], slot="c1")
    P.dma("pool", lambda: nc.gpsimd.dma_start(out=cbf[:, 640:896], in_=consts[:, 2944:3200]), writes=["cbf"], slot="c1b")
    P.dma("sp", lambda: nc.sync.dma_start(out=cf32[:, 0:256], in_=consts[:, 2688:2688 + 256]), writes=["cf32"], slot="c2")
    P.dma("sp", lambda: nc.sync.dma_start(out=cf32[:, 256:292], in_=consts[:, 3200:3236]), writes=["cf32"], slot="c2b")

    gon = P.sb("gon", [128, 128], F32)
    P.dma("sp", lambda: nc.sync.dma_start(out=gon[:], in_=da_out_norm.partition_broadcast(128)), writes=["gon"], slot="c4")
    P.op("dve", lambda: nc.vector.tensor_scalar(out=gon[:], in0=gon[:], scalar1=1.0 - LAM_INIT, scalar2=None, op0=ALU.mult),
         reads=["gon"], writes=["gon"])
    ggl = P.sb("ggl", [128, 128], F32)
    P.dma("sp", lambda: nc.sync.dma_start(out=ggl[:], in_=gla_out_norm.partition_broadcast(128)), writes=["ggl"], slot="c4b")
    gq = P.sb("gq", [128, 1], F32)
    gk = P.sb("gk", [128, 1], F32)
    for hh in range(2):
        P.dma("sp", lambda hh=hh: nc.sync.dma_start(out=gq[hh * 64:(hh + 1) * 64, :], in_=da_q_norm.rearrange("o d -> d o")),
              writes=["gq"], slot="c5")
        P.dma("sp", lambda hh=hh: nc.sync.dma_start(out=gk[hh * 64:(hh + 1) * 64, :], in_=da_k_norm.rearrange("o d -> d o")),
              writes=["gk"], slot="c6")
    P.op("dve", lambda: nc.vector.tensor_scalar(out=gq[:], in0=gq[:], scalar1=0.125, scalar2=None, op0=ALU.mult),
         reads=["gq"], writes=["gq"])
    lam4 = P.sb("lam4", [128, 4, 64], F32)
    for i, a in enumerate((lq1, lk1, lq2, lk2)):
        P.dma("sp", lambda i=i, a=a: nc.sync.dma_start(out=lam4[:, i, :], in_=a.partition_broadcast(128)),
              writes=["lam4"], slot="c7")
    lamw = P.sb("lamw", [128, 2, 64], F32)
    lams = P.sb("lams", [128, 2], F32)
    nlam = P.sb("nlam", [128, 1], F32)
    P.op("dve", lambda: nc.vector.tensor_tensor(out=lamw[:], in0=lam4[:, 0:4:2, :], in1=lam4[:, 1:4:2, :], op=ALU.mult),
         reads=["lam4"], writes=["lamw"])
    P.op("dve", lambda: nc.vector.reduce_sum(out=lams[:], in_=lamw[:], axis=AX.X), reads=["lamw"], writes=["lams"])
    P.op("act", lambda: nc.scalar.activation(out=lams[:], in_=lams[:], func=AF.Exp), reads=["lams"], writes=["lams"])
    P.op("dve", lambda: nc.vector.scalar_tensor_tensor(out=nlam[:], in0=lams[:, 1:2], scalar=-LAM_INIT, in1=lams[:, 0:1],
                                                        op0=ALU.add, op1=ALU.subtract),
         reads=["lams"], writes=["nlam"])

    bigs = [P.ps(f"big{j}", [128, 1024], F32) for j in range(4)]
    bigv = [b[:] for b in bigs]
    zt = P.sb("zt", [128, D], BF16)
    P.op("dve", lambda: nc.vector.memset(zt[:], 0.0), writes=["zt"])
    xp_z = x_pad.rearrange("(n p) d -> n p d", p=128)
    for zi in range(NSLOT // 128):
        P.dma("sp", lambda zi=zi: nc.sync.dma_start(out=xp_z[zi], in_=zt[:]), reads=["zt"], writes=["x_pad"], slot="xz")

    banks = []
    for j in range(4):
        banks.append(bigs[j][:, 0:512])
        banks.append(bigs[j][:, 512:1024])

    def bank_bf(i):
        return banks[i].bitcast(BF16)

    gmix = A.take([128, D], F32)
    P.dma("sp", lambda: nc.sync.dma_start(out=gmix, in_=norm_mix.partition_broadcast(128)), writes=["gmix"], slot="c3")
    xts = [A.take([128, D], F32) for i in range(3)]
    ubs = [A.take([128, D], BF16) for i in range(2)]
    junk = A.take([128, D], BF16)
    ssq = [A.take([128, 1], F32) for i in range(2)]
    rstd = [A.take([128, 1], F32) for i in range(2)]

    for t in range(NT):
        xs = t % 3
        s2 = t % 2
        P.dma("sp", lambda t=t, xs=xs: nc.sync.dma_start(out=xts[xs], in_=x[t * 128:(t + 1) * 128, :]),
              writes=[("xt", xs)], slot=("xt", xs))
        P.op("act", lambda xs=xs, s2=s2: nc.scalar.activation(out=junk, in_=xts[xs], func=AF.Square, accum_out=ssq[s2]),
             reads=[("xt", xs)], writes=["junk", ("ssq", s2)])
        P.op("act", lambda s2=s2: nc.scalar.activation(out=rstd[s2], in_=ssq[s2], func=AF.Ln, bias=EPS, scale=1.0 / D),
             reads=[("ssq", s2)], writes=[("rstd", s2)])
        P.op("act", lambda s2=s2: nc.scalar.activation(out=rstd[s2], in_=rstd[s2], func=AF.Exp, scale=-0.5),
             reads=[("rstd", s2)], writes=[("rstd", s2)])
        P.op("dve", lambda xs=xs, s2=s2: nc.vector.scalar_tensor_tensor(out=ubs[s2], in0=xts[xs], scalar=rstd[s2][:, 0:1],
                                                                         in1=gmix, op0=ALU.mult, op1=ALU.mult),
             reads=[("xt", xs), ("rstd", s2), "gmix"], writes=[("ub", s2)])
        bk = t % 2
        for c in range(KC):
            P.op("pe", lambda c=c, s2=s2, bk=bk: nc.tensor.transpose(out=bank_bf(bk)[:, c * 128:(c + 1) * 128],
                                                                      in_=ubs[s2][:, c * 128:(c + 1) * 128], identity=ident),
                 reads=[("ub", s2), "cbf"], writes=[("bank", bk)])
        if t % 2 == 0:
            P.op("act", lambda t=t, bk=bk: nc.scalar.copy(out=UT[:, :, t * 128:(t + 1) * 128],
                                                          in_=bank_bf(bk).rearrange("p (c n) -> p c n", c=KC)),
                 reads=[("bank", bk)], writes=[("UT", t)])
        else:
            P.op("dve", lambda t=t, bk=bk: nc.vector.tensor_copy(out=UT[:, :, t * 128:(t + 1) * 128],
                                                                  in_=bank_bf(bk).rearrange("p (c n) -> p c n", c=KC)),
                 reads=[("bank", bk)], writes=[("UT", t)])

    w_in_v = w_in.rearrange("(c p) n -> p c n", p=128)

    P.fence()
    A.reset()
    wda = [A.take([128, KC, 384], BF16) for i in range(2)]
    QT = A.take([128, T], BF16)
    KT = A.take([128, T], BF16)
    V = A.take([128, NT, 132], BF16)
    sqb = [A.take([128, 512], BF16) for i in range(2)]
    rsb = [A.take([128, 512], F32) for i in range(2)]
    pts = [[A.take([128, 512], BF16) for i in range(3)] for m in range(2)]
    rec = A.take([128, 2, 2], F32)
    t1 = A.take([128, 2, 128], F32)
    dd = A.take([128, 2, 128], F32)
    dsq = A.take([128, 2], F32)
    drs = A.take([128, 2], F32)
    ob = A.take([128, 2, 128], BF16)
    junk2 = A.take([128, 128], BF16)
    P.op("pool", lambda: nc.gpsimd.memset(V, 1.0), writes=["Vones"])

    n_heads = 4 if stage != "gla" else 0
    for h in range(n_heads):
        ws = h % 2
        for j, col0 in enumerate((h * 128, 512 + h * 128, 1024 + h * 128)):
            P.dma("pool", lambda ws=ws, j=j, col0=col0: nc.gpsimd.dma_start(out=wda[ws][:, :, j * 128:(j + 1) * 128],
                                                                             in_=w_in_v[:, :, col0:col0 + 128]),
                  writes=[("wda", ws)], slot=("wda", ws, j))
        for tc in range(8):
            for qk, (dst, gcol, dname) in enumerate(((QT, gq, "QT"), (KT, gk, "KT"))):
                ba = (2 * tc + qk) % 2
                bb = 2 + ba
                for c in range(KC):
                    P.op("pe", lambda c=c, ba=ba, ws=ws, qk=qk, tc=tc: nc.tensor.matmul(
                        banks[ba][:], wda[ws][:, c, qk * 128:(qk + 1) * 128], UT[:, c, tc * 512:(tc + 1) * 512],
                        start=(c == 0), stop=(c == KC - 1)),
                        reads=[("wda", ws)] + [("UT", 4 * tc + i) for i in range(4)], writes=[("bank", ba)])
                P.op("act", lambda ba=ba: nc.scalar.activation(out=sqb[ba], in_=banks[ba][:], func=AF.Square),
                     reads=[("bank", ba)], writes=[("sqb", ba)])
                P.op("pe", lambda ba=ba, bb=bb: nc.tensor.matmul(banks[bb][:], bones, sqb[ba], start=True, stop=True),
                     reads=["cbf", ("sqb", ba)], writes=[("bank", bb)])
                P.op("act", lambda ba=ba, bb=bb: nc.scalar.activation(out=rsb[ba], in_=banks[bb][:], func=AF.Ln, bias=EPS),
                     reads=[("bank", bb)], writes=[("rsb", ba)])
                P.op("act", lambda ba=ba: nc.scalar.activation(out=rsb[ba], in_=rsb[ba], func=AF.Exp, scale=-0.5),
                     reads=[("rsb", ba)], writes=[("rsb", ba)])
                P.op("dve", lambda ba=ba, dst=dst, gcol=gcol, tc=tc: nc.vector.scalar_tensor_tensor(
                    out=dst[:, tc * 512:(tc + 1) * 512], in0=banks[ba][:], scalar=gcol[:, 0:1], in1=rsb[ba],
                    op0=ALU.mult, op1=ALU.mult),
                    reads=[("bank", ba), ("rsb", ba), "gq" if qk == 0 else "gk"],
                    writes=[(dname, tc)])
        for g4 in range(8):
            bv = 4 + (g4 % 2)
            for i in range(4):
                t = g4 * 4 + i
                for c in range(KC):
                    P.op("pe", lambda c=c, t=t, i=i, bv=bv, ws=ws: nc.tensor.matmul(
                        banks[bv][:, i * 128:(i + 1) * 128], UT[:, c, t * 128:(t + 1) * 128], wda[ws][:, c, 256:384],
                        start=(c == 0), stop=(c == KC - 1)),
                        reads=[("wda", ws), ("UT", t)], writes=[("bank", bv)])
            P.op("act", lambda g4=g4, bv=bv: nc.scalar.copy(out=V[:, g4 * 4:(g4 + 1) * 4, 0:128],
                                                            in_=banks[bv][:].rearrange("p (a b) -> p a b", a=4)),
                 reads=[("bank", bv), "Vones"], writes=[("V", g4)])

        for qc in range(8):
            nkt = 4 * qc + 4

            def emit_S(kt, qc=qc):
                s = kt % 2
                r = kt - 4 * qc
                q0 = 128 * r if r > 0 else 0
                for m in range(2):
                    bk = 2 * s + m
                    P.op("pe", lambda m=m, bk=bk, kt=kt, q0=q0, qc=qc: nc.tensor.matmul(
                        banks[bk][:, q0:512], KT[m * 64:(m + 1) * 64, kt * 128:(kt + 1) * 128],
                        QT[m * 64:(m + 1) * 64, qc * 512 + q0:(qc + 1) * 512], start=True, stop=True),
                        reads=[("KT", kt // 4), ("QT", qc)], writes=[("bank", bk)])

            emit_S(0)
            for kt in range(nkt):
                s = kt % 2
                ps_ = kt % 3
                r = kt - 4 * qc
                q0 = 128 * r if r > 0 else 0
                for m in range(2):
                    bk = 2 * s + m
                    P.op("act", lambda m=m, bk=bk, ps_=ps_, q0=q0: nc.scalar.activation(
                        out=pts[m][ps_][:, q0:512], in_=banks[bk][:, q0:512], func=AF.Exp),
                        reads=[("bank", bk)], writes=[("pt", m, ps_)])
                    if r >= 0:
                        P.op("pool", lambda m=m, ps_=ps_, q0=q0, r=r: nc.gpsimd.memset(
                            pts[m][ps_][64:128, 128 * r:128 * r + 64], 0.0),
                            writes=[("pt", m, ps_)])
                if kt + 1 < nkt:
                    emit_S(kt + 1)
                qs0 = r if r > 0 else 0
                for qs in range(qs0, 4):
                    last = 4 * qc + qs
                    for m in range(2):
                        bo = 4 + 2 * m + qs // 2
                        P.op("pe", lambda m=m, bo=bo, qs=qs, kt=kt, ps_=ps_, last=last: nc.tensor.matmul(
                            banks[bo][:, (qs % 2) * 129:(qs % 2) * 129 + 129], pts[m][ps_][:, qs * 128:(qs + 1) * 128],
                            V[:, kt, 0:129], start=(kt == 0 and qs % 2 == 0), stop=(kt == last), skip_group_check=True),
                            reads=[("pt", m, ps_), ("V", kt // 4), "Vones"], writes=[("bank", bo)])
                for hf in range(2):
                    if kt == 4 * qc + 2 * hf + 1:
                        b1 = 4 + hf
                        b2 = 6 + hf
                        o1 = banks[b1][:, 0:258].rearrange("p (a b) -> p a b", a=2)
                        o2 = banks[b2][:, 0:258].rearrange("p (a b) -> p a b", a=2)
                        tok0 = qc * 512 + hf * 256
                        P.op("dve", lambda o1=o1: nc.vector.reciprocal(out=rec[:, 0, :], in_=o1[:, :, 128]),
                             reads=[("bank", b1)], writes=["rec0"])
                        P.op("dve", lambda o2=o2: nc.vector.reciprocal(out=rec[:, 1, :], in_=o2[:, :, 128]),
                             reads=[("bank", b2)], writes=["rec1"])
                        P.op("dve", lambda: nc.vector.tensor_scalar(out=rec[:, 1, :], in0=rec[:, 1, :], scalar1=nlam[:, 0:1],
                                                                    scalar2=None, op0=ALU.mult),
                             reads=["rec1", "nlam"], writes=["rec1"])
                        P.op("dve", lambda o1=o1: nc.vector.tensor_tensor(
                            out=t1, in0=o1[:, :, 0:128], in1=rec[:, 0, :].unsqueeze(2).to_broadcast([128, 2, 128]), op=ALU.mult),
                            reads=[("bank", b1), "rec0"], writes=["t1"])
                        P.op("dve", lambda o2=o2: nc.vector.tensor_tensor(
                            out=dd, in0=o2[:, :, 0:128], in1=rec[:, 1, :].unsqueeze(2).to_broadcast([128, 2, 128]), op=ALU.mult),
                            reads=[("bank", b2), "rec1"], writes=["dd"])
                        P.op("dve", lambda: nc.vector.tensor_tensor(out=dd, in0=dd, in1=t1, op=ALU.add),
                             reads=["dd", "t1"], writes=["dd"])
                        for a in range(2):
                            P.op("act", lambda a=a: nc.scalar.activation(out=junk2, in_=dd[:, a, :], func=AF.Square,
                                                                          accum_out=dsq[:, a:a + 1]),
                                 reads=["dd"], writes=["junk2", ("dsq", a)])
                        P.op("act", lambda: nc.scalar.activation(out=drs, in_=dsq, func=AF.Ln, bias=EPS, scale=1.0 / 128),
                             reads=[("dsq", 0), ("dsq", 1)], writes=["drs"])
                        P.op("act", lambda: nc.scalar.activation(out=drs, in_=drs, func=AF.Exp, scale=-0.5),
                             reads=["drs"], writes=["drs"])
                        for a in range(2):
                            P.op("dve", lambda a=a: nc.vector.scalar_tensor_tensor(
                                out=ob[:, a, :], in0=dd[:, a, :], scalar=drs[:, a:a + 1], in1=gon[:], op0=ALU.mult, op1=ALU.mult),
                                reads=["dd", "drs", "gon"], writes=[("ob", a)])
                        for a in range(2):
                            P.op("pe", lambda a=a, b1=b1: nc.tensor.transpose(out=bank_bf(b1)[:, a * 128:(a + 1) * 128],
                                                                              in_=ob[:, a, :], identity=ident),
                                 reads=[("ob", a), "cbf"], writes=[("bank", b1)])
                        P.op("act", lambda b1=b1, tok0=tok0, h=h: nc.scalar.copy(out=mixT[:, h, tok0:tok0 + 256],
                                                                                  in_=bank_bf(b1)[:, 0:256]),
                             reads=[("bank", b1)], writes=[("mixT", h, tok0 // 256)])

    if stage == "da":
        for h in range(n_heads):
            P.dma("sp", lambda h=h: nc.sync.dma_start(out=dbg[:, h, :], in_=mixT[:, h, :]),
                  reads=[("mixT", h, i) for i in range(16)], slot="out")
        P.emit()
        sem, v = P.final_slots["out"]
        nc.sync.wait_ge(sem, v)
        return nc, P

    P.fence()
    A.reset()
    wg = A.take([128, KC, 1552], BF16)
    for j in range(4):
        c0 = 1536 + j * 388
        P.dma("pool", lambda j=j, c0=c0: nc.gpsimd.dma_start(out=wg[:, :, j * 388:(j + 1) * 388], in_=w_in_v[:, :, c0:c0 + 388]),
              writes=["wg"], slot=("wg", j))
    gwa = A.take([32, 256], BF16)
    P.dma("pool", lambda: nc.gpsimd.dma_start(out=gwa[0:16, :], in_=gla_gate_w), writes=["gwa"], slot="gwa0")
    P.dma("pool", lambda: nc.gpsimd.dma_start(out=gwa[16:17, :], in_=gla_gate_b), writes=["gwa"], slot="gwa1")
    grT = A.take([32, 256], BF16)
    P.op("pool", lambda: nc.gpsimd.memset(grT, 1.0), writes=["grT1"])
    spe = A.take([128, 256], F32)
    spb = A.take([128, 256], BF16)
    EP = A.take([128, 2, 256], F32)
    EN = A.take([128, 2, 256], F32)
    ELa = A.take([128, 2, 256], F32)
    ELb = A.take([128, 2, 256], F32)
    P.op("pool", lambda: nc.gpsimd.memset(ELa, 0.0), writes=["ELa0"])
    P.op("pool", lambda: nc.gpsimd.memset(ELb, 0.0), writes=["ELb0"])
    QP = A.take([128, 2, 256], BF16)
    QN = A.take([128, 2, 256], BF16)
    KNh = [A.take([128, 2, 256], BF16) for i in range(2)]
    KPh = [A.take([128, 2, 256], BF16) for i in range(2)]
    QLah = [A.take([128, 2, 256], BF16) for i in range(2)]
    QLbh = [A.take([128, 2, 256], BF16) for i in range(2)]
    EE = [A.take([128, 256], F32) for i in range(2)]
    KE = [[A.take([128, 256], BF16) for ch in range(2)] for i in range(2)]
    Vg = [A.take([128, 512], BF16) for i in range(2)]
    sg = [A.take([128, 4, 128], F32) for i in range(2)]
    sge = A.take([128, 512], F32)
    at1 = A.take([128, 4, 128], F32)
    at2 = A.take([128, 4, 128], F32)
    ATb = A.take([128, 4, 128], BF16)
    Sst = [A.take([128, 128], F32) for p in range(2)]
    Sba2 = [[A.take([128, 128], BF16) for p in range(2)] for par in range(2)]
    Sbb2 = [[A.take([128, 128], BF16) for p in range(2)] for par in range(2)]
    osq = A.take([128, 4, 128], F32)
    oss = A.take([128, 4], F32)
    ors = A.take([128, 4], F32)
    otm = A.take([128, 4, 128], F32)
    ogb = A.take([128, 4, 128], BF16)
    for p in range(2):
        P.op("pool", lambda p=p: nc.gpsimd.memset(Sst[p], 0.0), writes=[("S", p)])
        P.op("pool", lambda p=p: nc.gpsimd.memset(Sba2[0][p], 0.0), writes=[("Sba", 0, p)])

    import os
    if stage == "gla":
        P.op("pool", lambda: nc.gpsimd.memset(mixT[:, 4:8, :], 0.0), writes=[("mixTg", i) for i in range(NT)])
    CUT = int(os.environ.get('GLA_CUT', '99'))
    NG = int(os.environ.get('GLA_NG', '16'))

    class _Cut(Exception):
        pass

    CUTT = int(os.environ.get('GLA_CUTT', '0'))
    cur = {"t": 0}

    def cut(k):
        if CUT == k and cur["t"] == CUTT:
            raise _Cut()

    def gla_all():
      for g in range(NG):
          P.op("pe", lambda: nc.tensor.matmul(banks[0][:, 0:128], ident, ident, start=True, stop=True, skip_group_check=True),
               reads=["cbf"], writes=[("bank", 0)])
          for c in range(KC):
              P.op("pe", lambda c=c, g=g: nc.tensor.matmul(banks[0][0:16, 0:256], wg[:, c, 1536:1552], UT[:, c, g * 256:(g + 1) * 256],
                                                            start=(c == 0), stop=(c == KC - 1)),
                   reads=["wg", ("UT", 2 * g), ("UT", 2 * g + 1)], writes=[("bank", 0)])
          P.op("act", lambda: nc.scalar.copy(out=grT[0:16, :], in_=banks[0][0:16, 0:256]),
               reads=[("bank", 0), "grT1"], writes=["grT"])
          for tt in range(2):
              t = 2 * g + tt
              cur["t"] = t
              cs = slice(tt * 128, (tt + 1) * 128)
              cut(1)
              P.op("pe", lambda cs=cs: nc.tensor.matmul(banks[0][:, 256:512], grT[0:17, cs], gwa[0:17, :], start=True, stop=True),
                   reads=["grT", "gwa"], writes=[("bank", 0)])
              P.op("act", lambda: nc.scalar.activation(out=spe, in_=banks[0][:, 256:512], func=AF.Exp, scale=-1.0),
                   reads=[("bank", 0)], writes=["spe"])
              P.op("act", lambda: nc.scalar.activation(out=spb, in_=spe, func=AF.Ln, bias=1.0),
                   reads=["spe"], writes=["spb"])
              cut(2)
              for p in range(2):
                  P.op("pe", lambda p=p: nc.tensor.matmul(banks[1][:, p * 128:(p + 1) * 128], spb[:, p * 128:(p + 1) * 128], TriCn,
                                                           start=True, stop=True),
                       reads=["spb", "cbf"], writes=[("bank", 1)])
              for p in range(2):
                  P.op("pe", lambda p=p: nc.tensor.matmul(banks[1][:, 256 + p * 128:256 + (p + 1) * 128],
                                                           spb[:, p * 128:(p + 1) * 128], Trin, start=True, stop=True),
                       reads=["spb", "cbf"], writes=[("bank", 1)])
              P.op("pe", lambda: nc.tensor.matmul(banks[2][:, 0:256], TriEn, spb, start=True, stop=True),
                   reads=["spb", "cbf"], writes=[("bank", 2)])
              lc = banks[1][:, 0:256].rearrange("p (a b) -> p a b", a=2)
              lt = banks[1][:, 256:512].rearrange("p (a b) -> p a b", a=2)
              P.op("act", lambda lc=lc, cs=cs: nc.scalar.activation(out=EP[:, :, cs], in_=lc, func=AF.Exp),
                   reads=[("bank", 1)], writes=["EP"])
              P.op("act", lambda lc=lc, cs=cs: nc.scalar.activation(out=EN[:, :, cs], in_=lc, func=AF.Exp, scale=-1.0),
                   reads=[("bank", 1)], writes=["EN"])
              P.op("act", lambda lt=lt, tt=tt: nc.scalar.activation(out=ELa[:, :, tt * 128:tt * 128 + 64], in_=lt[:, :, 0:64], func=AF.Exp),
                   reads=[("bank", 1), "ELa0"], writes=["ELa"])
              P.op("act", lambda lt=lt, tt=tt: nc.scalar.activation(out=ELb[:, :, tt * 128 + 64:tt * 128 + 128], in_=lt[:, :, 64:128], func=AF.Exp),
                   reads=[("bank", 1), "ELb0"], writes=["ELb"])
              P.op("act", lambda tt=tt: nc.scalar.activation(out=EE[tt], in_=banks[2][:, 0:256], func=AF.Exp),
                   reads=[("bank", 2)], writes=[("EE", tt)])
              cut(3)
              for c in range(KC):
                  P.op("pe", lambda c=c, t=t: nc.tensor.matmul(banks[2][:, 256:512], UT[:, c, t * 128:(t + 1) * 128], wg[:, c, 256:512],
                                                                start=(c == 0), stop=(c == KC - 1), skip_group_check=True),
                       reads=["wg", ("UT", t)], writes=[("bank", 2)])
              for c in range(KC):
                  P.op("pe", lambda c=c, t=t: nc.tensor.matmul(banks[5][:], UT[:, c, t * 128:(t + 1) * 128], wg[:, c, 512:1024],
                                                                start=(c == 0), stop=(c == KC - 1)),
                       reads=["wg", ("UT", t)], writes=[("bank", 5)])
              for c in range(KC):
                  P.op("pe", lambda c=c, t=t: nc.tensor.matmul(banks[6][:], UT[:, c, t * 128:(t + 1) * 128], wg[:, c, 1024:1536],
                                                                start=(c == 0), stop=(c == KC - 1)),
                       reads=["wg", ("UT", t)], writes=[("bank", 6)])
              for ch in range(2):
                  P.op("dve", lambda tt=tt, ch=ch: nc.vector.scalar_tensor_tensor(out=KE[tt][ch], in0=banks[2][:, 256:512],
                                                                                  scalar=cf32[:, 288 + ch:289 + ch], in1=EE[tt],
                                                                                  op0=ALU.mult, op1=ALU.mult),
                       reads=[("bank", 2), ("EE", tt), "cf32"], writes=[("KE", tt)])
              P.op("act", lambda tt=tt: nc.scalar.copy(out=Vg[tt], in_=banks[5][:]), reads=[("bank", 5)], writes=[("Vg", tt)])
              cut(4)
              P.op("act", lambda: nc.scalar.activation(out=sge, in_=banks[6][:], func=AF.Exp, scale=-1.0),
                   reads=[("bank", 6)], writes=["sge"])
              P.op("dve", lambda: nc.vector.tensor_scalar(out=sge, in0=sge, scalar1=1.0, scalar2=None, op0=ALU.add),
                   reads=["sge"], writes=["sge"])
              P.op("dve", lambda: nc.vector.reciprocal(out=sge, in_=sge), reads=["sge"], writes=["sge"])
              P.op("dve", lambda tt=tt: nc.vector.tensor_tensor(out=sg[tt].rearrange("p a b -> p (a b)"), in0=banks[6][:], in1=sge, op=ALU.mult),
                   reads=[("bank", 6), "sge"], writes=[("sg", tt)])
              P.op("dve", lambda tt=tt: nc.vector.tensor_tensor(out=sg[tt], in0=sg[tt],
                                                                  in1=ggl[:].unsqueeze(1).to_broadcast([128, 4, 128]), op=ALU.mult),
                   reads=[("sg", tt), "ggl"], writes=[("sg", tt)])
          cut(5)
          for qk, bk in ((0, 3), (1, 4)):
              for p in range(2):
                  for c in range(KC):
                      P.op("pe", lambda c=c, p=p, qk=qk, bk=bk, g=g: nc.tensor.matmul(
                          banks[bk][:, p * 256:(p + 1) * 256], wg[:, c, qk * 256 + p * 128:qk * 256 + (p + 1) * 128],
                          UT[:, c, g * 256:(g + 1) * 256], start=(c == 0), stop=(c == KC - 1), skip_group_check=True),
                          reads=["wg", ("UT", 2 * g), ("UT", 2 * g + 1)], writes=[("bank", bk)])
          qv = banks[3][:].rearrange("p (a b) -> p a b", a=2)
          kv = banks[4][:].rearrange("p (a b) -> p a b", a=2)
          for dst, src, ee, nm, sc in ((QP, qv, EP, "QP", 0.125), (QN, qv, EN, "QN", 0.125)):
              P.op("dve", lambda dst=dst, src=src, ee=ee, sc=sc: nc.vector.scalar_tensor_tensor(
                  out=dst, in0=src, scalar=sc, in1=ee, op0=ALU.mult, op1=ALU.mult),
                  reads=[("bank", 3), {"QP": "EP", "QN": "EN"}[nm]], writes=[nm])
          for hh in range(2):
              for dst, src, ee, nm, ekey, mcol, bk in ((QLah[hh], qv, ELa, "QLa", "ELa", 290 + hh, 3), (QLbh[hh], qv, ELb, "QLb", "ELb", 290 + hh, 3),
                                                       (KNh[hh], kv, EN, "KN", "EN", 288 + hh, 4), (KPh[hh], kv, EP, "KP", "EP", 288 + hh, 4)):
                  P.op("dve", lambda dst=dst, src=src, ee=ee, mcol=mcol: nc.vector.scalar_tensor_tensor(
                      out=dst, in0=src, scalar=cf32[:, mcol:mcol + 1], in1=ee, op0=ALU.mult, op1=ALU.mult),
                      reads=[("bank", bk), ekey, "cf32"], writes=[(nm, hh)])
          for tt in range(2):
              t = 2 * g + tt
              cur["t"] = t
              cs = slice(tt * 128, (tt + 1) * 128)
              cut(6)
              P.op("pe", lambda: nc.tensor.matmul(banks[5][:, 0:128], ident, ident, start=True, stop=True, skip_group_check=True),
                   reads=["cbf"], writes=[("bank", 5)])
              for h in range(4):
                  p, hh = h // 2, h % 2
                  rows = slice(hh * 64, (hh + 1) * 64)
                  P.op("pe", lambda h=h, p=p, hh=hh, cs=cs: nc.tensor.matmul(
                      banks[5][:, h * 128:(h + 1) * 128], KNh[hh][:, p, cs], QP[:, p, cs], start=True, stop=True, skip_group_check=True),
                      reads=[("KN", hh), "QP"], writes=[("bank", 5)])
                  P.op("pe", lambda h=h, p=p, hh=hh, cs=cs: nc.tensor.matmul(
                      banks[6][:, h * 128:(h + 1) * 128], KPh[hh][:, p, cs], QN[:, p, cs], start=True, stop=True, skip_group_check=True),
                      reads=[("KP", hh), "QN"], writes=[("bank", 6)])
              P.op("dve", lambda: nc.vector.tensor_tensor(out=at1, in0=banks[5][:].rearrange("p (a b) -> p a b", a=4),
                                                          in1=maskP.unsqueeze(1).to_broadcast([128, 4, 128]), op=ALU.mult),
                   reads=[("bank", 5), "cf32"], writes=["at1"])
              P.op("dve", lambda: nc.vector.tensor_tensor(out=at2, in0=banks[6][:].rearrange("p (a b) -> p a b", a=4),
                                                          in1=maskF.unsqueeze(1).to_broadcast([128, 4, 128]), op=ALU.mult),
                   reads=[("bank", 6), "cf32"], writes=["at2"])
              P.op("dve", lambda: nc.vector.tensor_tensor(out=ATb, in0=at1, in1=at2, op=ALU.add),
                   reads=["at1", "at2"], writes=["ATb"])
              cut(7)
              for ch, bu in ((0, 7), (1, 4)):
                  rows = slice(ch * 64, (ch + 1) * 64)
                  for p in range(2):
                      P.op("pe", lambda p=p, ch=ch, bu=bu, tt=tt: nc.tensor.matmul(
                          banks[bu][:, p * 256:(p + 1) * 256], KE[tt][ch][:, p * 128:(p + 1) * 128], Vg[tt][:, p * 256:(p + 1) * 256],
                          start=True, stop=True, skip_group_check=True),
                          reads=[("KE", tt), ("Vg", tt)], writes=[("bank", bu)])
              par = t % 2
              Sba, Sbb = Sba2[par], Sbb2[par]

              def upd(ch, bu, snap, sname, tt=tt):
                  dcol = tt * 128 + ch * 64 + 63
                  els = ELa if ch == 0 else ELb
                  for p in range(2):
                      for hh in range(2):
                          if ch == 1 and os.environ.get("GLA_VAR") == "B":
                              continue
                          rows = slice(hh * 64, (hh + 1) * 64)
                          P.op("dve", lambda p=p, rows=rows, bu=bu, els=els, dcol=dcol, hh=hh: nc.vector.scalar_tensor_tensor(
                              out=Sst[p][rows, :], in0=Sst[p][rows, :], scalar=els[rows, p, dcol:dcol + 1],
                              in1=banks[bu][rows, p * 256 + hh * 128:p * 256 + (hh + 1) * 128], op0=ALU.mult, op1=ALU.add),
                              reads=[("S", p), ("bank", bu), "ELa" if ch == 0 else "ELb"], writes=[("S", p)])
                      if ch == 1 and os.environ.get("GLA_VAR") == "A":
                          continue
                      P.op("act", lambda p=p, snap=snap: nc.scalar.copy(out=snap[p], in_=Sst[p]),
                           reads=[("S", p)], writes=[sname + (p,)])

              cut(8)
              upd(0, 7, Sbb, ("Sbb", par))
              cut(9)
              for h in range(4):
                  p, hh = h // 2, h % 2
                  rows = slice(hh * 64, (hh + 1) * 64)
                  oc = slice(h * 128, (h + 1) * 128)
                  P.op("pe", lambda p=p, hh=hh, oc=oc, cs=cs, Sba=Sba: nc.tensor.matmul(banks[1][:, oc], QLah[hh][:, p, cs], Sba[p],
                                                                                    start=True, stop=False, skip_group_check=True),
                       reads=[("QLa", hh), ("Sba", par, p)], writes=[("bank", 1)])
                  P.op("pe", lambda p=p, hh=hh, oc=oc, cs=cs, Sbb=Sbb: nc.tensor.matmul(banks[1][:, oc], QLbh[hh][:, p, cs], Sbb[p],
                                                                                    start=False, stop=False, skip_group_check=True),
                       reads=[("QLb", hh), ("Sbb", par, p)], writes=[("bank", 1)])
                  P.op("pe", lambda h=h, oc=oc, tt=tt: nc.tensor.matmul(banks[1][:, oc], ATb[:, h, :], Vg[tt][:, oc],
                                                                         start=False, stop=True, skip_group_check=True),
                       reads=["ATb", ("Vg", tt)], writes=[("bank", 1)])
              cut(11)
              upd(1, 4, Sba2[1 - par], ("Sba", 1 - par))
              cut(10)
              o4 = banks[1][:].rearrange("p (a b) -> p a b", a=4)
              P.op("act", lambda: nc.scalar.activation(out=osq.rearrange("p a b -> p (a b)"), in_=banks[1][:], func=AF.Square),
                   reads=[("bank", 1)], writes=["osq"])
              P.op("dve", lambda: nc.vector.reduce_sum(out=oss, in_=osq, axis=AX.X), reads=["osq"], writes=["oss"])
              P.op("act", lambda: nc.scalar.activation(out=ors, in_=oss, func=AF.Ln, bias=EPS, scale=1.0 / 128),
                   reads=["oss"], writes=["ors"])
              P.op("act", lambda: nc.scalar.activation(out=ors, in_=ors, func=AF.Exp, scale=-0.5), reads=["ors"], writes=["ors"])
              P.op("dve", lambda o4=o4: nc.vector.tensor_tensor(out=otm, in0=o4, in1=ors.unsqueeze(2).to_broadcast([128, 4, 128]),
                                                                 op=ALU.mult),
                   reads=[("bank", 1), "ors"], writes=["otm"])
              P.op("dve", lambda tt=tt: nc.vector.tensor_tensor(out=ogb, in0=otm, in1=sg[tt], op=ALU.mult),
                   reads=["otm", ("sg", tt)], writes=["ogb"])
              cut(121)
              for h in range(4):
                  P.op("pe", lambda h=h: nc.tensor.transpose(out=bank_bf(3)[:, h * 128:(h + 1) * 128], in_=ogb[:, h, :], identity=ident),
                       reads=["ogb", "cbf"], writes=[("bank", 3)])
              cut(122)
              P.op("act", lambda t=t: nc.scalar.copy(out=mixT[:, 4:8, t * 128:(t + 1) * 128],
                                                     in_=bank_bf(3)[:, 0:512].rearrange("p (a b) -> p a b", a=4)),
                   reads=[("bank", 3)], writes=[("mixTg", t)])


    try:
        gla_all()
    except _Cut:
        pass

    if stage == "gla":
        for h in range(4):
            P.dma("sp", lambda h=h: nc.sync.dma_start(out=dbg[:, h, :], in_=mixT[:, 4 + h, :]),
                  reads=[("mixTg", i) for i in range(NT)], slot="out")
        P.emit()
        sem, v = P.final_slots["out"]
        nc.sync.wait_ge(sem, v)
        return nc, P


    P.fence()
    A.reset()
    A2 = Arena(UT[:].rearrange("p c t -> p (c t)"), KC * T)
    w_o_v = w_o.rearrange("(c p) n -> p c n", p=128)
    w_cq_v = w_cq.rearrange("(c p) n -> p c n", p=128)
    w_co_v = w_co.rearrange("(c p) n -> p c n", p=128)
    w_ckv_v = w_ckv.rearrange("(c p) n -> p c n", p=128)
    wo = A.take([128, KC, D], BF16)
    wcq = A.take([128, KC, D], BF16)
    wco = A.take([128, KC, D], BF16)
    for nm_, dst_, src_ in (("wo", wo, w_o_v), ("wcq", wcq, w_cq_v), ("wco", wco, w_co_v)):
        for hf_ in range(2):
            P.dma("pool", lambda dst_=dst_, src_=src_, hf_=hf_: nc.gpsimd.dma_start(out=dst_[:, :, hf_ * 512:(hf_ + 1) * 512],
                                                                                     in_=src_[:, :, hf_ * 512:(hf_ + 1) * 512]),
                  writes=[nm_], slot=(nm_, hf_))
    gcross = A.take([128, D], F32)
    gffn = A.take([128, D], F32)
    gcq = A.take([128, 4, 256], F32)
    gck = A.take([128, 4, 256], F32)
    P.dma("sp", lambda: nc.sync.dma_start(out=gcross, in_=norm_cross.partition_broadcast(128)), writes=["gcross"], slot="t0")
    P.dma("sp", lambda: nc.sync.dma_start(out=gffn, in_=norm_ffn.partition_broadcast(128)), writes=["gffn"], slot="t1")
    for hq in range(4):
        P.dma("sp", lambda hq=hq: nc.sync.dma_start(out=gcq[:, hq, :], in_=cross_q_norm.partition_broadcast(128)), writes=["gcq"], slot="t2")
        P.dma("sp", lambda hq=hq: nc.sync.dma_start(out=gck[:, hq, :], in_=cross_k_norm.partition_broadcast(128)), writes=["gck"], slot="t3")
    P.op("dve", lambda: nc.vector.tensor_scalar(out=gcq, in0=gcq, scalar1=1.0 / 16, scalar2=None, op0=ALU.mult), reads=["gcq"], writes=["gcq"])
    wr = A.take([128, KC, 36], BF16)
    P.dma("pool", lambda: nc.gpsimd.dma_start(out=wr[:, :, 0:4], in_=w_group.rearrange("(c p) n -> p c n", p=128)), writes=["wr"], slot="t4")
    P.dma("pool", lambda: nc.gpsimd.dma_start(out=wr[:, :, 4:36], in_=w_expert.rearrange("(c p) n -> p c n", p=128)), writes=["wr"], slot="t5")
    rbias = A.take([128, 36], F32)
    P.dma("sp", lambda: nc.sync.dma_start(out=rbias[:, 0:4], in_=b_group.partition_broadcast(128)), writes=["rbias"], slot="t6")
    P.dma("sp", lambda: nc.sync.dma_start(out=rbias[:, 4:36], in_=b_expert.partition_broadcast(128)), writes=["rbias"], slot="t7")
    _bc = {}

    def get_bc():
        if "r" not in _bc:
            _bc["r"] = nc.gpsimd.to_reg(NSLOT - 1)
        return _bc["r"]

    destAll = P.sb("destAll", [128, NT, 2], I32)
    gateAll = P.sb("gateAll", [128, NT, 2], F32)
    base = A.take([128, 32], F32)
    P.op("dve", lambda: nc.vector.memset(base, 0.0), writes=["base"])

    KcT = A2.take([128, KC, 256], BF16)
    Vc = A2.take([128, 2, D], BF16)
    a2_mark = A2.off
    gmem = A2.take([128, D], F32)
    P.dma("sp", lambda: nc.sync.dma_start(out=gmem, in_=norm_mem.partition_broadcast(128)), writes=["gmem"], slot="t8")
    wkv = A2.take([128, KC, D], BF16)
    MT = A2.take([128, KC, 256], BF16)
    xtm = [A2.take([128, D], F32) for i in range(2)]
    scr0 = (A2.take([128, D], BF16), A2.take([128, 4], F32), A2.take([128, 4], F32), A2.take([128, D], F32), "m")
    nb16 = A2.take([128, D], BF16)

    def transpose8(src_bf, src_key, dst3, dst_key, copy_eng):
        for c in range(KC):
            P.op("pe", lambda c=c, src_bf=src_bf: nc.tensor.transpose(out=bank_bf(4)[:, c * 128:(c + 1) * 128],
                                                                      in_=src_bf[:, c * 128:(c + 1) * 128], identity=ident),
                 reads=[src_key, "cbf"], writes=[("bank", 4)])
        if copy_eng == "act":
            P.op("act", lambda dst3=dst3: nc.scalar.copy(out=dst3, in_=bank_bf(4).rearrange("p (c n) -> p c n", c=KC)),
                 reads=[("bank", 4)], writes=[dst_key])
        else:
            P.op("dve", lambda dst3=dst3: nc.vector.tensor_copy(out=dst3, in_=bank_bf(4).rearrange("p (c n) -> p c n", c=KC)),
                 reads=[("bank", 4)], writes=[dst_key])

    def rms_rows(srcap, src_keys, ngrp, gain3, gain_key, dst_bf, dst_key, scr):
        sj, stss, strs, snf, tag = scr
        kj, kt_, kr, kn = ("junk3", tag), ("tss", tag), ("trs", tag), ("nf32", tag)
        w = D // ngrp
        for gi in range(ngrp):
            P.op("act", lambda gi=gi, sj=sj, stss=stss, srcap=srcap, w=w: nc.scalar.activation(
                out=sj[:, 0:w], in_=srcap[:, gi * w:(gi + 1) * w], func=AF.Square, accum_out=stss[:, gi:gi + 1]),
                reads=list(src_keys), writes=[kj, kt_])
        P.op("act", lambda stss=stss, strs=strs, ngrp=ngrp, w=w: nc.scalar.activation(out=strs[:, 0:ngrp], in_=stss[:, 0:ngrp], func=AF.Ln,
                                                                                      bias=EPS, scale=1.0 / w),
             reads=[kt_], writes=[kr])
        P.op("act", lambda strs=strs, ngrp=ngrp: nc.scalar.activation(out=strs[:, 0:ngrp], in_=strs[:, 0:ngrp], func=AF.Exp, scale=-0.5),
             reads=[kr], writes=[kr])
        P.op("dve", lambda snf=snf, srcap=srcap, strs=strs, ngrp=ngrp, w=w: nc.vector.tensor_tensor(
            out=snf.rearrange("p (a b) -> p a b", a=ngrp), in0=srcap.rearrange("p (a b) -> p a b", a=ngrp),
            in1=strs[:, 0:ngrp].unsqueeze(2).to_broadcast([128, ngrp, w]), op=ALU.mult),
            reads=list(src_keys) + [kr], writes=[kn])
        P.op("dve", lambda snf=snf, dst_bf=dst_bf, gain3=gain3: nc.vector.tensor_tensor(out=dst_bf, in0=snf, in1=gain3, op=ALU.mult),
             reads=[kn, gain_key], writes=[dst_key])

    for mt in range(2):
        P.dma("sp", lambda mt=mt: nc.sync.dma_start(out=xtm[mt], in_=mem[mt * 128:(mt + 1) * 128, :]), writes=[("xtm", mt)], slot=("xtm", mt))
        rms_rows(xtm[mt], [("xtm", mt)], 1, gmem, "gmem", nb16, "nb16m", scr0)
        transpose8(nb16, "nb16m", MT[:, :, mt * 128:(mt + 1) * 128], ("MT", mt), "act")
    for part in range(2):
        for hf_ in range(2):
            P.dma("pool", lambda part=part, hf_=hf_: nc.gpsimd.dma_start(
                out=wkv[:, :, hf_ * 512:(hf_ + 1) * 512], in_=w_ckv_v[:, :, part * D + hf_ * 512:part * D + (hf_ + 1) * 512]),
                writes=["wkv"], slot=("wkv", hf_))
        for mt in range(2):
            for hf_ in range(2):
                for c in range(KC):
                    P.op("pe", lambda c=c, mt=mt, hf_=hf_: nc.tensor.matmul(banks[hf_], MT[:, c, mt * 128:(mt + 1) * 128],
                                                                             wkv[:, c, hf_ * 512:(hf_ + 1) * 512],
                                                                             start=(c == 0), stop=(c == KC - 1)),
                         reads=[("MT", 0), ("MT", 1), "wkv"], writes=[("bank", hf_)])
            if part == 0:
                rms_rows(bigv[0], [("bank", 0), ("bank", 1)], 4, gck.rearrange("p a b -> p (a b)"), "gck", nb16, "nb16m", scr0)
                transpose8(nb16, "nb16m", KcT[:, :, mt * 128:(mt + 1) * 128], ("KcT", mt), "act")
            else:
                P.op("act", lambda mt=mt: nc.scalar.copy(out=Vc[:, mt, :], in_=bigv[0]), reads=[("bank", 0), ("bank", 1)], writes=[("Vc", mt)])

    P.fence()
    A2.off = a2_mark
    xtt = [A2.take([128, D], F32) for i in range(2)]
    scr1 = (A2.take([128, D], BF16), A2.take([128, 4], F32), A2.take([128, 4], F32), A2.take([128, D], F32), "t")
    h1 = A2.take([128, D], F32)
    u2 = A2.take([128, D], BF16)
    U2T = A2.take([128, KC, 128], BF16)
    qnb = A2.take([128, D], BF16)
    QcT = A2.take([128, KC, 128], BF16)
    PT = A2.take([128, 8, 128], BF16)
    ocb = A2.take([128, 4, 256], BF16)
    OcT = A2.take([128, KC, 128], BF16)
    h2 = [A2.take([128, D], F32) for i in range(2)]
    xfb = [A2.take([128, D], BF16) for i in range(2)]
    XfT = A2.take([128, KC, 128], BF16)
    csum = A2.take([128, 4], F32)
    L = A2.take([128, 36], F32)
    rt = A2.take([128, 64], F32)
    tmp48 = A2.take([128, 4, 8], F32)
    OH1 = A2.take([128, 4, 8], F32)
    OH2 = A2.take([128, 4, 8], F32)
    OHs = A2.take([128, 32], BF16)
    posE = A2.take([128, 32], F32)
    t32 = A2.take([128, 32], F32)
    dstf = A2.take([128, 2], F32)
    NT_RUN = int(os.environ.get("TAIL_NT", str(NT)))

    for t in range(NT_RUN):
        tk = slice(t * 128, (t + 1) * 128)
        xs = t % 2
        P.dma("sp", lambda t=t, xs=xs: nc.sync.dma_start(out=xtt[xs], in_=x[t * 128:(t + 1) * 128, :]), writes=[("xtt", xs)], slot=("xtt", xs))
        for hf_ in range(2):
            for c in range(KC):
                P.op("pe", lambda c=c, hf_=hf_, tk=tk: nc.tensor.matmul(banks[hf_], mixT[:, c, tk], wo[:, c, hf_ * 512:(hf_ + 1) * 512],
                                                                         start=(c == 0), stop=(c == KC - 1)),
                     reads=["wo", ("mixT", c, t // 2) if c < 4 else ("mixTg", t)], writes=[("bank", hf_)])
        P.op("dve", lambda xs=xs: nc.vector.tensor_tensor(out=h1, in0=bigv[0], in1=xtt[xs], op=ALU.add),
             reads=[("bank", 0), ("bank", 1), ("xtt", xs)], writes=["h1"])
        rms_rows(h1, ["h1"], 1, gcross, "gcross", u2, "u2", scr1)
        transpose8(u2, "u2", U2T, "U2T", "act")
        for hf_ in range(2):
            for c in range(KC):
                P.op("pe", lambda c=c, hf_=hf_: nc.tensor.matmul(banks[2 + hf_], U2T[:, c, :], wcq[:, c, hf_ * 512:(hf_ + 1) * 512],
                                                                  start=(c == 0), stop=(c == KC - 1)),
                     reads=["wcq", "U2T"], writes=[("bank", 2 + hf_)])
        rms_rows(bigv[1], [("bank", 2), ("bank", 3)], 4, gcq.rearrange("p a b -> p (a b)"), "gcq", qnb, "qnb", scr1)
        transpose8(qnb, "qnb", QcT, "QcT", "dve")
        for hq in range(4):
            for mt in range(2):
                col = (hq * 2 + mt) * 128
                bk = 6 + col // 512
                for dc in range(2):
                    P.op("pe", lambda hq=hq, mt=mt, dc=dc, col=col, bk=bk: nc.tensor.matmul(
                        banks[bk][:, col % 512:col % 512 + 128], KcT[:, 2 * hq + dc, mt * 128:(mt + 1) * 128], QcT[:, 2 * hq + dc, :],
                        start=(dc == 0), stop=(dc == 1), skip_group_check=True),
                        reads=[("KcT", 0), ("KcT", 1), "QcT"], writes=[("bank", bk)])
        P.op("act", lambda: nc.scalar.activation(out=PT.rearrange("p a b -> p (a b)"), in_=bigv[3], func=AF.Exp),
             reads=[("bank", 6), ("bank", 7)], writes=["PT"])
        for hq in range(4):
            for mt in range(2):
                P.op("pe", lambda hq=hq, mt=mt: nc.tensor.matmul(banks[2 + hq // 2][:, (hq % 2) * 256:(hq % 2) * 256 + 256], PT[:, hq * 2 + mt, :],
                                                                  Vc[:, mt, hq * 256:(hq + 1) * 256], start=(mt == 0), stop=(mt == 1),
                                                                  skip_group_check=True),
                     reads=["PT", ("Vc", 0), ("Vc", 1)], writes=[("bank", 2 + hq // 2)])
            for mt in range(2):
                P.op("pe", lambda hq=hq, mt=mt: nc.tensor.matmul(banks[5][:, hq:hq + 1], PT[:, hq * 2 + mt, :], ones_bf[:, 0:1],
                                                                  start=(mt == 0), stop=(mt == 1), skip_group_check=True),
                     reads=["PT", "cbf"], writes=[("bank", 5)])
        P.op("dve", lambda: nc.vector.reciprocal(out=csum, in_=banks[5][:, 0:4]), reads=[("bank", 5)], writes=["csum"])
        P.op("dve", lambda: nc.vector.tensor_tensor(out=ocb, in0=bigv[1].rearrange("p (a b) -> p a b", a=4),
                                                    in1=csum.unsqueeze(2).to_broadcast([128, 4, 256]), op=ALU.mult),
             reads=[("bank", 2), ("bank", 3), "csum"], writes=["ocb"])
        transpose8(ocb.rearrange("p a b -> p (a b)"), "ocb", OcT, "OcT", "act")
        for hf_ in range(2):
            for c in range(KC):
                P.op("pe", lambda c=c, hf_=hf_: nc.tensor.matmul(banks[hf_], OcT[:, c, :], wco[:, c, hf_ * 512:(hf_ + 1) * 512],
                                                                  start=(c == 0), stop=(c == KC - 1)),
                     reads=["wco", "OcT"], writes=[("bank", hf_)])
        P.op("dve", lambda xs=xs: nc.vector.tensor_tensor(out=h2[xs], in0=bigv[0], in1=h1, op=ALU.add),
             reads=[("bank", 0), ("bank", 1), "h1"], writes=[("h2", xs)])
        P.dma("sp", lambda t=t, xs=xs: nc.sync.dma_start(out=out[t * 128:(t + 1) * 128, :], in_=h2[xs]),
              reads=[("h2", xs)], writes=[("h2d", t)], slot=("h2d", xs))
        if stage == "tail":
            continue
        rms_rows(h2[xs], [("h2", xs)], 1, gffn, "gffn", xfb[xs], ("xfb", xs), scr1)
        transpose8(xfb[xs], ("xfb", xs), XfT, "XfT", "dve")
        for c in range(KC):
            P.op("pe", lambda c=c: nc.tensor.matmul(banks[5][:, 8:44], XfT[:, c, :], wr[:, c, :], start=(c == 0), stop=(c == KC - 1),
                                                     skip_group_check=True),
                 reads=["XfT", "wr"], writes=[("bank", 5)])
        P.op("dve", lambda: nc.vector.tensor_tensor(out=L, in0=banks[5][:, 8:44], in1=rbias, op=ALU.add),
             reads=[("bank", 5), "rbias"], writes=["L"])
        gl = L[:, 0:4]
        el = L[:, 4:36].rearrange("p (a b) -> p a b", a=4)
        R = "rt"
        P.op("dve", lambda: nc.vector.reduce_max(out=rt[:, 0:1], in_=gl, axis=AX.X), reads=["L"], writes=[R])
        P.op("dve", lambda: nc.vector.tensor_scalar(out=rt[:, 1:2], in0=rt[:, 0:1], scalar1=-1.0, scalar2=None, op0=ALU.mult), reads=[R], writes=[R])
        P.op("dve", lambda: nc.vector.tensor_scalar(out=rt[:, 16:20], in0=gl, scalar1=rt[:, 0:1], scalar2=None, op0=ALU.is_ge), reads=["L", R], writes=[R])
        P.op("act", lambda: nc.scalar.activation(out=rt[:, 20:24], in_=gl, func=AF.Exp, bias=rt[:, 1:2], accum_out=rt[:, 2:3]), reads=["L", R], writes=[R])
        P.op("dve", lambda: nc.vector.reciprocal(out=rt[:, 3:4], in_=rt[:, 2:3]), reads=[R], writes=[R])
        P.op("dve", lambda: nc.vector.tensor_tensor(out=tmp48, in0=el, in1=rt[:, 16:20].unsqueeze(2).to_broadcast([128, 4, 8]), op=ALU.mult),
             reads=["L", R], writes=["tmp48"])
        P.op("dve", lambda: nc.vector.reduce_sum(out=rt[:, 24:32], in_=tmp48.rearrange("p a b -> p b a"), axis=AX.X), reads=["tmp48"], writes=[R])
        P.op("dve", lambda: nc.vector.reduce_max(out=rt[:, 4:5], in_=rt[:, 24:32], axis=AX.X), reads=[R], writes=[R])
        P.op("dve", lambda: nc.vector.tensor_scalar(out=rt[:, 32:40], in0=rt[:, 24:32], scalar1=rt[:, 4:5], scalar2=None, op0=ALU.is_ge), reads=[R], writes=[R])
        P.op("dve", lambda: nc.vector.scalar_tensor_tensor(out=rt[:, 40:48], in0=rt[:, 32:40], scalar=-1.0e9, in1=rt[:, 24:32],
                                                            op0=ALU.mult, op1=ALU.add), reads=[R], writes=[R])
        P.op("dve", lambda: nc.vector.reduce_max(out=rt[:, 5:6], in_=rt[:, 40:48], axis=AX.X), reads=[R], writes=[R])
        P.op("dve", lambda: nc.vector.tensor_scalar(out=rt[:, 48:56], in0=rt[:, 40:48], scalar1=rt[:, 5:6], scalar2=None, op0=ALU.is_ge), reads=[R], writes=[R])
        P.op("dve", lambda: nc.vector.tensor_tensor(out=rt[:, 6:7], in0=rt[:, 5:6], in1=rt[:, 4:5], op=ALU.subtract), reads=[R], writes=[R])
        P.op("act", lambda: nc.scalar.activation(out=rt[:, 7:8], in_=rt[:, 6:7], func=AF.Exp), reads=[R], writes=[R])
        P.op("dve", lambda: nc.vector.tensor_scalar(out=rt[:, 7:8], in0=rt[:, 7:8], scalar1=1.0, scalar2=None, op0=ALU.add), reads=[R], writes=[R])
        P.op("dve", lambda: nc.vector.reciprocal(out=rt[:, 7:8], in_=rt[:, 7:8]), reads=[R], writes=[R])
        P.op("dve", lambda: nc.vector.tensor_tensor(out=rt[:, 8:9], in0=rt[:, 7:8], in1=rt[:, 3:4], op=ALU.mult), reads=[R], writes=[R])
        P.op("dve", lambda: nc.vector.tensor_tensor(out=rt[:, 9:10], in0=rt[:, 3:4], in1=rt[:, 8:9], op=ALU.subtract), reads=[R], writes=[R])
        P.op("dve", lambda t=t: nc.vector.tensor_copy(out=gateAll[:, t, :], in_=rt[:, 8:10]), reads=[R], writes=[("gate", t)])
        for ohx, c0_, nm2 in ((OH1, 32, "OH1"), (OH2, 48, "OH2")):
            P.op("dve", lambda ohx=ohx, c0_=c0_: nc.vector.tensor_tensor(
                out=ohx, in0=rt[:, 16:20].unsqueeze(2).to_broadcast([128, 4, 8]),
                in1=rt[:, c0_:c0_ + 8].unsqueeze(1).to_broadcast([128, 4, 8]), op=ALU.mult), reads=[R], writes=[nm2])
        P.op("dve", lambda: nc.vector.tensor_tensor(out=OHs, in0=OH1.rearrange("p a b -> p (a b)"), in1=OH2.rearrange("p a b -> p (a b)"), op=ALU.add),
             reads=["OH1", "OH2"], writes=["OHs"])
        P.op("pe", lambda: nc.tensor.matmul(banks[5][:, 64:96], TriS, OHs, start=True, stop=True, skip_group_check=True),
             reads=["OHs", "cbf"], writes=[("bank", 5)])
        P.op("pe", lambda: nc.tensor.matmul(banks[5][:, 96:128], ones_bf, OHs, start=True, stop=True, skip_group_check=True),
             reads=["OHs", "cbf"], writes=[("bank", 5)])
        P.op("dve", lambda: nc.vector.tensor_tensor(out=posE, in0=banks[5][:, 64:96], in1=base, op=ALU.add),
             reads=[("bank", 5), "base"], writes=["posE"])
        P.op("dve", lambda: nc.vector.tensor_tensor(out=base, in0=banks[5][:, 96:128], in1=base, op=ALU.add),
             reads=[("bank", 5), "base"], writes=["base"])
        for kx, ohx, nm2 in ((0, OH1, "OH1"), (1, OH2, "OH2")):
            P.op("dve", lambda ohx=ohx: nc.vector.tensor_tensor(out=t32, in0=ohx.rearrange("p a b -> p (a b)"), in1=posE, op=ALU.mult),
                 reads=[nm2, "posE"], writes=["t32"])
            P.op("dve", lambda kx=kx: nc.vector.reduce_sum(out=rt[:, 10 + kx:11 + kx], in_=t32, axis=AX.X), reads=["t32"], writes=[R])
            P.op("dve", lambda ohx=ohx: nc.vector.tensor_tensor(out=t32, in0=ohx.rearrange("p a b -> p (a b)"), in1=iota32, op=ALU.mult),
                 reads=[nm2, "cf32"], writes=["t32"])
            P.op("dve", lambda kx=kx: nc.vector.reduce_sum(out=rt[:, 12 + kx:13 + kx], in_=t32, axis=AX.X), reads=["t32"], writes=[R])
            P.op("dve", lambda kx=kx: nc.vector.tensor_scalar(out=rt[:, 14 + kx:15 + kx], in0=rt[:, 10 + kx:11 + kx], scalar1=float(CAP),
                                                              scalar2=1.0e6, op0=ALU.is_ge, op1=ALU.mult), reads=[R], writes=[R])
            P.op("dve", lambda kx=kx: nc.vector.scalar_tensor_tensor(out=dstf[:, kx:kx + 1], in0=rt[:, 12 + kx:13 + kx], scalar=float(CAP),
                                                                      in1=rt[:, 10 + kx:11 + kx], op0=ALU.mult, op1=ALU.add),
                 reads=[R], writes=["dstf"])
            P.op("dve", lambda kx=kx: nc.vector.tensor_tensor(out=dstf[:, kx:kx + 1], in0=dstf[:, kx:kx + 1], in1=rt[:, 14 + kx:15 + kx], op=ALU.add),
                 reads=[R, "dstf"], writes=["dstf"])
        P.op("dve", lambda t=t: nc.vector.tensor_copy(out=destAll[:, t, :], in_=dstf), reads=["dstf"], writes=[("dest", t)])
        for kx in range(2):
            P.dma("pool", lambda t=t, kx=kx, xs=xs: nc.gpsimd.indirect_dma_start(
                out=x_pad[:, :], out_offset=bass.IndirectOffsetOnAxis(ap=destAll[:, t, kx:kx + 1], axis=0),
                in_=xfb[xs], in_offset=None, bounds_check=get_bc(), oob_is_err=False),
                reads=[("dest", t), ("xfb", xs)], writes=["x_pad"], slot="scat")

    if stage == "tail":
        P.emit()
        return nc, P


    P.fence()
    A.reset()
    A3 = Arena(mixT[:].rearrange("p c t -> p (c t)"), KC * T)
    wv_g = w_e_gate.rearrange("e (c p) n -> e p c n", p=128)
    wv_u = w_e_up.rearrange("e (c p) n -> e p c n", p=128)
    wv_d = w_e_down.rearrange("e (c p) n -> e p c n", p=128)
    xp_v = x_pad.rearrange("(e j p) d -> e p j d", j=CAP // 128, p=128)
    NB = CAP // 128
    wgs = [A3.take([128, KC, 512], BF16) for i in range(2)]
    wus = [A3.take([128, KC, 512], BF16) for i in range(2)]
    wds = [A3.take([128, 4, D], BF16) for i in range(2)]
    xblk = [A.take([128, NB, D], BF16) for i in range(2)]
    XeT = [A.take([128, KC, CAP], BF16) for i in range(2)]
    hidT = A.take([128, 4, CAP], BF16)
    sil = [A.take([128, CAP], F32) for i in range(2)]
    oblk = [A.take([128, D], F32) for i in range(2)]
    wdstage = A.take([128, 4, D], F32)
    NE = int(os.environ.get("MOE_NE", "32"))
    for e in range(NE):
        wsl = e % 2
        for hf_ in range(2):
            P.dma("pool", lambda e=e, wsl=wsl, hf_=hf_: nc.gpsimd.dma_start(out=wgs[wsl][:, hf_ * 4:(hf_ + 1) * 4, :],
                                                                            in_=wv_g[e, :, hf_ * 4:(hf_ + 1) * 4, :]),
                  writes=[("wgs", wsl)], slot=("wgs", wsl, hf_))
            P.dma("pool", lambda e=e, wsl=wsl, hf_=hf_: nc.gpsimd.dma_start(out=wus[wsl][:, hf_ * 4:(hf_ + 1) * 4, :],
                                                                            in_=wv_u[e, :, hf_ * 4:(hf_ + 1) * 4, :]),
                  writes=[("wus", wsl)], slot=("wus", wsl, hf_))
            P.dma("sp", lambda e=e, hf_=hf_: nc.sync.dma_start(out=wdstage[:, :, hf_ * 512:(hf_ + 1) * 512],
                                                               in_=wv_d[e, :, :, hf_ * 512:(hf_ + 1) * 512]),
                  writes=["wdstage"], slot=("wdstage", hf_))
        P.op("act", lambda wsl=wsl: nc.scalar.copy(out=wds[wsl], in_=wdstage), reads=["wdstage"], writes=[("wds", wsl)])
        P.dma("sp", lambda e=e, wsl=wsl: nc.sync.dma_start(out=xblk[wsl], in_=xp_v[e]), reads=["x_pad"], writes=[("xblk", wsl)],
              slot=("xblk", wsl))
        for j in range(NB):
            for c in range(KC):
                P.op("pe", lambda c=c, j=j, wsl=wsl: nc.tensor.transpose(out=bank_bf(4)[:, c * 128:(c + 1) * 128],
                                                                         in_=xblk[wsl][:, j, c * 128:(c + 1) * 128], identity=ident),
                     reads=[("xblk", wsl), "cbf"], writes=[("bank", 4)])
            if j % 2 == 0:
                P.op("act", lambda j=j, wsl=wsl: nc.scalar.copy(out=XeT[wsl][:, :, j * 128:(j + 1) * 128],
                                                                in_=bank_bf(4).rearrange("p (c n) -> p c n", c=KC)),
                     reads=[("bank", 4)], writes=[("XeT", wsl)])
            else:
                P.op("dve", lambda j=j, wsl=wsl: nc.vector.tensor_copy(out=XeT[wsl][:, :, j * 128:(j + 1) * 128],
                                                                       in_=bank_bf(4).rearrange("p (c n) -> p c n", c=KC)),
                     reads=[("bank", 4)], writes=[("XeT", wsl)])
        for hc in range(4):
            bg = 0 if hc % 2 == 0 else 2
            for wsrc, bk, wkey in ((wgs, bg, "wgs"), (wus, bg + 1, "wus")):
                for c in range(KC):
                    P.op("pe", lambda c=c, hc=hc, wsl=wsl, wsrc=wsrc, bk=bk: nc.tensor.matmul(
                        banks[bk], wsrc[wsl][:, c, hc * 128:(hc + 1) * 128], XeT[wsl][:, c, :], start=(c == 0), stop=(c == KC - 1)),
                        reads=[(wkey, wsl), ("XeT", wsl)], writes=[("bank", bk)])
            P.op("act", lambda hc=hc, bg=bg: nc.scalar.activation(out=sil[hc % 2], in_=banks[bg], func=AF.Silu),
                 reads=[("bank", bg)], writes=[("sil", hc % 2)])
            P.op("dve", lambda hc=hc, bg=bg: nc.vector.tensor_tensor(out=hidT[:, hc, :], in0=banks[bg + 1], in1=sil[hc % 2], op=ALU.mult),
                 reads=[("bank", bg + 1), ("sil", hc % 2)], writes=[("hidT", hc)])
        for j in range(NB):
            for half in range(2):
                for hc in range(4):
                    P.op("pe", lambda hc=hc, j=j, half=half, wsl=wsl: nc.tensor.matmul(
                        banks[6 + half], hidT[:, hc, j * 128:(j + 1) * 128], wds[wsl][:, hc, half * 512:(half + 1) * 512],
                        start=(hc == 0), stop=(hc == 3)),
                        reads=[("hidT", hc), ("wds", wsl)], writes=[("bank", 6 + half)])
            if j % 2 == 0:
                P.op("act", lambda j=j: nc.scalar.copy(out=oblk[j % 2], in_=bigv[3]), reads=[("bank", 6), ("bank", 7)], writes=[("oblk", j % 2)])
            else:
                P.op("dve", lambda j=j: nc.vector.tensor_copy(out=oblk[j % 2], in_=bigv[3]), reads=[("bank", 6), ("bank", 7)], writes=[("oblk", j % 2)])
            r0 = e * CAP + j * 128
            P.dma("sp", lambda j=j, r0=r0: nc.sync.dma_start(out=o_pad[r0:r0 + 128, :], in_=oblk[j % 2]),
                  reads=[("oblk", j % 2)], writes=["o_pad"], slot="opad")

    P.fence()
    A2.off = 0
    g1s = [A2.take([128, D], F32) for i in range(2)]
    g2s = [A2.take([128, D], F32) for i in range(2)]
    hhs = [A2.take([128, D], F32) for i in range(2)]
    fins = [A2.take([128, D], F32) for i in range(2)]
    for t in range(NT_RUN):
        s2 = t % 2
        for kx, gs, gname in ((0, g1s, "g1s"), (1, g2s, "g2s")):
            P.op("dve", lambda gs=gs, s2=s2: nc.vector.memset(gs[s2], 0.0), writes=[(gname, s2)])
            P.dma("pool", lambda gs=gs, s2=s2, t=t, kx=kx: nc.gpsimd.indirect_dma_start(
                out=gs[s2], out_offset=None, in_=o_pad[:, :], in_offset=bass.IndirectOffsetOnAxis(ap=destAll[:, t, kx:kx + 1], axis=0),
                bounds_check=get_bc(), oob_is_err=False),
                reads=["o_pad", ("dest", t)], writes=[(gname, s2)], slot=(gname, s2))
        P.dma("sp", lambda t=t, s2=s2: nc.sync.dma_start(out=hhs[s2], in_=out[t * 128:(t + 1) * 128, :]),
              reads=[("h2d", t)], writes=[("hhs", s2)], slot=("hhs", s2))
        P.op("dve", lambda t=t, s2=s2: nc.vector.scalar_tensor_tensor(out=fins[s2], in0=g1s[s2], scalar=gateAll[:, t, 0:1], in1=hhs[s2],
                                                                       op0=ALU.mult, op1=ALU.add),
             reads=[("g1s", s2), ("hhs", s2), ("gate", t)], writes=[("fins", s2)])
        P.op("dve", lambda t=t, s2=s2: nc.vector.scalar_tensor_tensor(out=fins[s2], in0=g2s[s2], scalar=gateAll[:, t, 1:2], in1=fins[s2],
                                                                       op0=ALU.mult, op1=ALU.add),
             reads=[("g2s", s2), ("fins", s2), ("gate", t)], writes=[("fins", s2)])
        P.dma("sp", lambda t=t, s2=s2: nc.sync.dma_start(out=out[t * 128:(t + 1) * 128, :], in_=fins[s2]),
              reads=[("fins", s2), ("hhs", s2)], writes=[("h2d", t)], slot=("fin", s2))
    P.emit()
    return nc, P


_CACHE = {}


def kernel(**inputs):
    nb = inputs["x"].shape[0]
    if "nc" not in _CACHE:
        _CACHE["nc"] = build("full")[0]
    nc = _CACHE["nc"]
    consts = make_consts()
    shared = {}
    for k, v in inputs.items():
        if k in ("x", "mem"):
            continue
        v = np.ascontiguousarray(np.asarray(v, dtype=np.float32))
        shared[k] = v[0] if v.ndim >= 3 else v
    in_maps = []
    for b in range(nb):
        m = dict(shared)
        m["x"] = np.ascontiguousarray(np.asarray(inputs["x"][b], dtype=np.float32))
        m["mem"] = np.ascontiguousarray(np.asarray(inputs["mem"][b], dtype=np.float32))
        m["consts"] = consts
        in_maps.append(m)
    res = run_bass_kernel_spmd(nc, in_maps, core_ids=list(range(nb)))
    return np.stack([np.asarray(r["out"], dtype=np.float32) for r in res.results], axis=0)


def make_consts():
    c = np.zeros((128, NCONST), np.float32)
    c[:, 0:128] = np.eye(128, dtype=np.float32)
    bo = np.zeros((128, 128), np.float32)
    bo[0:64, 0:64] = 1.0 / 64
    bo[64:128, 64:128] = 1.0 / 64
    c[:, 128:256] = bo
    j = np.arange(128)[:, None]
    i = np.arange(128)[None, :]
    same = (j // 64 == i // 64).astype(np.float32)
    mid = (i // 64) * 64 + 31
    c[:, 2304:2432] = -(1.0 / 16) * same * (j <= i)
    c[:, 2432:2560] = -(1.0 / 16) * same * ((j <= i).astype(np.float32) - (j <= mid).astype(np.float32))
    c[:, 2560:2688] = -(1.0 / 16) * same * (j > i)
    c[:, 2688:2816] = same * (j <= i)
    c[:, 2816:2944] = same * (j > i)
    c[:, 2944:3072] = (j < i)
    c[:, 3072:3200] = 1.0
    c[:, 3200:3232] = np.arange(32, dtype=np.float32)[None, :]
    c[0:64, 3232] = 1.0
    c[64:128, 3233] = 1.0
    c[0:64, 3234] = 0.125
    c[64:128, 3235] = 0.125
    return c
```

```python
import contextlib
import numpy as np
import concourse.bass as bass
import concourse.mybir as mybir
from concourse.bass_utils import run_bass_kernel_spmd

F32 = mybir.dt.float32
BF16 = mybir.dt.bfloat16
I32 = mybir.dt.int32
AF = mybir.ActivationFunctionType
ALU = mybir.AluOpType
AX = mybir.AxisListType

T = 4096
NT = 32
D = 1024
KC = 8
EPS = 1e-6
IN_W = 3088
LAM_INIT = 0.2
SEM_CHUNK = 16000
SAME_ENGINE_SYNC = True


class Prog:
    def __init__(self, nc):
        self.nc = nc
        self.es = contextlib.ExitStack()
        self.insts = []
        self.engs = {"pe": nc.tensor, "act": nc.scalar, "dve": nc.vector, "pool": nc.gpsimd, "sp": nc.sync}
        self.slot_sems = {}
        self.slot_vals = {}

    def sb(self, name, shape, dtype):
        return self.es.enter_context(self.nc.sbuf_tensor(name, list(shape), dtype))

    def ps(self, name, shape, dtype):
        return self.es.enter_context(self.nc.psum_tensor(name, list(shape), dtype))

    LOOPVARS = {'t', 'tt', 'g', 'h', 'p', 'hh', 'c', 'cs', 'rows', 'kt', 'qc', 'm', 'qs', 's', 'r', 'q0', 'bk', 'ba', 'bb', 'bv', 'bo',
                'b1', 'b2', 'o1', 'o2', 'tok0', 'ws', 'xs', 's2', 'i', 'j', 'a', 'ch', 'bu', 'snap', 'par', 'Sba', 'Sbb', 'els', 'dcol',
                'oc', 'lc', 'lt', 'o4', 'dst', 'src', 'ee', 'sc', 'nm', 'col0', 'c0', 'last', 'ps_', 'hf', 'e', 'blk', 'half', 'hc',
                'k', 'kc', 'mt', 'dc', 'eb', 'wsl', 'xb', 'hb'}

    def _chk(self, fn):
        bad = set(fn.__code__.co_freevars) & self.LOOPVARS
        assert not bad, ("late-bound loop variable in lambda", bad, fn.__code__.co_firstlineno)

    def op(self, eng, fn, reads=(), writes=()):
        self._chk(fn)
        self.insts.append(dict(kind="op", eng=eng, fn=fn, reads=tuple(reads), writes=tuple(writes)))

    def dma(self, eng, fn, reads=(), writes=(), slot=None, n=1):
        assert slot is not None
        self._chk(fn)
        self.insts.append(dict(kind="dma", eng=eng, fn=fn, reads=tuple(reads), writes=tuple(writes), slot=slot, n=n))

    def rename(self, old_keys, new_keys):
        self.insts.append(dict(kind="rename", old=tuple(old_keys), new=tuple(new_keys)))

    def fence(self):
        self.insts.append(dict(kind="fence"))

    def emit(self):
        nc = self.nc
        last_w = {}
        readers = {}
        eng_count = {e: 0 for e in self.engs}
        marked = {e: set() for e in self.engs}
        slot_val = {}
        fence_toks = []
        seen_since_fence = set()
        for ins in self.insts:
            if ins["kind"] == "fence":
                toks = list(fence_toks)
                for k, v in last_w.items():
                    if v is not None:
                        toks.append(v)
                for k, v in readers.items():
                    toks.extend(v)
                best = {}
                for tk in toks:
                    kk = (tk[0], tk[1])
                    if kk not in best or tk[2] > best[kk][2]:
                        best[kk] = tk
                fence_toks = list(best.values())
                seen_since_fence = set(last_w.keys()) | set(readers.keys())
                continue
            if ins["kind"] == "rename":
                toks = []
                for k in ins["old"]:
                    if last_w.get(k) is not None:
                        toks.append(last_w[k])
                    toks.extend(readers.get(k, []))
                    last_w.pop(k, None)
                    readers.pop(k, None)
                for k in ins["new"]:
                    readers.setdefault(k, []).extend(toks)
                continue
            e = ins["eng"]
            idx = eng_count[e]
            eng_count[e] += 1
            deps = set()
            for k in ins["reads"] + ins["writes"]:
                if k not in seen_since_fence:
                    seen_since_fence.add(k)
                    deps.update(fence_toks)
            for k in ins["reads"]:
                if last_w.get(k) is not None:
                    deps.add(last_w[k])
            for k in ins["writes"]:
                if last_w.get(k) is not None:
                    deps.add(last_w[k])
                deps.update(readers.get(k, []))
            if ins["kind"] == "dma":
                s = ins["slot"]
                slot_val[s] = slot_val.get(s, 0) + 16 * ins["n"]
                tok = ("d", s, slot_val[s])
            else:
                tok = ("e", e, idx)
            best = {}
            for tk in deps:
                kk = (tk[0], tk[1])
                if kk not in best or tk[2] > best[kk][2]:
                    best[kk] = tk
            deps = set(best.values())
            ins["idx"] = idx
            ins["tok"] = tok
            ins["deps"] = deps
            for dp in deps:
                if dp[0] == "e":
                    marked[dp[1]].add(dp[2])
            for k in ins["reads"]:
                readers.setdefault(k, []).append(tok)
            for k in ins["writes"]:
                last_w[k] = tok
                readers[k] = []
        for e in self.engs:
            if eng_count[e] > 0:
                marked[e].add(eng_count[e] - 1)
        self._last_idx = {e: eng_count[e] - 1 for e in self.engs if eng_count[e] > 0}
        rank = {}
        for e in self.engs:
            for r, idx in enumerate(sorted(marked[e])):
                rank[(e, idx)] = r
        nchunks = {e: (len(marked[e]) + SEM_CHUNK - 1) // SEM_CHUNK for e in self.engs}
        esems = {e: [self.es.enter_context(nc.semaphore(f"q_{e}_{i}")) for i in range(max(1, nchunks[e]))] for e in self.engs}
        dsems = {}
        for s in slot_val:
            dsems[s] = self.es.enter_context(nc.semaphore(f"d_{len(dsems)}"))
        waited_e = {e: {b: -1 for b in self.engs} for e in self.engs}
        waited_d = {e: {} for e in self.engs}
        nwaits = 0
        ins_kind_last = {}
        for ins in self.insts:
            if ins["kind"] in ("op", "dma"):
                ins_kind_last[ins["eng"]] = ins["kind"]
        trace = {e: [] for e in self.engs}
        for ins in self.insts:
            if ins["kind"] in ("rename", "fence"):
                continue
            e = ins["eng"]
            h = self.engs[e]
            tw = []
            trace[e].append((tw, ins))
            for dp in sorted(ins["deps"], key=str):
                if dp[0] == "e":
                    b, j = dp[1], dp[2]
                    if b == e and (e == "pe" or not SAME_ENGINE_SYNC):
                        continue
                    r = rank[(b, j)]
                    if waited_e[e][b] >= r:
                        continue
                    waited_e[e][b] = r
                    h.wait_ge(esems[b][r // SEM_CHUNK], (r % SEM_CHUNK) + 1)
                    tw.append(("e", b, r + 1))
                    nwaits += 1
                else:
                    s, v = dp[1], dp[2]
                    if waited_d[e].get(s, 0) >= v:
                        continue
                    waited_d[e][s] = v
                    h.wait_ge(dsems[s], v)
                    tw.append(("d", s, v))
                    nwaits += 1
            bi = ins["fn"]()
            if ins["kind"] == "dma":
                bi.then_inc(dsems[ins["slot"]], 16)
            elif (e, ins["idx"]) in rank:
                r = rank[(e, ins["idx"])]
                bi.then_inc(esems[e][r // SEM_CHUNK], 1)
        self.final_slots = {s: (dsems[s], v) for s, v in slot_val.items()}
        for e, li in self._last_idx.items():
            if e == "sp" or ins_kind_last.get(e) == "dma":
                continue
            r = rank[(e, li)]
            nc.sync.wait_ge(esems[e][r // SEM_CHUNK], (r % SEM_CHUNK) + 1)
        for s_, v in slot_val.items():
            nc.sync.wait_ge(dsems[s_], v)
        semv = {}
        pos = {e: 0 for e in self.engs}
        progress = True
        while progress:
            progress = False
            for e in self.engs:
                while pos[e] < len(trace[e]):
                    tw, ins = trace[e][pos[e]]
                    if all(semv.get((w[0], w[1]), 0) >= w[2] for w in tw):
                        if ins["kind"] == "dma":
                            semv[("d", ins["slot"])] = semv.get(("d", ins["slot"]), 0) + 16
                        elif (e, ins["idx"]) in rank:
                            semv[("e", e)] = semv.get(("e", e), 0) + 1
                        pos[e] += 1
                        progress = True
                    else:
                        break
        stuck = {e: (pos[e], len(trace[e])) for e in self.engs if pos[e] < len(trace[e])}
        if stuck:
            for e in stuck:
                tw, ins = trace[e][pos[e]]
                print("DEADLOCK", e, pos[e], tw, ins["reads"], ins["writes"], {k: v for k, v in semv.items()})
            raise RuntimeError("deadlock in generated program")
        self.stats = dict(n_inst=len(self.insts), n_waits=nwaits, marked={e: len(marked[e]) for e in self.engs})


class Arena:
    def __init__(self, buf, nelem):
        self.buf = buf
        self.n = nelem
        self.off = 0

    def reset(self):
        self.off = 0

    def take(self, shape, dtype):
        free = 1
        for d_ in shape[1:]:
            free *= d_
        ne = free * (2 if dtype == F32 else 1)
        ne = (ne + 15) // 16 * 16
        assert self.off + ne <= self.n, ("arena overflow", self.off, ne, self.n)
        ap = self.buf[0:shape[0], self.off:self.off + (free * (2 if dtype == F32 else 1))]
        self.off += ne
        if dtype == F32:
            ap = ap.bitcast(F32)
        if len(shape) == 3:
            ap = ap.rearrange("p (a b) -> p a b", a=shape[1])
        elif len(shape) == 4:
            ap = ap.rearrange("p (a b c) -> p a b c", a=shape[1], b=shape[2])
        return ap


NCONST = 3236
CAP = 512
NSLOT = 32 * CAP


def build(stage="full"):
    nc = bass.Bass("TRN2", target_bir_lowering=False)
    P = Prog(nc)

    def din(name, shape):
        return nc.dram_tensor(name, list(shape), F32, kind="ExternalInput").ap()

    x = din("x", [T, D])
    mem = din("mem", [256, D])
    norm_mix = din("norm_mix", [1, D])
    w_in = din("w_in", [D, IN_W])
    da_q_norm = din("da_q_norm", [1, 64])
    da_k_norm = din("da_k_norm", [1, 64])
    lq1 = din("lambda_q1", [1, 64])
    lk1 = din("lambda_k1", [1, 64])
    lq2 = din("lambda_q2", [1, 64])
    lk2 = din("lambda_k2", [1, 64])
    da_out_norm = din("da_out_norm", [1, 128])
    gla_gate_w = din("gla_gate_w", [16, 256])
    gla_gate_b = din("gla_gate_b", [1, 256])
    gla_out_norm = din("gla_out_norm", [1, 128])
    w_o = din("w_o", [D, D])
    norm_cross = din("norm_cross", [1, D])
    norm_mem = din("norm_mem", [1, D])
    w_cq = din("w_cq", [D, D])
    w_ckv = din("w_ckv", [D, 2 * D])
    cross_q_norm = din("cross_q_norm", [1, 256])
    cross_k_norm = din("cross_k_norm", [1, 256])
    w_co = din("w_co", [D, D])
    norm_ffn = din("norm_ffn", [1, D])
    w_group = din("w_group", [D, 4])
    b_group = din("b_group", [1, 4])
    w_expert = din("w_expert", [D, 32])
    b_expert = din("b_expert", [1, 32])
    w_e_gate = din("w_e_gate", [32, D, 512])
    w_e_up = din("w_e_up", [32, D, 512])
    w_e_down = din("w_e_down", [32, 512, D])
    consts = din("consts", [128, NCONST])
    out = nc.dram_tensor("out", [T, D], F32, kind="ExternalOutput").ap()
    x_pad = nc.dram_tensor("x_pad", [NSLOT, D], BF16, kind="Internal").ap()
    o_pad = nc.dram_tensor("o_pad", [NSLOT, D], F32, kind="Internal").ap()
    if stage in ("da", "gla"):
        dbg = nc.dram_tensor("dbg", [128, 4, T], BF16, kind="ExternalOutput").ap()

    UT = P.sb("UT", [128, KC, T], BF16)
    mixT = P.sb("mixT", [128, KC, T], BF16)
    ARENA_N = 36 * 1024
    arena_t = P.sb("arena", [128, ARENA_N], BF16)
    A = Arena(arena_t, ARENA_N)
    cbf = P.sb("cbf", [128, 256 + 5 * 128], BF16)
    cf32 = P.sb("cf32", [128, 292], F32)
    TriS = cbf[:, 640:768]
    ones_bf = cbf[:, 768:896]
    iota32 = cf32[:, 256:288]
    ident = cbf[:, 0:128]
    bones = cbf[:, 128:256]
    Trin = cbf[:, 256:384]
    TriCn = cbf[:, 384:512]
    TriEn = cbf[:, 512:640]
    maskP = cf32[:, 0:128]
    maskF = cf32[:, 128:256]
    P.dma("pool", lambda: nc.gpsimd.dma_start(out=cbf[:, 0:256], in_=consts[:, 0:256]), writes=["cbf"], slot="c0")
    P.dma("pool", lambda: nc.gpsimd.dma_start(out=cbf[:, 256:640], in_=consts[:, 2304:2304 + 384]), writes=["cbf"], slot="c1")
    P.dma("pool", lambda: nc.gpsimd.dma_start(out=cbf[:, 640:896], in_=consts[:, 2944:3200]), writes=["cbf"], slot="c1b")
    P.dma("sp", lambda: nc.sync.dma_start(out=cf32[:, 0:256], in_=consts[:, 2688:2688 + 256]), writes=["cf32"], slot="c2")
    P.dma("sp", lambda: nc.sync.dma_start(out=cf32[:, 256:292], in_=consts[:, 3200:3236]), writes=["cf32"], slot="c2b")

    gon = P.sb("gon", [128, 128], F32)
    P.dma("sp", lambda: nc.sync.dma_start(out=gon[:], in_=da_out_norm.partition_broadcast(128)), writes=["gon"], slot="c4")
    P.op("dve", lambda: nc.vector.tensor_scalar(out=gon[:], in0=gon[:], scalar1=1.0 - LAM_INIT, scalar2=None, op0=ALU.mult),
         reads=["gon"], writes=["gon"])
    ggl = P.sb("ggl", [128, 128], F32)
    P.dma("sp", lambda: nc.sync.dma_start(out=ggl[:], in_=gla_out_norm.partition_broadcast(128)), writes=["ggl"], slot="c4b")
    gq = P.sb("gq", [128, 1], F32)
    gk = P.sb("gk", [128, 1], F32)
    for hh in range(2):
        P.dma("sp", lambda hh=hh: nc.sync.dma_start(out=gq[hh * 64:(hh + 1) * 64, :], in_=da_q_norm.rearrange("o d -> d o")),
              writes=["gq"], slot="c5")
        P.dma("sp", lambda hh=hh: nc.sync.dma_start(out=gk[hh * 64:(hh + 1) * 64, :], in_=da_k_norm.rearrange("o d -> d o")),
              writes=["gk"], slot="c6")
    P.op("dve", lambda: nc.vector.tensor_scalar(out=gq[:], in0=gq[:], scalar1=0.125, scalar2=None, op0=ALU.mult),
         reads=["gq"], writes=["gq"])
    lam4 = P.sb("lam4", [128, 4, 64], F32)
    for i, a in enumerate((lq1, lk1, lq2, lk2)):
        P.dma("sp", lambda i=i, a=a: nc.sync.dma_start(out=lam4[:, i, :], in_=a.partition_broadcast(128)),
              writes=["lam4"], slot="c7")
    lamw = P.sb("lamw", [128, 2, 64], F32)
    lams = P.sb("lams", [128, 2], F32)
    nlam = P.sb("nlam", [128, 1], F32)
    P.op("dve", lambda: nc.vector.tensor_tensor(out=lamw[:], in0=lam4[:, 0:4:2, :], in1=lam4[:, 1:4:2, :], op=ALU.mult),
         reads=["lam4"], writes=["lamw"])
    P.op("dve", lambda: nc.vector.reduce_sum(out=lams[:], in_=lamw[:], axis=AX.X), reads=["lamw"], writes=["lams"])
    P.op("act", lambda: nc.scalar.activation(out=lams[:], in_=lams[:], func=AF.Exp), reads=["lams"], writes=["lams"])
    P.op("dve", lambda: nc.vector.scalar_tensor_tensor(out=nlam[:], in0=lams[:, 1:2], scalar=-LAM_INIT, in1=lams[:, 0:1],
                                                        op0=ALU.add, op1=ALU.subtract),
         reads=["lams"], writes=["nlam"])

    bigs = [P.ps(f"big{j}", [128, 1024], F32) for j in range(4)]
    bigv = [b[:] for b in bigs]
    banks = []
    for j in range(4):
        banks.append(bigs[j][:, 0:512])
        banks.append(bigs[j][:, 512:1024])

    def bank_bf(i):
        return banks[i].bitcast(BF16)

    gmix = A.take([128, D], F32)
    P.dma("sp", lambda: nc.sync.dma_start(out=gmix, in_=norm_mix.partition_broadcast(128)), writes=["gmix"], slot="c3")
    xts = [A.take([128, D], F32) for i in range(3)]
    ubs = [A.take([128, D], BF16) for i in range(2)]
    junk = A.take([128, D], BF16)
    ssq = [A.take([128, 1], F32) for i in range(2)]
    rstd = [A.take([128, 1], F32) for i in range(2)]

    for t in range(NT):
        xs = t % 3
        s2 = t % 2
        P.dma("sp", lambda t=t, xs=xs: nc.sync.dma_start(out=xts[xs], in_=x[t * 128:(t + 1) * 128, :]),
              writes=[("xt", xs)], slot=("xt", xs))
        P.op("act", lambda xs=xs, s2=s2: nc.scalar.activation(out=junk, in_=xts[xs], func=AF.Square, accum_out=ssq[s2]),
             reads=[("xt", xs)], writes=["junk", ("ssq", s2)])
        P.op("act", lambda s2=s2: nc.scalar.activation(out=rstd[s2], in_=ssq[s2], func=AF.Ln, bias=EPS, scale=1.0 / D),
             reads=[("ssq", s2)], writes=[("rstd", s2)])
        P.op("act", lambda s2=s2: nc.scalar.activation(out=rstd[s2], in_=rstd[s2], func=AF.Exp, scale=-0.5),
             reads=[("rstd", s2)], writes=[("rstd", s2)])
        P.op("dve", lambda xs=xs, s2=s2: nc.vector.scalar_tensor_tensor(out=ubs[s2], in0=xts[xs], scalar=rstd[s2][:, 0:1],
                                                                         in1=gmix, op0=ALU.mult, op1=ALU.mult),
             reads=[("xt", xs), ("rstd", s2), "gmix"], writes=[("ub", s2)])
        bk = t % 2
        for c in range(KC):
            P.op("pe", lambda c=c, s2=s2, bk=bk: nc.tensor.transpose(out=bank_bf(bk)[:, c * 128:(c + 1) * 128],
                                                                      in_=ubs[s2][:, c * 128:(c + 1) * 128], identity=ident),
                 reads=[("ub", s2), "cbf"], writes=[("bank", bk)])
        if t % 2 == 0:
            P.op("act", lambda t=t, bk=bk: nc.scalar.copy(out=UT[:, :, t * 128:(t + 1) * 128],
                                                          in_=bank_bf(bk).rearrange("p (c n) -> p c n", c=KC)),
                 reads=[("bank", bk)], writes=[("UT", t)])
        else:
            P.op("dve", lambda t=t, bk=bk: nc.vector.tensor_copy(out=UT[:, :, t * 128:(t + 1) * 128],
                                                                  in_=bank_bf(bk).rearrange("p (c n) -> p c n", c=KC)),
                 reads=[("bank", bk)], writes=[("UT", t)])

    w_in_v = w_in.rearrange("(c p) n -> p c n", p=128)

    P.fence()
    A.reset()
    wda = [A.take([128, KC, 384], BF16) for i in range(2)]
    QT = A.take([128, T], BF16)
    KT = A.take([128, T], BF16)
    V = A.take([128, NT, 132], BF16)
    sqb = [A.take([128, 512], BF16) for i in range(2)]
    rsb = [A.take([128, 512], F32) for i in range(2)]
    pts = [[A.take([128, 512], BF16) for i in range(3)] for m in range(2)]
    rec = A.take([128, 2, 2], F32)
    t1 = A.take([128, 2, 128], F32)
    dd = A.take([128, 2, 128], F32)
    dsq = A.take([128, 2], F32)
    drs = A.take([128, 2], F32)
    ob = A.take([128, 2, 128], BF16)
    junk2 = A.take([128, 128], BF16)
    P.op("pool", lambda: nc.gpsimd.memset(V, 1.0), writes=["Vones"])

    n_heads = 4 if stage != "gla" else 0
    for h in range(n_heads):
        ws = h % 2
        for j, col0 in enumerate((h * 128, 512 + h * 128, 1024 + h * 128)):
            P.dma("pool", lambda ws=ws, j=j, col0=col0: nc.gpsimd.dma_start(out=wda[ws][:, :, j * 128:(j + 1) * 128],
                                                                             in_=w_in_v[:, :, col0:col0 + 128]),
                  writes=[("wda", ws)], slot=("wda", ws, j))
        for tc in range(8):
            for qk, (dst, gcol, dname) in enumerate(((QT, gq, "QT"), (KT, gk, "KT"))):
                ba = (2 * tc + qk) % 2
                bb = 2 + ba
                for c in range(KC):
                    P.op("pe", lambda c=c, ba=ba, ws=ws, qk=qk, tc=tc: nc.tensor.matmul(
                        banks[ba][:], wda[ws][:, c, qk * 128:(qk + 1) * 128], UT[:, c, tc * 512:(tc + 1) * 512],
                        start=(c == 0), stop=(c == KC - 1)),
                        reads=[("wda", ws)] + [("UT", 4 * tc + i) for i in range(4)], writes=[("bank", ba)])
                P.op("act", lambda ba=ba: nc.scalar.activation(out=sqb[ba], in_=banks[ba][:], func=AF.Square),
                     reads=[("bank", ba)], writes=[("sqb", ba)])
                P.op("pe", lambda ba=ba, bb=bb: nc.tensor.matmul(banks[bb][:], bones, sqb[ba], start=True, stop=True),
                     reads=["cbf", ("sqb", ba)], writes=[("bank", bb)])
                P.op("act", lambda ba=ba, bb=bb: nc.scalar.activation(out=rsb[ba], in_=banks[bb][:], func=AF.Ln, bias=EPS),
                     reads=[("bank", bb)], writes=[("rsb", ba)])
                P.op("act", lambda ba=ba: nc.scalar.activation(out=rsb[ba], in_=rsb[ba], func=AF.Exp, scale=-0.5),
                     reads=[("rsb", ba)], writes=[("rsb", ba)])
                P.op("dve", lambda ba=ba, dst=dst, gcol=gcol, tc=tc: nc.vector.scalar_tensor_tensor(
                    out=dst[:, tc * 512:(tc + 1) * 512], in0=banks[ba][:], scalar=gcol[:, 0:1], in1=rsb[ba],
                    op0=ALU.mult, op1=ALU.mult),
                    reads=[("bank", ba), ("rsb", ba), "gq" if qk == 0 else "gk"],
                    writes=[(dname, tc)])
        for g4 in range(8):
            bv = 4 + (g4 % 2)
            for i in range(4):
                t = g4 * 4 + i
                for c in range(KC):
                    P.op("pe", lambda c=c, t=t, i=i, bv=bv, ws=ws: nc.tensor.matmul(
                        banks[bv][:, i * 128:(i + 1) * 128], UT[:, c, t * 128:(t + 1) * 128], wda[ws][:, c, 256:384],
                        start=(c == 0), stop=(c == KC - 1)),
                        reads=[("wda", ws), ("UT", t)], writes=[("bank", bv)])
            P.op("act", lambda g4=g4, bv=bv: nc.scalar.copy(out=V[:, g4 * 4:(g4 + 1) * 4, 0:128],
                                                            in_=banks[bv][:].rearrange("p (a b) -> p a b", a=4)),
                 reads=[("bank", bv), "Vones"], writes=[("V", g4)])

        for qc in range(8):
            nkt = 4 * qc + 4

            def emit_S(kt, qc=qc):
                s = kt % 2
                r = kt - 4 * qc
                q0 = 128 * r if r > 0 else 0
                for m in range(2):
                    bk = 2 * s + m
                    P.op("pe", lambda m=m, bk=bk, kt=kt, q0=q0, qc=qc: nc.tensor.matmul(
                        banks[bk][:, q0:512], KT[m * 64:(m + 1) * 64, kt * 128:(kt + 1) * 128],
                        QT[m * 64:(m + 1) * 64, qc * 512 + q0:(qc + 1) * 512], start=True, stop=True),
                        reads=[("KT", kt // 4), ("QT", qc)], writes=[("bank", bk)])

            emit_S(0)
            for kt in range(nkt):
                s = kt % 2
                ps_ = kt % 3
                r = kt - 4 * qc
                q0 = 128 * r if r > 0 else 0
                for m in range(2):
                    bk = 2 * s + m
                    P.op("act", lambda m=m, bk=bk, ps_=ps_, q0=q0: nc.scalar.activation(
                        out=pts[m][ps_][:, q0:512], in_=banks[bk][:, q0:512], func=AF.Exp),
                        reads=[("bank", bk)], writes=[("pt", m, ps_)])
                    if r >= 0:
                        P.op("pool", lambda m=m, ps_=ps_, q0=q0, r=r: nc.gpsimd.memset(
                            pts[m][ps_][64:128, 128 * r:128 * r + 64], 0.0),
                            writes=[("pt", m, ps_)])
                if kt + 1 < nkt:
                    emit_S(kt + 1)
                qs0 = r if r > 0 else 0
                for qs in range(qs0, 4):
                    last = 4 * qc + qs
                    for m in range(2):
                        bo = 4 + 2 * m + qs // 2
                        P.op("pe", lambda m=m, bo=bo, qs=qs, kt=kt, ps_=ps_, last=last: nc.tensor.matmul(
                            banks[bo][:, (qs % 2) * 129:(qs % 2) * 129 + 129], pts[m][ps_][:, qs * 128:(qs + 1) * 128],
                            V[:, kt, 0:129], start=(kt == 0 and qs % 2 == 0), stop=(kt == last), skip_group_check=True),
                            reads=[("pt", m, ps_), ("V", kt // 4), "Vones"], writes=[("bank", bo)])
                for hf in range(2):
                    if kt == 4 * qc + 2 * hf + 1:
                        b1 = 4 + hf
                        b2 = 6 + hf
                        o1 = banks[b1][:, 0:258].rearrange("p (a b) -> p a b", a=2)
                        o2 = banks[b2][:, 0:258].rearrange("p (a b) -> p a b", a=2)
                        tok0 = qc * 512 + hf * 256
                        P.op("dve", lambda o1=o1: nc.vector.reciprocal(out=rec[:, 0, :], in_=o1[:, :, 128]),
                             reads=[("bank", b1)], writes=["rec0"])
                        P.op("dve", lambda o2=o2: nc.vector.reciprocal(out=rec[:, 1, :], in_=o2[:, :, 128]),
                             reads=[("bank", b2)], writes=["rec1"])
                        P.op("dve", lambda: nc.vector.tensor_scalar(out=rec[:, 1, :], in0=rec[:, 1, :], scalar1=nlam[:, 0:1],
                                                                    scalar2=None, op0=ALU.mult),
                             reads=["rec1", "nlam"], writes=["rec1"])
                        P.op("dve", lambda o1=o1: nc.vector.tensor_tensor(
                            out=t1, in0=o1[:, :, 0:128], in1=rec[:, 0, :].unsqueeze(2).to_broadcast([128, 2, 128]), op=ALU.mult),
                            reads=[("bank", b1), "rec0"], writes=["t1"])
                        P.op("dve", lambda o2=o2: nc.vector.tensor_tensor(
                            out=dd, in0=o2[:, :, 0:128], in1=rec[:, 1, :].unsqueeze(2).to_broadcast([128, 2, 128]), op=ALU.mult),
                            reads=[("bank", b2), "rec1"], writes=["dd"])
                        P.op("dve", lambda: nc.vector.tensor_tensor(out=dd, in0=dd, in1=t1, op=ALU.add),
                             reads=["dd", "t1"], writes=["dd"])
                        for a in range(2):
                            P.op("act", lambda a=a: nc.scalar.activation(out=junk2, in_=dd[:, a, :], func=AF.Square,
                                                                          accum_out=dsq[:, a:a + 1]),
                                 reads=["dd"], writes=["junk2", ("dsq", a)])
                        P.op("act", lambda: nc.scalar.activation(out=drs, in_=dsq, func=AF.Ln, bias=EPS, scale=1.0 / 128),
                             reads=[("dsq", 0), ("dsq", 1)], writes=["drs"])
                        P.op("act", lambda: nc.scalar.activation(out=drs, in_=drs, func=AF.Exp, scale=-0.5),
                             reads=["drs"], writes=["drs"])
                        for a in range(2):
                            P.op("dve", lambda a=a: nc.vector.scalar_tensor_tensor(
                                out=ob[:, a, :], in0=dd[:, a, :], scalar=drs[:, a:a + 1], in1=gon[:], op0=ALU.mult, op1=ALU.mult),
                                reads=["dd", "drs", "gon"], writes=[("ob", a)])
                        for a in range(2):
                            P.op("pe", lambda a=a, b1=b1: nc.tensor.transpose(out=bank_bf(b1)[:, a * 128:(a + 1) * 128],
                                                                              in_=ob[:, a, :], identity=ident),
                                 reads=[("ob", a), "cbf"], writes=[("bank", b1)])
                        P.op("act", lambda b1=b1, tok0=tok0, h=h: nc.scalar.copy(out=mixT[:, h, tok0:tok0 + 256],
                                                                                  in_=bank_bf(b1)[:, 0:256]),
                             reads=[("bank", b1)], writes=[("mixT", h, tok0 // 256)])

    if stage == "da":
        for h in range(n_heads):
            P.dma("sp", lambda h=h: nc.sync.dma_start(out=dbg[:, h, :], in_=mixT[:, h, :]),
                  reads=[("mixT", h, i) for i in range(16)], slot="out")
        P.emit()
        sem, v = P.final_slots["out"]
        nc.sync.wait_ge(sem, v)
        return nc, P

    P.fence()
    A.reset()
    wg = A.take([128, KC, 1552], BF16)
    for j in range(4):
        c0 = 1536 + j * 388
        P.dma("pool", lambda j=j, c0=c0: nc.gpsimd.dma_start(out=wg[:, :, j * 388:(j + 1) * 388], in_=w_in_v[:, :, c0:c0 + 388]),
              writes=["wg"], slot=("wg", j))
    gwa = A.take([32, 256], BF16)
    P.dma("pool", lambda: nc.gpsimd.dma_start(out=gwa[0:16, :], in_=gla_gate_w), writes=["gwa"], slot="gwa0")
    P.dma("pool", lambda: nc.gpsimd.dma_start(out=gwa[16:17, :], in_=gla_gate_b), writes=["gwa"], slot="gwa1")
    grT = A.take([32, 256], BF16)
    P.op("pool", lambda: nc.gpsimd.memset(grT, 1.0), writes=["grT1"])
    spe = A.take([128, 256], F32)
    spb = A.take([128, 256], BF16)
    EP = A.take([128, 2, 256], F32)
    EN = A.take([128, 2, 256], F32)
    ELa = A.take([128, 2, 256], F32)
    ELb = A.take([128, 2, 256], F32)
    P.op("pool", lambda: nc.gpsimd.memset(ELa, 0.0), writes=["ELa0"])
    P.op("pool", lambda: nc.gpsimd.memset(ELb, 0.0), writes=["ELb0"])
    QP = A.take([128, 2, 256], BF16)
    QN = A.take([128, 2, 256], BF16)
    KNh = [A.take([128, 2, 256], BF16) for i in range(2)]
    KPh = [A.take([128, 2, 256], BF16) for i in range(2)]
    QLah = [A.take([128, 2, 256], BF16) for i in range(2)]
    QLbh = [A.take([128, 2, 256], BF16) for i in range(2)]
    EE = [A.take([128, 256], F32) for i in range(2)]
    KE = [[A.take([128, 256], BF16) for ch in range(2)] for i in range(2)]
    Vg = [A.take([128, 512], BF16) for i in range(2)]
    sg = [A.take([128, 4, 128], F32) for i in range(2)]
    sge = A.take([128, 512], F32)
    at1 = A.take([128, 4, 128], F32)
    at2 = A.take([128, 4, 128], F32)
    ATb = A.take([128, 4, 128], BF16)
    Sst = [A.take([128, 128], F32) for p in range(2)]
    Sba2 = [[A.take([128, 128], BF16) for p in range(2)] for par in range(2)]
    Sbb2 = [[A.take([128, 128], BF16) for p in range(2)] for par in range(2)]
    osq = A.take([128, 4, 128], F32)
    oss = A.take([128, 4], F32)
    ors = A.take([128, 4], F32)
    otm = A.take([128, 4, 128], F32)
    ogb = A.take([128, 4, 128], BF16)
    for p in range(2):
        P.op("pool", lambda p=p: nc.gpsimd.memset(Sst[p], 0.0), writes=[("S", p)])
        P.op("pool", lambda p=p: nc.gpsimd.memset(Sba2[0][p], 0.0), writes=[("Sba", 0, p)])

    import os
    if stage == "gla":
        P.op("pool", lambda: nc.gpsimd.memset(mixT[:, 4:8, :], 0.0), writes=[("mixTg", i) for i in range(NT)])
    CUT = int(os.environ.get('GLA_CUT', '99'))
    NG = int(os.environ.get('GLA_NG', '16'))

    class _Cut(Exception):
        pass

    CUTT = int(os.environ.get('GLA_CUTT', '0'))
    cur = {"t": 0}

    def cut(k):
        if CUT == k and cur["t"] == CUTT:
            raise _Cut()

    def gla_all():
      for g in range(NG):
          P.op("pe", lambda: nc.tensor.matmul(banks[0][:, 0:128], ident, ident, start=True, stop=True, skip_group_check=True),
               reads=["cbf"], writes=[("bank", 0)])
          for c in range(KC):
              P.op("pe", lambda c=c, g=g: nc.tensor.matmul(banks[0][0:16, 0:256], wg[:, c, 1536:1552], UT[:, c, g * 256:(g + 1) * 256],
                                                            start=(c == 0), stop=(c == KC - 1)),
                   reads=["wg", ("UT", 2 * g), ("UT", 2 * g + 1)], writes=[("bank", 0)])
          P.op("act", lambda: nc.scalar.copy(out=grT[0:16, :], in_=banks[0][0:16, 0:256]),
               reads=[("bank", 0), "grT1"], writes=["grT"])
          for tt in range(2):
              t = 2 * g + tt
              cur["t"] = t
              cs = slice(tt * 128, (tt + 1) * 128)
              cut(1)
              P.op("pe", lambda cs=cs: nc.tensor.matmul(banks[0][:, 256:512], grT[0:17, cs], gwa[0:17, :], start=True, stop=True),
                   reads=["grT", "gwa"], writes=[("bank", 0)])
              P.op("act", lambda: nc.scalar.activation(out=spe, in_=banks[0][:, 256:512], func=AF.Exp, scale=-1.0),
                   reads=[("bank", 0)], writes=["spe"])
              P.op("act", lambda: nc.scalar.activation(out=spb, in_=spe, func=AF.Ln, bias=1.0),
                   reads=["spe"], writes=["spb"])
              cut(2)
              for p in range(2):
                  P.op("pe", lambda p=p: nc.tensor.matmul(banks[1][:, p * 128:(p + 1) * 128], spb[:, p * 128:(p + 1) * 128], TriCn,
                                                           start=True, stop=True),
                       reads=["spb", "cbf"], writes=[("bank", 1)])
              for p in range(2):
                  P.op("pe", lambda p=p: nc.tensor.matmul(banks[1][:, 256 + p * 128:256 + (p + 1) * 128],
                                                           spb[:, p * 128:(p + 1) * 128], Trin, start=True, stop=True),
                       reads=["spb", "cbf"], writes=[("bank", 1)])
              P.op("pe", lambda: nc.tensor.matmul(banks[2][:, 0:256], TriEn, spb, start=True, stop=True),
                   reads=["spb", "cbf"], writes=[("bank", 2)])
              lc = banks[1][:, 0:256].rearrange("p (a b) -> p a b", a=2)
              lt = banks[1][:, 256:512].rearrange("p (a b) -> p a b", a=2)
              P.op("act", lambda lc=lc, cs=cs: nc.scalar.activation(out=EP[:, :, cs], in_=lc, func=AF.Exp),
                   reads=[("bank", 1)], writes=["EP"])
              P.op("act", lambda lc=lc, cs=cs: nc.scalar.activation(out=EN[:, :, cs], in_=lc, func=AF.Exp, scale=-1.0),
                   reads=[("bank", 1)], writes=["EN"])
              P.op("act", lambda lt=lt, tt=tt: nc.scalar.activation(out=ELa[:, :, tt * 128:tt * 128 + 64], in_=lt[:, :, 0:64], func=AF.Exp),
                   reads=[("bank", 1), "ELa0"], writes=["ELa"])
              P.op("act", lambda lt=lt, tt=tt: nc.scalar.activation(out=ELb[:, :, tt * 128 + 64:tt * 128 + 128], in_=lt[:, :, 64:128], func=AF.Exp),
                   reads=[("bank", 1), "ELb0"], writes=["ELb"])
              P.op("act", lambda tt=tt: nc.scalar.activation(out=EE[tt], in_=banks[2][:, 0:256], func=AF.Exp),
                   reads=[("bank", 2)], writes=[("EE", tt)])
              cut(3)
              for c in range(KC):
                  P.op("pe", lambda c=c, t=t: nc.tensor.matmul(banks[2][:, 256:512], UT[:, c, t * 128:(t + 1) * 128], wg[:, c, 256:512],
                                                                start=(c == 0), stop=(c == KC - 1), skip_group_check=True),
                       reads=["wg", ("UT", t)], writes=[("bank", 2)])
              for c in range(KC):
                  P.op("pe", lambda c=c, t=t: nc.tensor.matmul(banks[5][:], UT[:, c, t * 128:(t + 1) * 128], wg[:, c, 512:1024],
                                                                start=(c == 0), stop=(c == KC - 1)),
                       reads=["wg", ("UT", t)], writes=[("bank", 5)])
              for c in range(KC):
                  P.op("pe", lambda c=c, t=t: nc.tensor.matmul(banks[6][:], UT[:, c, t * 128:(t + 1) * 128], wg[:, c, 1024:1536],
                                                                start=(c == 0), stop=(c == KC - 1)),
                       reads=["wg", ("UT", t)], writes=[("bank", 6)])
              for ch in range(2):
                  P.op("dve", lambda tt=tt, ch=ch: nc.vector.scalar_tensor_tensor(out=KE[tt][ch], in0=banks[2][:, 256:512],
                                                                                  scalar=cf32[:, 288 + ch:289 + ch], in1=EE[tt],
                                                                                  op0=ALU.mult, op1=ALU.mult),
                       reads=[("bank", 2), ("EE", tt), "cf32"], writes=[("KE", tt)])
              P.op("act", lambda tt=tt: nc.scalar.copy(out=Vg[tt], in_=banks[5][:]), reads=[("bank", 5)], writes=[("Vg", tt)])
              cut(4)
              P.op("act", lambda: nc.scalar.activation(out=sge, in_=banks[6][:], func=AF.Exp, scale=-1.0),
                   reads=[("bank", 6)], writes=["sge"])
              P.op("dve", lambda: nc.vector.tensor_scalar(out=sge, in0=sge, scalar1=1.0, scalar2=None, op0=ALU.add),
                   reads=["sge"], writes=["sge"])
              P.op("dve", lambda: nc.vector.reciprocal(out=sge, in_=sge), reads=["sge"], writes=["sge"])
              P.op("dve", lambda tt=tt: nc.vector.tensor_tensor(out=sg[tt].rearrange("p a b -> p (a b)"), in0=banks[6][:], in1=sge, op=ALU.mult),
                   reads=[("bank", 6), "sge"], writes=[("sg", tt)])
              P.op("dve", lambda tt=tt: nc.vector.tensor_tensor(out=sg[tt], in0=sg[tt],
                                                                  in1=ggl[:].unsqueeze(1).to_broadcast([128, 4, 128]), op=ALU.mult),
                   reads=[("sg", tt), "ggl"], writes=[("sg", tt)])
          cut(5)
          for qk, bk in ((0, 3), (1, 4)):
              for p in range(2):
                  for c in range(KC):
                      P.op("pe", lambda c=c, p=p, qk=qk, bk=bk, g=g: nc.tensor.matmul(
                          banks[bk][:, p * 256:(p + 1) * 256], wg[:, c, qk * 256 + p * 128:qk * 256 + (p + 1) * 128],
                          UT[:, c, g * 256:(g + 1) * 256], start=(c == 0), stop=(c == KC - 1), skip_group_check=True),
                          reads=["wg", ("UT", 2 * g), ("UT", 2 * g + 1)], writes=[("bank", bk)])
          qv = banks[3][:].rearrange("p (a b) -> p a b", a=2)
          kv = banks[4][:].rearrange("p (a b) -> p a b", a=2)
          for dst, src, ee, nm, sc in ((QP, qv, EP, "QP", 0.125), (QN, qv, EN, "QN", 0.125)):
              P.op("dve", lambda dst=dst, src=src, ee=ee, sc=sc: nc.vector.scalar_tensor_tensor(
                  out=dst, in0=src, scalar=sc, in1=ee, op0=ALU.mult, op1=ALU.mult),
                  reads=[("bank", 3), {"QP": "EP", "QN": "EN"}[nm]], writes=[nm])
          for hh in range(2):
              for dst, src, ee, nm, ekey, mcol, bk in ((QLah[hh], qv, ELa, "QLa", "ELa", 290 + hh, 3), (QLbh[hh], qv, ELb, "QLb", "ELb", 290 + hh, 3),
                                                       (KNh[hh], kv, EN, "KN", "EN", 288 + hh, 4), (KPh[hh], kv, EP, "KP", "EP", 288 + hh, 4)):
                  P.op("dve", lambda dst=dst, src=src, ee=ee, mcol=mcol: nc.vector.scalar_tensor_tensor(
                      out=dst, in0=src, scalar=cf32[:, mcol:mcol + 1], in1=ee, op0=ALU.mult, op1=ALU.mult),
                      reads=[("bank", bk), ekey, "cf32"], writes=[(nm, hh)])
          for tt in range(2):
              t = 2 * g + tt
              cur["t"] = t
              cs = slice(tt * 128, (tt + 1) * 128)
              cut(6)
              P.op("pe", lambda: nc.tensor.matmul(banks[5][:, 0:128], ident, ident, start=True, stop=True, skip_group_check=True),
                   reads=["cbf"], writes=[("bank", 5)])
              for h in range(4):
                  p, hh = h // 2, h % 2
                  rows = slice(hh * 64, (hh + 1) * 64)
                  P.op("pe", lambda h=h, p=p, hh=hh, cs=cs: nc.tensor.matmul(
                      banks[5][:, h * 128:(h + 1) * 128], KNh[hh][:, p, cs], QP[:, p, cs], start=True, stop=True, skip_group_check=True),
                      reads=[("KN", hh), "QP"], writes=[("bank", 5)])
                  P.op("pe", lambda h=h, p=p, hh=hh, cs=cs: nc.tensor.matmul(
                      banks[6][:, h * 128:(h + 1) * 128], KPh[hh][:, p, cs], QN[:, p, cs], start=True, stop=True, skip_group_check=True),
                      reads=[("KP", hh), "QN"], writes=[("bank", 6)])
              P.op("dve", lambda: nc.vector.tensor_tensor(out=at1, in0=banks[5][:].rearrange("p (a b) -> p a b", a=4),
                                                          in1=maskP.unsqueeze(1).to_broadcast([128, 4, 128]), op=ALU.mult),
                   reads=[("bank", 5), "cf32"], writes=["at1"])
              P.op("dve", lambda: nc.vector.tensor_tensor(out=at2, in0=banks[6][:].rearrange("p (a b) -> p a b", a=4),
                                                          in1=maskF.unsqueeze(1).to_broadcast([128, 4, 128]), op=ALU.mult),
                   reads=[("bank", 6), "cf32"], writes=["at2"])
              P.op("dve", lambda: nc.vector.tensor_tensor(out=ATb, in0=at1, in1=at2, op=ALU.add),
                   reads=["at1", "at2"], writes=["ATb"])
              cut(7)
              for ch, bu in ((0, 7), (1, 4)):
                  rows = slice(ch * 64, (ch + 1) * 64)
                  for p in range(2):
                      P.op("pe", lambda p=p, ch=ch, bu=bu, tt=tt: nc.tensor.matmul(
                          banks[bu][:, p * 256:(p + 1) * 256], KE[tt][ch][:, p * 128:(p + 1) * 128], Vg[tt][:, p * 256:(p + 1) * 256],
                          start=True, stop=True, skip_group_check=True),
                          reads=[("KE", tt), ("Vg", tt)], writes=[("bank", bu)])
              par = t % 2
              Sba, Sbb = Sba2[par], Sbb2[par]

              def upd(ch, bu, snap, sname, tt=tt):
                  dcol = tt * 128 + ch * 64 + 63
                  els = ELa if ch == 0 else ELb
                  for p in range(2):
                      for hh in range(2):
                          if ch == 1 and os.environ.get("GLA_VAR") == "B":
                              continue
                          rows = slice(hh * 64, (hh + 1) * 64)
                          P.op("dve", lambda p=p, rows=rows, bu=bu, els=els, dcol=dcol, hh=hh: nc.vector.scalar_tensor_tensor(
                              out=Sst[p][rows, :], in0=Sst[p][rows, :], scalar=els[rows, p, dcol:dcol + 1],
                              in1=banks[bu][rows, p * 256 + hh * 128:p * 256 + (hh + 1) * 128], op0=ALU.mult, op1=ALU.add),
                              reads=[("S", p), ("bank", bu), "ELa" if ch == 0 else "ELb"], writes=[("S", p)])
                      if ch == 1 and os.environ.get("GLA_VAR") == "A":
                          continue
                      P.op("act", lambda p=p, snap=snap: nc.scalar.copy(out=snap[p], in_=Sst[p]),
                           reads=[("S", p)], writes=[sname + (p,)])

              cut(8)
              upd(0, 7, Sbb, ("Sbb", par))
              cut(9)
              for h in range(4):
                  p, hh = h // 2, h % 2
                  rows = slice(hh * 64, (hh + 1) * 64)
                  oc = slice(h * 128, (h + 1) * 128)
                  P.op("pe", lambda p=p, hh=hh, oc=oc, cs=cs, Sba=Sba: nc.tensor.matmul(banks[1][:, oc], QLah[hh][:, p, cs], Sba[p],
                                                                                    start=True, stop=False, skip_group_check=True),
                       reads=[("QLa", hh), ("Sba", par, p)], writes=[("bank", 1)])
                  P.op("pe", lambda p=p, hh=hh, oc=oc, cs=cs, Sbb=Sbb: nc.tensor.matmul(banks[1][:, oc], QLbh[hh][:, p, cs], Sbb[p],
                                                                                    start=False, stop=False, skip_group_check=True),
                       reads=[("QLb", hh), ("Sbb", par, p)], writes=[("bank", 1)])
                  P.op("pe", lambda h=h, oc=oc, tt=tt: nc.tensor.matmul(banks[1][:, oc], ATb[:, h, :], Vg[tt][:, oc],
                                                                         start=False, stop=True, skip_group_check=True),
                       reads=["ATb", ("Vg", tt)], writes=[("bank", 1)])
              cut(11)
              upd(1, 4, Sba2[1 - par], ("Sba", 1 - par))
              cut(10)
              o4 = banks[1][:].rearrange("p (a b) -> p a b", a=4)
              P.op("act", lambda: nc.scalar.activation(out=osq.rearrange("p a b -> p (a b)"), in_=banks[1][:], func=AF.Square),
                   reads=[("bank", 1)], writes=["osq"])
              P.op("dve", lambda: nc.vector.reduce_sum(out=oss, in_=osq, axis=AX.X), reads=["osq"], writes=["oss"])
              P.op("act", lambda: nc.scalar.activation(out=ors, in_=oss, func=AF.Ln, bias=EPS, scale=1.0 / 128),
                   reads=["oss"], writes=["ors"])
              P.op("act", lambda: nc.scalar.activation(out=ors, in_=ors, func=AF.Exp, scale=-0.5), reads=["ors"], writes=["ors"])
              P.op("dve", lambda o4=o4: nc.vector.tensor_tensor(out=otm, in0=o4, in1=ors.unsqueeze(2).to_broadcast([128, 4, 128]),
                                                                 op=ALU.mult),
                   reads=[("bank", 1), "ors"], writes=["otm"])
              P.op("dve", lambda tt=tt: nc.vector.tensor_tensor(out=ogb, in0=otm, in1=sg[tt], op=ALU.mult),
                   reads=["otm", ("sg", tt)], writes=["ogb"])
              cut(121)
              for h in range(4):
                  P.op("pe", lambda h=h: nc.tensor.transpose(out=bank_bf(3)[:, h * 128:(h + 1) * 128], in_=ogb[:, h, :], identity=ident),
                       reads=["ogb", "cbf"], writes=[("bank", 3)])
              cut(122)
              P.op("act", lambda t=t: nc.scalar.copy(out=mixT[:, 4:8, t * 128:(t + 1) * 128],
                                                     in_=bank_bf(3)[:, 0:512].rearrange("p (a b) -> p a b", a=4)),
                   reads=[("bank", 3)], writes=[("mixTg", t)])


    try:
        gla_all()
    except _Cut:
        pass

    if stage == "gla":
        for h in range(4):
            P.dma("sp", lambda h=h: nc.sync.dma_start(out=dbg[:, h, :], in_=mixT[:, 4 + h, :]),
                  reads=[("mixTg", i) for i in range(NT)], slot="out")
        P.emit()
        sem, v = P.final_slots["out"]
        nc.sync.wait_ge(sem, v)
        return nc, P


    P.fence()
    A.reset()
    A2 = Arena(UT[:].rearrange("p c t -> p (c t)"), KC * T)
    w_o_v = w_o.rearrange("(c p) n -> p c n", p=128)
    w_cq_v = w_cq.rearrange("(c p) n -> p c n", p=128)
    w_co_v = w_co.rearrange("(c p) n -> p c n", p=128)
    w_ckv_v = w_ckv.rearrange("(c p) n -> p c n", p=128)
    wo = A.take([128, KC, D], BF16)
    wcq = A.take([128, KC, D], BF16)
    wco = A.take([128, KC, D], BF16)
    for nm_, dst_, src_ in (("wo", wo, w_o_v), ("wcq", wcq, w_cq_v), ("wco", wco, w_co_v)):
        for hf_ in range(2):
            P.dma("pool", lambda dst_=dst_, src_=src_, hf_=hf_: nc.gpsimd.dma_start(out=dst_[:, :, hf_ * 512:(hf_ + 1) * 512],
                                                                                     in_=src_[:, :, hf_ * 512:(hf_ + 1) * 512]),
                  writes=[nm_], slot=(nm_, hf_))
    gcross = A.take([128, D], F32)
    gffn = A.take([128, D], F32)
    gcq = A.take([128, 4, 256], F32)
    gck = A.take([128, 4, 256], F32)
    P.dma("sp", lambda: nc.sync.dma_start(out=gcross, in_=norm_cross.partition_broadcast(128)), writes=["gcross"], slot="t0")
    P.dma("sp", lambda: nc.sync.dma_start(out=gffn, in_=norm_ffn.partition_broadcast(128)), writes=["gffn"], slot="t1")
    for hq in range(4):
        P.dma("sp", lambda hq=hq: nc.sync.dma_start(out=gcq[:, hq, :], in_=cross_q_norm.partition_broadcast(128)), writes=["gcq"], slot="t2")
        P.dma("sp", lambda hq=hq: nc.sync.dma_start(out=gck[:, hq, :], in_=cross_k_norm.partition_broadcast(128)), writes=["gck"], slot="t3")
    P.op("dve", lambda: nc.vector.tensor_scalar(out=gcq, in0=gcq, scalar1=1.0 / 16, scalar2=None, op0=ALU.mult), reads=["gcq"], writes=["gcq"])
    wr = A.take([128, KC, 36], BF16)
    P.dma("pool", lambda: nc.gpsimd.dma_start(out=wr[:, :, 0:4], in_=w_group.rearrange("(c p) n -> p c n", p=128)), writes=["wr"], slot="t4")
    P.dma("pool", lambda: nc.gpsimd.dma_start(out=wr[:, :, 4:36], in_=w_expert.rearrange("(c p) n -> p c n", p=128)), writes=["wr"], slot="t5")
    rbias = A.take([128, 36], F32)
    P.dma("sp", lambda: nc.sync.dma_start(out=rbias[:, 0:4], in_=b_group.partition_broadcast(128)), writes=["rbias"], slot="t6")
    P.dma("sp", lambda: nc.sync.dma_start(out=rbias[:, 4:36], in_=b_expert.partition_broadcast(128)), writes=["rbias"], slot="t7")
    _bc = {}

    def get_bc():
        if "r" not in _bc:
            _bc["r"] = nc.gpsimd.to_reg(NSLOT - 1)
        return _bc["r"]

    destAll = P.sb("destAll", [128, NT, 2], I32)
    gateAll = P.sb("gateAll", [128, NT, 2], F32)
    base = A.take([128, 32], F32)
    P.op("dve", lambda: nc.vector.memset(base, 0.0), writes=["base"])

    zt = A.take([128, D], BF16)
    P.op("dve", lambda: nc.vector.memset(zt, 0.0), writes=["zt"])
    xp_z = x_pad.rearrange("(n p) d -> n p d", p=128)
    for zi in range(NSLOT // 128):
        P.dma("sp", lambda zi=zi: nc.sync.dma_start(out=xp_z[zi], in_=zt), reads=["zt"], writes=["x_pad"], slot="xz")

    KcT = A2.take([128, KC, 256], BF16)
    Vc = A2.take([128, 2, D], BF16)
    a2_mark = A2.off
    gmem = A2.take([128, D], F32)
    P.dma("sp", lambda: nc.sync.dma_start(out=gmem, in_=norm_mem.partition_broadcast(128)), writes=["gmem"], slot="t8")
    wkv = A2.take([128, KC, D], BF16)
    MT = A2.take([128, KC, 256], BF16)
    xtm = [A2.take([128, D], F32) for i in range(2)]
    scr0 = (A2.take([128, D], BF16), A2.take([128, 4], F32), A2.take([128, 4], F32), A2.take([128, D], F32), "m")
    nb16 = A2.take([128, D], BF16)

    def transpose8(src_bf, src_key, dst3, dst_key, copy_eng):
        for c in range(KC):
            P.op("pe", lambda c=c, src_bf=src_bf: nc.tensor.transpose(out=bank_bf(4)[:, c * 128:(c + 1) * 128],
                                                                      in_=src_bf[:, c * 128:(c + 1) * 128], identity=ident),
                 reads=[src_key, "cbf"], writes=[("bank", 4)])
        if copy_eng == "act":
            P.op("act", lambda dst3=dst3: nc.scalar.copy(out=dst3, in_=bank_bf(4).rearrange("p (c n) -> p c n", c=KC)),
                 reads=[("bank", 4)], writes=[dst_key])
        else:
            P.op("dve", lambda dst3=dst3: nc.vector.tensor_copy(out=dst3, in_=bank_bf(4).rearrange("p (c n) -> p c n", c=KC)),
                 reads=[("bank", 4)], writes=[dst_key])

    def rms_rows(srcap, src_keys, ngrp, gain3, gain_key, dst_bf, dst_key, scr):
        sj, stss, strs, snf, tag = scr
        kj, kt_, kr, kn = ("junk3", tag), ("tss", tag), ("trs", tag), ("nf32", tag)
        w = D // ngrp
        for gi in range(ngrp):
            P.op("act", lambda gi=gi, sj=sj, stss=stss, srcap=srcap, w=w: nc.scalar.activation(
                out=sj[:, 0:w], in_=srcap[:, gi * w:(gi + 1) * w], func=AF.Square, accum_out=stss[:, gi:gi + 1]),
                reads=list(src_keys), writes=[kj, kt_])
        P.op("act", lambda stss=stss, strs=strs, ngrp=ngrp, w=w: nc.scalar.activation(out=strs[:, 0:ngrp], in_=stss[:, 0:ngrp], func=AF.Ln,
                                                                                      bias=EPS, scale=1.0 / w),
             reads=[kt_], writes=[kr])
        P.op("act", lambda strs=strs, ngrp=ngrp: nc.scalar.activation(out=strs[:, 0:ngrp], in_=strs[:, 0:ngrp], func=AF.Exp, scale=-0.5),
             reads=[kr], writes=[kr])
        P.op("dve", lambda snf=snf, srcap=srcap, strs=strs, ngrp=ngrp, w=w: nc.vector.tensor_tensor(
            out=snf.rearrange("p (a b) -> p a b", a=ngrp), in0=srcap.rearrange("p (a b) -> p a b", a=ngrp),
            in1=strs[:, 0:ngrp].unsqueeze(2).to_broadcast([128, ngrp, w]), op=ALU.mult),
            reads=list(src_keys) + [kr], writes=[kn])
        P.op("dve", lambda snf=snf, dst_bf=dst_bf, gain3=gain3: nc.vector.tensor_tensor(out=dst_bf, in0=snf, in1=gain3, op=ALU.mult),
             reads=[kn, gain_key], writes=[dst_key])

    for mt in range(2):
        P.dma("sp", lambda mt=mt: nc.sync.dma_start(out=xtm[mt], in_=mem[mt * 128:(mt + 1) * 128, :]), writes=[("xtm", mt)], slot=("xtm", mt))
        rms_rows(xtm[mt], [("xtm", mt)], 1, gmem, "gmem", nb16, "nb16m", scr0)
        transpose8(nb16, "nb16m", MT[:, :, mt * 128:(mt + 1) * 128], ("MT", mt), "act")
    for part in range(2):
        for hf_ in range(2):
            P.dma("pool", lambda part=part, hf_=hf_: nc.gpsimd.dma_start(
                out=wkv[:, :, hf_ * 512:(hf_ + 1) * 512], in_=w_ckv_v[:, :, part * D + hf_ * 512:part * D + (hf_ + 1) * 512]),
                writes=["wkv"], slot=("wkv", hf_))
        for mt in range(2):
            for hf_ in range(2):
                for c in range(KC):
                    P.op("pe", lambda c=c, mt=mt, hf_=hf_: nc.tensor.matmul(banks[hf_], MT[:, c, mt * 128:(mt + 1) * 128],
                                                                             wkv[:, c, hf_ * 512:(hf_ + 1) * 512],
                                                                             start=(c == 0), stop=(c == KC - 1)),
                         reads=[("MT", 0), ("MT", 1), "wkv"], writes=[("bank", hf_)])
            if part == 0:
                rms_rows(bigv[0], [("bank", 0), ("bank", 1)], 4, gck.rearrange("p a b -> p (a b)"), "gck", nb16, "nb16m", scr0)
                transpose8(nb16, "nb16m", KcT[:, :, mt * 128:(mt + 1) * 128], ("KcT", mt), "act")
            else:
                P.op("act", lambda mt=mt: nc.scalar.copy(out=Vc[:, mt, :], in_=bigv[0]), reads=[("bank", 0), ("bank", 1)], writes=[("Vc", mt)])

    P.fence()
    A2.off = a2_mark
    xtt = [A2.take([128, D], F32) for i in range(2)]
    scr1 = (A2.take([128, D], BF16), A2.take([128, 4], F32), A2.take([128, 4], F32), A2.take([128, D], F32), "t")
    h1 = A2.take([128, D], F32)
    u2 = A2.take([128, D], BF16)
    U2T = A2.take([128, KC, 128], BF16)
    qnb = A2.take([128, D], BF16)
    QcT = A2.take([128, KC, 128], BF16)
    PT = A2.take([128, 8, 128], BF16)
    ocb = A2.take([128, 4, 256], BF16)
    OcT = A2.take([128, KC, 128], BF16)
    h2 = [A2.take([128, D], F32) for i in range(2)]
    xfb = [A2.take([128, D], BF16) for i in range(2)]
    XfT = A2.take([128, KC, 128], BF16)
    csum = A2.take([128, 4], F32)
    L = A2.take([128, 36], F32)
    rt = A2.take([128, 64], F32)
    tmp48 = A2.take([128, 4, 8], F32)
    OH1 = A2.take([128, 4, 8], F32)
    OH2 = A2.take([128, 4, 8], F32)
    OHs = A2.take([128, 32], BF16)
    posE = A2.take([128, 32], F32)
    t32 = A2.take([128, 32], F32)
    dstf = A2.take([128, 2], F32)
    NT_RUN = int(os.environ.get("TAIL_NT", str(NT)))

    def tail_a(t):
        tk = slice(t * 128, (t + 1) * 128)
        xs = t % 2
        P.dma("sp", lambda t=t, xs=xs: nc.sync.dma_start(out=xtt[xs], in_=x[t * 128:(t + 1) * 128, :]), writes=[("xtt", xs)], slot=("xtt", xs))
        for hf_ in range(2):
            for c in range(KC):
                P.op("pe", lambda c=c, hf_=hf_, tk=tk: nc.tensor.matmul(banks[hf_], mixT[:, c, tk], wo[:, c, hf_ * 512:(hf_ + 1) * 512],
                                                                         start=(c == 0), stop=(c == KC - 1)),
                     reads=["wo", ("mixT", c, t // 2) if c < 4 else ("mixTg", t)], writes=[("bank", hf_)])
        P.op("dve", lambda xs=xs: nc.vector.tensor_tensor(out=h1, in0=bigv[0], in1=xtt[xs], op=ALU.add),
             reads=[("bank", 0), ("bank", 1), ("xtt", xs)], writes=["h1"])
        rms_rows(h1, ["h1"], 1, gcross, "gcross", u2, "u2", scr1)
        transpose8(u2, "u2", U2T, "U2T", "act")
        for hf_ in range(2):
            for c in range(KC):
                P.op("pe", lambda c=c, hf_=hf_: nc.tensor.matmul(banks[2 + hf_], U2T[:, c, :], wcq[:, c, hf_ * 512:(hf_ + 1) * 512],
                                                                  start=(c == 0), stop=(c == KC - 1)),
                     reads=["wcq", "U2T"], writes=[("bank", 2 + hf_)])
        rms_rows(bigv[1], [("bank", 2), ("bank", 3)], 4, gcq.rearrange("p a b -> p (a b)"), "gcq", qnb, "qnb", scr1)
        transpose8(qnb, "qnb", QcT, "QcT", "dve")
        for hq in range(4):
            for mt in range(2):
                col = (hq * 2 + mt) * 128
                bk = 6 + col // 512
                for dc in range(2):
                    P.op("pe", lambda hq=hq, mt=mt, dc=dc, col=col, bk=bk: nc.tensor.matmul(
                        banks[bk][:, col % 512:col % 512 + 128], KcT[:, 2 * hq + dc, mt * 128:(mt + 1) * 128], QcT[:, 2 * hq + dc, :],
                        start=(dc == 0), stop=(dc == 1), skip_group_check=True),
                        reads=[("KcT", 0), ("KcT", 1), "QcT"], writes=[("bank", bk)])
        P.op("act", lambda: nc.scalar.activation(out=PT.rearrange("p a b -> p (a b)"), in_=bigv[3], func=AF.Exp),
             reads=[("bank", 6), ("bank", 7)], writes=["PT"])
        for hq in range(4):
            for mt in range(2):
                P.op("pe", lambda hq=hq, mt=mt: nc.tensor.matmul(banks[2 + hq // 2][:, (hq % 2) * 256:(hq % 2) * 256 + 256], PT[:, hq * 2 + mt, :],
                                                                  Vc[:, mt, hq * 256:(hq + 1) * 256], start=(mt == 0), stop=(mt == 1),
                                                                  skip_group_check=True),
                     reads=["PT", ("Vc", 0), ("Vc", 1)], writes=[("bank", 2 + hq // 2)])
            for mt in range(2):
                P.op("pe", lambda hq=hq, mt=mt: nc.tensor.matmul(banks[5][:, hq:hq + 1], PT[:, hq * 2 + mt, :], ones_bf[:, 0:1],
                                                                  start=(mt == 0), stop=(mt == 1), skip_group_check=True),
                     reads=["PT", "cbf"], writes=[("bank", 5)])
        P.op("dve", lambda: nc.vector.reciprocal(out=csum, in_=banks[5][:, 0:4]), reads=[("bank", 5)], writes=["csum"])
        P.op("dve", lambda: nc.vector.tensor_tensor(out=ocb, in0=bigv[1].rearrange("p (a b) -> p a b", a=4),
                                                    in1=csum.unsqueeze(2).to_broadcast([128, 4, 256]), op=ALU.mult),
             reads=[("bank", 2), ("bank", 3), "csum"], writes=["ocb"])
        transpose8(ocb.rearrange("p a b -> p (a b)"), "ocb", OcT, "OcT", "act")
        for hf_ in range(2):
            for c in range(KC):
                P.op("pe", lambda c=c, hf_=hf_: nc.tensor.matmul(banks[hf_], OcT[:, c, :], wco[:, c, hf_ * 512:(hf_ + 1) * 512],
                                                                  start=(c == 0), stop=(c == KC - 1)),
                     reads=["wco", "OcT"], writes=[("bank", hf_)])
        P.op("dve", lambda xs=xs: nc.vector.tensor_tensor(out=h2[xs], in0=bigv[0], in1=h1, op=ALU.add),
             reads=[("bank", 0), ("bank", 1), "h1"], writes=[("h2", xs)])
        P.dma("sp", lambda t=t, xs=xs: nc.sync.dma_start(out=out[t * 128:(t + 1) * 128, :], in_=h2[xs]),
              reads=[("h2", xs)], writes=[("h2d", t)], slot=("h2d", xs))

    def tail_b(t):
        tk = slice(t * 128, (t + 1) * 128)
        xs = t % 2
        rms_rows(h2[xs], [("h2", xs)], 1, gffn, "gffn", xfb[xs], ("xfb", xs), scr1)
        transpose8(xfb[xs], ("xfb", xs), XfT, "XfT", "dve")
        for c in range(KC):
            P.op("pe", lambda c=c: nc.tensor.matmul(banks[5][:, 8:44], XfT[:, c, :], wr[:, c, :], start=(c == 0), stop=(c == KC - 1),
                                                     skip_group_check=True),
                 reads=["XfT", "wr"], writes=[("bank", 5)])
        P.op("dve", lambda: nc.vector.tensor_tensor(out=L, in0=banks[5][:, 8:44], in1=rbias, op=ALU.add),
             reads=[("bank", 5), "rbias"], writes=["L"])
        gl = L[:, 0:4]
        el = L[:, 4:36].rearrange("p (a b) -> p a b", a=4)
        R = "rt"
        P.op("dve", lambda: nc.vector.reduce_max(out=rt[:, 0:1], in_=gl, axis=AX.X), reads=["L"], writes=[R])
        P.op("dve", lambda: nc.vector.tensor_scalar(out=rt[:, 1:2], in0=rt[:, 0:1], scalar1=-1.0, scalar2=None, op0=ALU.mult), reads=[R], writes=[R])
        P.op("dve", lambda: nc.vector.tensor_scalar(out=rt[:, 16:20], in0=gl, scalar1=rt[:, 0:1], scalar2=None, op0=ALU.is_ge), reads=["L", R], writes=[R])
        P.op("act", lambda: nc.scalar.activation(out=rt[:, 20:24], in_=gl, func=AF.Exp, bias=rt[:, 1:2], accum_out=rt[:, 2:3]), reads=["L", R], writes=[R])
        P.op("dve", lambda: nc.vector.reciprocal(out=rt[:, 3:4], in_=rt[:, 2:3]), reads=[R], writes=[R])
        P.op("dve", lambda: nc.vector.tensor_tensor(out=tmp48, in0=el, in1=rt[:, 16:20].unsqueeze(2).to_broadcast([128, 4, 8]), op=ALU.mult),
             reads=["L", R], writes=["tmp48"])
        P.op("dve", lambda: nc.vector.reduce_sum(out=rt[:, 24:32], in_=tmp48.rearrange("p a b -> p b a"), axis=AX.X), reads=["tmp48"], writes=[R])
        P.op("dve", lambda: nc.vector.reduce_max(out=rt[:, 4:5], in_=rt[:, 24:32], axis=AX.X), reads=[R], writes=[R])
        P.op("dve", lambda: nc.vector.tensor_scalar(out=rt[:, 32:40], in0=rt[:, 24:32], scalar1=rt[:, 4:5], scalar2=None, op0=ALU.is_ge), reads=[R], writes=[R])
        P.op("dve", lambda: nc.vector.scalar_tensor_tensor(out=rt[:, 40:48], in0=rt[:, 32:40], scalar=-1.0e9, in1=rt[:, 24:32],
                                                            op0=ALU.mult, op1=ALU.add), reads=[R], writes=[R])
        P.op("dve", lambda: nc.vector.reduce_max(out=rt[:, 5:6], in_=rt[:, 40:48], axis=AX.X), reads=[R], writes=[R])
        P.op("dve", lambda: nc.vector.tensor_scalar(out=rt[:, 48:56], in0=rt[:, 40:48], scalar1=rt[:, 5:6], scalar2=None, op0=ALU.is_ge), reads=[R], writes=[R])
        P.op("dve", lambda: nc.vector.tensor_tensor(out=rt[:, 6:7], in0=rt[:, 5:6], in1=rt[:, 4:5], op=ALU.subtract), reads=[R], writes=[R])
        P.op("act", lambda: nc.scalar.activation(out=rt[:, 7:8], in_=rt[:, 6:7], func=AF.Exp), reads=[R], writes=[R])
        P.op("dve", lambda: nc.vector.tensor_scalar(out=rt[:, 7:8], in0=rt[:, 7:8], scalar1=1.0, scalar2=None, op0=ALU.add), reads=[R], writes=[R])
        P.op("dve", lambda: nc.vector.reciprocal(out=rt[:, 7:8], in_=rt[:, 7:8]), reads=[R], writes=[R])
        P.op("dve", lambda: nc.vector.tensor_tensor(out=rt[:, 8:9], in0=rt[:, 7:8], in1=rt[:, 3:4], op=ALU.mult), reads=[R], writes=[R])
        P.op("dve", lambda: nc.vector.tensor_tensor(out=rt[:, 9:10], in0=rt[:, 3:4], in1=rt[:, 8:9], op=ALU.subtract), reads=[R], writes=[R])
        P.op("dve", lambda t=t: nc.vector.tensor_copy(out=gateAll[:, t, :], in_=rt[:, 8:10]), reads=[R], writes=[("gate", t)])
        for ohx, c0_, nm2 in ((OH1, 32, "OH1"), (OH2, 48, "OH2")):
            P.op("dve", lambda ohx=ohx, c0_=c0_: nc.vector.tensor_tensor(
                out=ohx, in0=rt[:, 16:20].unsqueeze(2).to_broadcast([128, 4, 8]),
                in1=rt[:, c0_:c0_ + 8].unsqueeze(1).to_broadcast([128, 4, 8]), op=ALU.mult), reads=[R], writes=[nm2])
        P.op("dve", lambda: nc.vector.tensor_tensor(out=OHs, in0=OH1.rearrange("p a b -> p (a b)"), in1=OH2.rearrange("p a b -> p (a b)"), op=ALU.add),
             reads=["OH1", "OH2"], writes=["OHs"])
        P.op("pe", lambda: nc.tensor.matmul(banks[5][:, 64:96], TriS, OHs, start=True, stop=True, skip_group_check=True),
             reads=["OHs", "cbf"], writes=[("bank", 5)])
        P.op("pe", lambda: nc.tensor.matmul(banks[5][:, 96:128], ones_bf, OHs, start=True, stop=True, skip_group_check=True),
             reads=["OHs", "cbf"], writes=[("bank", 5)])
        P.op("dve", lambda: nc.vector.tensor_tensor(out=posE, in0=banks[5][:, 64:96], in1=base, op=ALU.add),
             reads=[("bank", 5), "base"], writes=["posE"])
        P.op("dve", lambda: nc.vector.tensor_tensor(out=base, in0=banks[5][:, 96:128], in1=base, op=ALU.add),
             reads=[("bank", 5), "base"], writes=["base"])
        for kx, ohx, nm2 in ((0, OH1, "OH1"), (1, OH2, "OH2")):
            P.op("dve", lambda ohx=ohx: nc.vector.tensor_tensor(out=t32, in0=ohx.rearrange("p a b -> p (a b)"), in1=posE, op=ALU.mult),
                 reads=[nm2, "posE"], writes=["t32"])
            P.op("dve", lambda kx=kx: nc.vector.reduce_sum(out=rt[:, 10 + kx:11 + kx], in_=t32, axis=AX.X), reads=["t32"], writes=[R])
            P.op("dve", lambda ohx=ohx: nc.vector.tensor_tensor(out=t32, in0=ohx.rearrange("p a b -> p (a b)"), in1=iota32, op=ALU.mult),
                 reads=[nm2, "cf32"], writes=["t32"])
            P.op("dve", lambda kx=kx: nc.vector.reduce_sum(out=rt[:, 12 + kx:13 + kx], in_=t32, axis=AX.X), reads=["t32"], writes=[R])
            P.op("dve", lambda kx=kx: nc.vector.tensor_scalar(out=rt[:, 14 + kx:15 + kx], in0=rt[:, 10 + kx:11 + kx], scalar1=float(CAP),
                                                              scalar2=1.0e6, op0=ALU.is_ge, op1=ALU.mult), reads=[R], writes=[R])
            P.op("dve", lambda kx=kx: nc.vector.scalar_tensor_tensor(out=dstf[:, kx:kx + 1], in0=rt[:, 12 + kx:13 + kx], scalar=float(CAP),
                                                                      in1=rt[:, 10 + kx:11 + kx], op0=ALU.mult, op1=ALU.add),
                 reads=[R], writes=["dstf"])
            P.op("dve", lambda kx=kx: nc.vector.tensor_tensor(out=dstf[:, kx:kx + 1], in0=dstf[:, kx:kx + 1], in1=rt[:, 14 + kx:15 + kx], op=ALU.add),
                 reads=[R, "dstf"], writes=["dstf"])
        P.op("dve", lambda t=t: nc.vector.tensor_copy(out=destAll[:, t, :], in_=dstf), reads=["dstf"], writes=[("dest", t)])
        for kx in range(2):
            P.dma("pool", lambda t=t, kx=kx, xs=xs: nc.gpsimd.indirect_dma_start(
                out=x_pad[:, :], out_offset=bass.IndirectOffsetOnAxis(ap=destAll[:, t, kx:kx + 1], axis=0),
                in_=xfb[xs], in_offset=None, bounds_check=get_bc(), oob_is_err=False),
                reads=[("dest", t), ("xfb", xs)], writes=["x_pad"], slot="scat")


    for t in range(NT_RUN + 1):
        if t < NT_RUN:
            tail_a(t)
        if t >= 1 and stage != "tail":
            tail_b(t - 1)

    if stage == "tail":
        P.emit()
        return nc, P


    P.fence()
    A.reset()
    A3 = Arena(mixT[:].rearrange("p c t -> p (c t)"), KC * T)
    wv_g = w_e_gate.rearrange("e (c p) n -> e p c n", p=128)
    wv_u = w_e_up.rearrange("e (c p) n -> e p c n", p=128)
    wv_d = w_e_down.rearrange("e (c p) n -> e p c n", p=128)
    xp_v = x_pad.rearrange("(e j p) d -> e p j d", j=CAP // 128, p=128)
    NB = CAP // 128
    wgs = [A3.take([128, KC, 512], BF16) for i in range(2)]
    wus = [A3.take([128, KC, 512], BF16) for i in range(2)]
    wds = [A3.take([128, 4, D], BF16) for i in range(2)]
    xblk = [A.take([128, NB, D], BF16) for i in range(2)]
    XeT = [A.take([128, KC, CAP], BF16) for i in range(2)]
    hidT = A.take([128, 4, CAP], BF16)
    sil = [A.take([128, CAP], F32) for i in range(2)]
    oblk = [A.take([128, D], F32) for i in range(2)]
    NE = int(os.environ.get("MOE_NE", "32"))
    for e in range(NE):
        wsl = e % 2
        for hf_ in range(2):
            P.dma("pool", lambda e=e, wsl=wsl, hf_=hf_: nc.gpsimd.dma_start(out=wgs[wsl][:, :, hf_ * 256:(hf_ + 1) * 256],
                                                                            in_=wv_g[e, :, :, hf_ * 256:(hf_ + 1) * 256]),
                  writes=[("wgs", wsl)], slot=("wgs", wsl, hf_))
            P.dma("pool", lambda e=e, wsl=wsl, hf_=hf_: nc.gpsimd.dma_start(out=wus[wsl][:, :, hf_ * 256:(hf_ + 1) * 256],
                                                                            in_=wv_u[e, :, :, hf_ * 256:(hf_ + 1) * 256]),
                  writes=[("wus", wsl)], slot=("wus", wsl, hf_))
            P.dma("pool", lambda e=e, wsl=wsl, hf_=hf_: nc.gpsimd.dma_start(out=wds[wsl][:, :, hf_ * 512:(hf_ + 1) * 512],
                                                                            in_=wv_d[e, :, :, hf_ * 512:(hf_ + 1) * 512]),
                  writes=[("wds", wsl)], slot=("wds", wsl, hf_))
        P.dma("sp", lambda e=e, wsl=wsl: nc.sync.dma_start(out=xblk[wsl], in_=xp_v[e]), reads=["x_pad"], writes=[("xblk", wsl)],
              slot=("xblk", wsl))
        for j in range(NB):
            for c in range(KC):
                P.op("pe", lambda c=c, j=j, wsl=wsl: nc.tensor.transpose(out=bank_bf(4)[:, c * 128:(c + 1) * 128],
                                                                         in_=xblk[wsl][:, j, c * 128:(c + 1) * 128], identity=ident),
                     reads=[("xblk", wsl), "cbf"], writes=[("bank", 4)])
            if j % 2 == 0:
                P.op("act", lambda j=j, wsl=wsl: nc.scalar.copy(out=XeT[wsl][:, :, j * 128:(j + 1) * 128],
                                                                in_=bank_bf(4).rearrange("p (c n) -> p c n", c=KC)),
                     reads=[("bank", 4)], writes=[("XeT", wsl)])
            else:
                P.op("dve", lambda j=j, wsl=wsl: nc.vector.tensor_copy(out=XeT[wsl][:, :, j * 128:(j + 1) * 128],
                                                                       in_=bank_bf(4).rearrange("p (c n) -> p c n", c=KC)),
                     reads=[("bank", 4)], writes=[("XeT", wsl)])
        for hc in range(4):
            bg = 0 if hc % 2 == 0 else 2
            for wsrc, bk, wkey in ((wgs, bg, "wgs"), (wus, bg + 1, "wus")):
                for c in range(KC):
                    P.op("pe", lambda c=c, hc=hc, wsl=wsl, wsrc=wsrc, bk=bk: nc.tensor.matmul(
                        banks[bk], wsrc[wsl][:, c, hc * 128:(hc + 1) * 128], XeT[wsl][:, c, :], start=(c == 0), stop=(c == KC - 1)),
                        reads=[(wkey, wsl), ("XeT", wsl)], writes=[("bank", bk)])
            P.op("act", lambda hc=hc, bg=bg: nc.scalar.activation(out=sil[hc % 2], in_=banks[bg], func=AF.Silu),
                 reads=[("bank", bg)], writes=[("sil", hc % 2)])
            P.op("dve", lambda hc=hc, bg=bg: nc.vector.tensor_tensor(out=hidT[:, hc, :], in0=banks[bg + 1], in1=sil[hc % 2], op=ALU.mult),
                 reads=[("bank", bg + 1), ("sil", hc % 2)], writes=[("hidT", hc)])
        for j in range(NB):
            for half in range(2):
                for hc in range(4):
                    P.op("pe", lambda hc=hc, j=j, half=half, wsl=wsl: nc.tensor.matmul(
                        banks[6 + half], hidT[:, hc, j * 128:(j + 1) * 128], wds[wsl][:, hc, half * 512:(half + 1) * 512],
                        start=(hc == 0), stop=(hc == 3)),
                        reads=[("hidT", hc), ("wds", wsl)], writes=[("bank", 6 + half)])
            if j % 2 == 0:
                P.op("act", lambda j=j: nc.scalar.copy(out=oblk[j % 2], in_=bigv[3]), reads=[("bank", 6), ("bank", 7)], writes=[("oblk", j % 2)])
            else:
                P.op("dve", lambda j=j: nc.vector.tensor_copy(out=oblk[j % 2], in_=bigv[3]), reads=[("bank", 6), ("bank", 7)], writes=[("oblk", j % 2)])
            r0 = e * CAP + j * 128
            P.dma("sp", lambda j=j, r0=r0: nc.sync.dma_start(out=o_pad[r0:r0 + 128, :], in_=oblk[j % 2]),
                  reads=[("oblk", j % 2)], writes=["o_pad"], slot="opad")

    P.fence()
    A2.off = 0
    g1s = [A2.take([128, D], F32) for i in range(2)]
    g2s = [A2.take([128, D], F32) for i in range(2)]
    hhs = [A2.take([128, D], F32) for i in range(2)]
    fins = [A2.take([128, D], F32) for i in range(2)]
    for t in range(NT_RUN):
        s2 = t % 2
        for kx, gs, gname in ((0, g1s, "g1s"), (1, g2s, "g2s")):
            P.op("dve", lambda gs=gs, s2=s2: nc.vector.memset(gs[s2], 0.0), writes=[(gname, s2)])
            P.dma("pool", lambda gs=gs, s2=s2, t=t, kx=kx: nc.gpsimd.indirect_dma_start(
                out=gs[s2], out_offset=None, in_=o_pad[:, :], in_offset=bass.IndirectOffsetOnAxis(ap=destAll[:, t, kx:kx + 1], axis=0),
                bounds_check=get_bc(), oob_is_err=False),
                reads=["o_pad", ("dest", t)], writes=[(gname, s2)], slot=(gname, s2))
        P.dma("sp", lambda t=t, s2=s2: nc.sync.dma_start(out=hhs[s2], in_=out[t * 128:(t + 1) * 128, :]),
              reads=[("h2d", t)], writes=[("hhs", s2)], slot=("hhs", s2))
        P.op("dve", lambda t=t, s2=s2: nc.vector.scalar_tensor_tensor(out=fins[s2], in0=g1s[s2], scalar=gateAll[:, t, 0:1], in1=hhs[s2],
                                                                       op0=ALU.mult, op1=ALU.add),
             reads=[("g1s", s2), ("hhs", s2), ("gate", t)], writes=[("fins", s2)])
        P.op("dve", lambda t=t, s2=s2: nc.vector.scalar_tensor_tensor(out=fins[s2], in0=g2s[s2], scalar=gateAll[:, t, 1:2], in1=fins[s2],
                                                                       op0=ALU.mult, op1=ALU.add),
             reads=[("g2s", s2), ("fins", s2), ("gate", t)], writes=[("fins", s2)])
        P.dma("sp", lambda t=t, s2=s2: nc.sync.dma_start(out=out[t * 128:(t + 1) * 128, :], in_=fins[s2]),
              reads=[("fins", s2), ("hhs", s2)], writes=[("h2d", t)], slot=("fin", s2))
    P.emit()
    return nc, P


_CACHE = {}


def kernel(**inputs):
    nb = inputs["x"].shape[0]
    if "nc" not in _CACHE:
        _CACHE["nc"] = build("full")[0]
    nc = _CACHE["nc"]
    consts = make_consts()
    shared = {}
    for k, v in inputs.items():
        if k in ("x", "mem"):
            continue
        v = np.ascontiguousarray(np.asarray(v, dtype=np.float32))
        shared[k] = v[0] if v.ndim >= 3 else v
    in_maps = []
    for b in range(nb):
        m = dict(shared)
        m["x"] = np.ascontiguousarray(np.asarray(inputs["x"][b], dtype=np.float32))
        m["mem"] = np.ascontiguousarray(np.asarray(inputs["mem"][b], dtype=np.float32))
        m["consts"] = consts
        in_maps.append(m)
    res = run_bass_kernel_spmd(nc, in_maps, core_ids=list(range(nb)))
    return np.stack([np.asarray(r["out"], dtype=np.float32) for r in res.results], axis=0)


def make_consts():
    c = np.zeros((128, NCONST), np.float32)
    c[:, 0:128] = np.eye(128, dtype=np.float32)
    bo = np.zeros((128, 128), np.float32)
    bo[0:64, 0:64] = 1.0 / 64
    bo[64:128, 64:128] = 1.0 / 64
    c[:, 128:256] = bo
    j = np.arange(128)[:, None]
    i = np.arange(128)[None, :]
    same = (j // 64 == i // 64).astype(np.float32)
    mid = (i // 64) * 64 + 31
    c[:, 2304:2432] = -(1.0 / 16) * same * (j <= i)
    c[:, 2432:2560] = -(1.0 / 16) * same * ((j <= i).astype(np.float32) - (j <= mid).astype(np.float32))
    c[:, 2560:2688] = -(1.0 / 16) * same * (j > i)
    c[:, 2688:2816] = same * (j <= i)
    c[:, 2816:2944] = same * (j > i)
    c[:, 2944:3072] = (j < i)
    c[:, 3072:3200] = 1.0
    c[:, 3200:3232] = np.arange(32, dtype=np.float32)[None, :]
    c[0:64, 3232] = 1.0
    c[64:128, 3233] = 1.0
    c[0:64, 3234] = 0.125
    c[64:128, 3235] = 0.125
    return c
```

```python
import contextlib
import numpy as np
import concourse.bass as bass
import concourse.mybir as mybir
from concourse.bass_utils import run_bass_kernel_spmd

F32 = mybir.dt.float32
BF16 = mybir.dt.bfloat16
I32 = mybir.dt.int32
AF = mybir.ActivationFunctionType
ALU = mybir.AluOpType
AX = mybir.AxisListType

T = 4096
NT = 32
D = 1024
KC = 8
EPS = 1e-6
IN_W = 3088
LAM_INIT = 0.2
SEM_CHUNK = 16000
SAME_ENGINE_SYNC = True


class Prog:
    def __init__(self, nc):
        self.nc = nc
        self.es = contextlib.ExitStack()
        self.insts = []
        self.engs = {"pe": nc.tensor, "act": nc.scalar, "dve": nc.vector, "pool": nc.gpsimd, "sp": nc.sync}
        self.slot_sems = {}
        self.slot_vals = {}

    def sb(self, name, shape, dtype):
        return self.es.enter_context(self.nc.sbuf_tensor(name, list(shape), dtype))

    def ps(self, name, shape, dtype):
        return self.es.enter_context(self.nc.psum_tensor(name, list(shape), dtype))

    LOOPVARS = {'t', 'tt', 'g', 'h', 'p', 'hh', 'c', 'cs', 'rows', 'kt', 'qc', 'm', 'qs', 's', 'r', 'q0', 'bk', 'ba', 'bb', 'bv', 'bo',
                'b1', 'b2', 'o1', 'o2', 'tok0', 'ws', 'xs', 's2', 'i', 'j', 'a', 'ch', 'bu', 'snap', 'par', 'Sba', 'Sbb', 'els', 'dcol',
                'oc', 'lc', 'lt', 'o4', 'dst', 'src', 'ee', 'sc', 'nm', 'col0', 'c0', 'last', 'ps_', 'hf', 'e', 'blk', 'half', 'hc',
                'k', 'kc', 'mt', 'dc', 'eb', 'wsl', 'xb', 'hb'}

    def _chk(self, fn):
        bad = set(fn.__code__.co_freevars) & self.LOOPVARS
        assert not bad, ("late-bound loop variable in lambda", bad, fn.__code__.co_firstlineno)

    def op(self, eng, fn, reads=(), writes=()):
        self._chk(fn)
        self.insts.append(dict(kind="op", eng=eng, fn=fn, reads=tuple(reads), writes=tuple(writes)))

    def dma(self, eng, fn, reads=(), writes=(), slot=None, n=1):
        assert slot is not None
        self._chk(fn)
        self.insts.append(dict(kind="dma", eng=eng, fn=fn, reads=tuple(reads), writes=tuple(writes), slot=slot, n=n))

    def rename(self, old_keys, new_keys):
        self.insts.append(dict(kind="rename", old=tuple(old_keys), new=tuple(new_keys)))

    def fence(self):
        self.insts.append(dict(kind="fence"))

    def emit(self):
        nc = self.nc
        last_w = {}
        readers = {}
        eng_count = {e: 0 for e in self.engs}
        marked = {e: set() for e in self.engs}
        slot_val = {}
        fence_toks = []
        seen_since_fence = set()
        for ins in self.insts:
            if ins["kind"] == "fence":
                toks = list(fence_toks)
                for k, v in last_w.items():
                    if v is not None:
                        toks.append(v)
                for k, v in readers.items():
                    toks.extend(v)
                best = {}
                for tk in toks:
                    kk = (tk[0], tk[1])
                    if kk not in best or tk[2] > best[kk][2]:
                        best[kk] = tk
                fence_toks = list(best.values())
                seen_since_fence = set(last_w.keys()) | set(readers.keys())
                continue
            if ins["kind"] == "rename":
                toks = []
                for k in ins["old"]:
                    if last_w.get(k) is not None:
                        toks.append(last_w[k])
                    toks.extend(readers.get(k, []))
                    last_w.pop(k, None)
                    readers.pop(k, None)
                for k in ins["new"]:
                    readers.setdefault(k, []).extend(toks)
                continue
            e = ins["eng"]
            idx = eng_count[e]
            eng_count[e] += 1
            deps = set()
            for k in ins["reads"] + ins["writes"]:
                if k not in seen_since_fence:
                    seen_since_fence.add(k)
                    deps.update(fence_toks)
            for k in ins["reads"]:
                if last_w.get(k) is not None:
                    deps.add(last_w[k])
            for k in ins["writes"]:
                if last_w.get(k) is not None:
                    deps.add(last_w[k])
                deps.update(readers.get(k, []))
            if ins["kind"] == "dma":
                s = ins["slot"]
                slot_val[s] = slot_val.get(s, 0) + 16 * ins["n"]
                tok = ("d", s, slot_val[s])
            else:
                tok = ("e", e, idx)
            best = {}
            for tk in deps:
                kk = (tk[0], tk[1])
                if kk not in best or tk[2] > best[kk][2]:
                    best[kk] = tk
            deps = set(best.values())
            ins["idx"] = idx
            ins["tok"] = tok
            ins["deps"] = deps
            for dp in deps:
                if dp[0] == "e":
                    marked[dp[1]].add(dp[2])
            for k in ins["reads"]:
                readers.setdefault(k, []).append(tok)
            for k in ins["writes"]:
                last_w[k] = tok
                readers[k] = []
        for e in self.engs:
            if eng_count[e] > 0:
                marked[e].add(eng_count[e] - 1)
        self._last_idx = {e: eng_count[e] - 1 for e in self.engs if eng_count[e] > 0}
        rank = {}
        for e in self.engs:
            for r, idx in enumerate(sorted(marked[e])):
                rank[(e, idx)] = r
        nchunks = {e: (len(marked[e]) + SEM_CHUNK - 1) // SEM_CHUNK for e in self.engs}
        esems = {e: [self.es.enter_context(nc.semaphore(f"q_{e}_{i}")) for i in range(max(1, nchunks[e]))] for e in self.engs}
        dsems = {}
        for s in slot_val:
            dsems[s] = self.es.enter_context(nc.semaphore(f"d_{len(dsems)}"))
        waited_e = {e: {b: -1 for b in self.engs} for e in self.engs}
        waited_d = {e: {} for e in self.engs}
        nwaits = 0
        ins_kind_last = {}
        for ins in self.insts:
            if ins["kind"] in ("op", "dma"):
                ins_kind_last[ins["eng"]] = ins["kind"]
        trace = {e: [] for e in self.engs}
        for ins in self.insts:
            if ins["kind"] in ("rename", "fence"):
                continue
            e = ins["eng"]
            h = self.engs[e]
            tw = []
            trace[e].append((tw, ins))
            for dp in sorted(ins["deps"], key=str):
                if dp[0] == "e":
                    b, j = dp[1], dp[2]
                    if b == e and (e == "pe" or not SAME_ENGINE_SYNC):
                        continue
                    r = rank[(b, j)]
                    if waited_e[e][b] >= r:
                        continue
                    waited_e[e][b] = r
                    h.wait_ge(esems[b][r // SEM_CHUNK], (r % SEM_CHUNK) + 1)
                    tw.append(("e", b, r + 1))
                    nwaits += 1
                else:
                    s, v = dp[1], dp[2]
                    if waited_d[e].get(s, 0) >= v:
                        continue
                    waited_d[e][s] = v
                    h.wait_ge(dsems[s], v)
                    tw.append(("d", s, v))
                    nwaits += 1
            bi = ins["fn"]()
            if ins["kind"] == "dma":
                bi.then_inc(dsems[ins["slot"]], 16)
            elif (e, ins["idx"]) in rank:
                r = rank[(e, ins["idx"])]
                bi.then_inc(esems[e][r // SEM_CHUNK], 1)
        self.final_slots = {s: (dsems[s], v) for s, v in slot_val.items()}
        for e, li in self._last_idx.items():
            if e == "sp" or ins_kind_last.get(e) == "dma":
                continue
            r = rank[(e, li)]
            nc.sync.wait_ge(esems[e][r // SEM_CHUNK], (r % SEM_CHUNK) + 1)
        for s_, v in slot_val.items():
            nc.sync.wait_ge(dsems[s_], v)
        semv = {}
        pos = {e: 0 for e in self.engs}
        progress = True
        while progress:
            progress = False
            for e in self.engs:
                while pos[e] < len(trace[e]):
                    tw, ins = trace[e][pos[e]]
                    if all(semv.get((w[0], w[1]), 0) >= w[2] for w in tw):
                        if ins["kind"] == "dma":
                            semv[("d", ins["slot"])] = semv.get(("d", ins["slot"]), 0) + 16
                        elif (e, ins["idx"]) in rank:
                            semv[("e", e)] = semv.get(("e", e), 0) + 1
                        pos[e] += 1
                        progress = True
                    else:
                        break
        stuck = {e: (pos[e], len(trace[e])) for e in self.engs if pos[e] < len(trace[e])}
        if stuck:
            for e in stuck:
                tw, ins = trace[e][pos[e]]
                print("DEADLOCK", e, pos[e], tw, ins["reads"], ins["writes"], {k: v for k, v in semv.items()})
            raise RuntimeError("deadlock in generated program")
        self.stats = dict(n_inst=len(self.insts), n_waits=nwaits, marked={e: len(marked[e]) for e in self.engs})


class Arena:
    def __init__(self, buf, nelem):
        self.buf = buf
        self.n = nelem
        self.off = 0

    def reset(self):
        self.off = 0

    def take(self, shape, dtype):
        free = 1
        for d_ in shape[1:]:
            free *= d_
        ne = free * (2 if dtype == F32 else 1)
        ne = (ne + 15) // 16 * 16
        assert self.off + ne <= self.n, ("arena overflow", self.off, ne, self.n)
        ap = self.buf[0:shape[0], self.off:self.off + (free * (2 if dtype == F32 else 1))]
        self.off += ne
        if dtype == F32:
            ap = ap.bitcast(F32)
        if len(shape) == 3:
            ap = ap.rearrange("p (a b) -> p a b", a=shape[1])
        elif len(shape) == 4:
            ap = ap.rearrange("p (a b c) -> p a b c", a=shape[1], b=shape[2])
        return ap


NCONST = 3236
CAP = 512
NSLOT = 32 * CAP


def build(stage="full"):
    nc = bass.Bass("TRN2", target_bir_lowering=False)
    P = Prog(nc)

    def din(name, shape):
        return nc.dram_tensor(name, list(shape), F32, kind="ExternalInput").ap()

    x = din("x", [T, D])
    mem = din("mem", [256, D])
    norm_mix = din("norm_mix", [1, D])
    w_in = din("w_in", [D, IN_W])
    da_q_norm = din("da_q_norm", [1, 64])
    da_k_norm = din("da_k_norm", [1, 64])
    lq1 = din("lambda_q1", [1, 64])
    lk1 = din("lambda_k1", [1, 64])
    lq2 = din("lambda_q2", [1, 64])
    lk2 = din("lambda_k2", [1, 64])
    da_out_norm = din("da_out_norm", [1, 128])
    gla_gate_w = din("gla_gate_w", [16, 256])
    gla_gate_b = din("gla_gate_b", [1, 256])
    gla_out_norm = din("gla_out_norm", [1, 128])
    w_o = din("w_o", [D, D])
    norm_cross = din("norm_cross", [1, D])
    norm_mem = din("norm_mem", [1, D])
    w_cq = din("w_cq", [D, D])
    w_ckv = din("w_ckv", [D, 2 * D])
    cross_q_norm = din("cross_q_norm", [1, 256])
    cross_k_norm = din("cross_k_norm", [1, 256])
    w_co = din("w_co", [D, D])
    norm_ffn = din("norm_ffn", [1, D])
    w_group = din("w_group", [D, 4])
    b_group = din("b_group", [1, 4])
    w_expert = din("w_expert", [D, 32])
    b_expert = din("b_expert", [1, 32])
    w_e_gate = din("w_e_gate", [32, D, 512])
    w_e_up = din("w_e_up", [32, D, 512])
    w_e_down = din("w_e_down", [32, 512, D])
    consts = din("consts", [128, NCONST])
    out = nc.dram_tensor("out", [T, D], F32, kind="ExternalOutput").ap()
    x_pad = nc.dram_tensor("x_pad", [NSLOT, D], BF16, kind="Internal").ap()
    o_pad = nc.dram_tensor("o_pad", [NSLOT, D], F32, kind="Internal").ap()
    if stage in ("da", "gla"):
        dbg = nc.dram_tensor("dbg", [128, 4, T], BF16, kind="ExternalOutput").ap()

    UT = P.sb("UT", [128, KC, T], BF16)
    mixT = P.sb("mixT", [128, KC, T], BF16)
    ARENA_N = 36 * 1024
    arena_t = P.sb("arena", [128, ARENA_N], BF16)
    A = Arena(arena_t, ARENA_N)
    cbf = P.sb("cbf", [128, 256 + 5 * 128], BF16)
    cf32 = P.sb("cf32", [128, 292], F32)
    TriS = cbf[:, 640:768]
    ones_bf = cbf[:, 768:896]
    iota32 = cf32[:, 256:288]
    ident = cbf[:, 0:128]
    bones = cbf[:, 128:256]
    Trin = cbf[:, 256:384]
    TriCn = cbf[:, 384:512]
    TriEn = cbf[:, 512:640]
    maskP = cf32[:, 0:128]
    maskF = cf32[:, 128:256]
    P.dma("pool", lambda: nc.gpsimd.dma_start(out=cbf[:, 0:256], in_=consts[:, 0:256]), writes=["cbf"], slot="c0")
    P.dma("pool", lambda: nc.gpsimd.dma_start(out=cbf[:, 256:640], in_=consts[:, 2304:2304 + 384]), writes=["cbf"], slot="c1")
    P.dma("pool", lambda: nc.gpsimd.dma_start(out=cbf[:, 640:896], in_=consts[:, 2944:3200]), writes=["cbf"], slot="c1b")
    P.dma("sp", lambda: nc.sync.dma_start(out=cf32[:, 0:256], in_=consts[:, 2688:2688 + 256]), writes=["cf32"], slot="c2")
    P.dma("sp", lambda: nc.sync.dma_start(out=cf32[:, 256:292], in_=consts[:, 3200:3236]), writes=["cf32"], slot="c2b")

    gon = P.sb("gon", [128, 128], F32)
    P.dma("sp", lambda: nc.sync.dma_start(out=gon[:], in_=da_out_norm.partition_broadcast(128)), writes=["gon"], slot="c4")
    P.op("dve", lambda: nc.vector.tensor_scalar(out=gon[:], in0=gon[:], scalar1=1.0 - LAM_INIT, scalar2=None, op0=ALU.mult),
         reads=["gon"], writes=["gon"])
    ggl = P.sb("ggl", [128, 128], F32)
    P.dma("sp", lambda: nc.sync.dma_start(out=ggl[:], in_=gla_out_norm.partition_broadcast(128)), writes=["ggl"], slot="c4b")
    gq = P.sb("gq", [128, 1], F32)
    gk = P.sb("gk", [128, 1], F32)
    for hh in range(2):
        P.dma("sp", lambda hh=hh: nc.sync.dma_start(out=gq[hh * 64:(hh + 1) * 64, :], in_=da_q_norm.rearrange("o d -> d o")),
              writes=["gq"], slot="c5")
        P.dma("sp", lambda hh=hh: nc.sync.dma_start(out=gk[hh * 64:(hh + 1) * 64, :], in_=da_k_norm.rearrange("o d -> d o")),
              writes=["gk"], slot="c6")
    P.op("dve", lambda: nc.vector.tensor_scalar(out=gq[:], in0=gq[:], scalar1=0.125, scalar2=None, op0=ALU.mult),
         reads=["gq"], writes=["gq"])
    lam4 = P.sb("lam4", [128, 4, 64], F32)
    for i, a in enumerate((lq1, lk1, lq2, lk2)):
        P.dma("sp", lambda i=i, a=a: nc.sync.dma_start(out=lam4[:, i, :], in_=a.partition_broadcast(128)),
              writes=["lam4"], slot="c7")
    lamw = P.sb("lamw", [128, 2, 64], F32)
    lams = P.sb("lams", [128, 2], F32)
    nlam = P.sb("nlam", [128, 1], F32)
    P.op("dve", lambda: nc.vector.tensor_tensor(out=lamw[:], in0=lam4[:, 0:4:2, :], in1=lam4[:, 1:4:2, :], op=ALU.mult),
         reads=["lam4"], writes=["lamw"])
    P.op("dve", lambda: nc.vector.reduce_sum(out=lams[:], in_=lamw[:], axis=AX.X), reads=["lamw"], writes=["lams"])
    P.op("act", lambda: nc.scalar.activation(out=lams[:], in_=lams[:], func=AF.Exp), reads=["lams"], writes=["lams"])
    P.op("dve", lambda: nc.vector.scalar_tensor_tensor(out=nlam[:], in0=lams[:, 1:2], scalar=-LAM_INIT, in1=lams[:, 0:1],
                                                        op0=ALU.add, op1=ALU.subtract),
         reads=["lams"], writes=["nlam"])

    bigs = [P.ps(f"big{j}", [128, 1024], F32) for j in range(4)]
    bigv = [b[:] for b in bigs]
    banks = []
    for j in range(4):
        banks.append(bigs[j][:, 0:512])
        banks.append(bigs[j][:, 512:1024])

    def bank_bf(i):
        return banks[i].bitcast(BF16)

    gmix = A.take([128, D], F32)
    P.dma("sp", lambda: nc.sync.dma_start(out=gmix, in_=norm_mix.partition_broadcast(128)), writes=["gmix"], slot="c3")
    xts = [A.take([128, D], F32) for i in range(3)]
    ubs = [A.take([128, D], BF16) for i in range(2)]
    junk = A.take([128, D], BF16)
    ssq = [A.take([128, 1], F32) for i in range(2)]
    rstd = [A.take([128, 1], F32) for i in range(2)]

    for t in range(NT):
        xs = t % 3
        s2 = t % 2
        P.dma("sp", lambda t=t, xs=xs: nc.sync.dma_start(out=xts[xs], in_=x[t * 128:(t + 1) * 128, :]),
              writes=[("xt", xs)], slot=("xt", xs))
        P.op("act", lambda xs=xs, s2=s2: nc.scalar.activation(out=junk, in_=xts[xs], func=AF.Square, accum_out=ssq[s2]),
             reads=[("xt", xs)], writes=["junk", ("ssq", s2)])
        P.op("act", lambda s2=s2: nc.scalar.activation(out=rstd[s2], in_=ssq[s2], func=AF.Ln, bias=EPS, scale=1.0 / D),
             reads=[("ssq", s2)], writes=[("rstd", s2)])
        P.op("act", lambda s2=s2: nc.scalar.activation(out=rstd[s2], in_=rstd[s2], func=AF.Exp, scale=-0.5),
             reads=[("rstd", s2)], writes=[("rstd", s2)])
        P.op("dve", lambda xs=xs, s2=s2: nc.vector.scalar_tensor_tensor(out=ubs[s2], in0=xts[xs], scalar=rstd[s2][:, 0:1],
                                                                         in1=gmix, op0=ALU.mult, op1=ALU.mult),
             reads=[("xt", xs), ("rstd", s2), "gmix"], writes=[("ub", s2)])
        bk = t % 2
        for c in range(KC):
            P.op("pe", lambda c=c, s2=s2, bk=bk: nc.tensor.transpose(out=bank_bf(bk)[:, c * 128:(c + 1) * 128],
                                                                      in_=ubs[s2][:, c * 128:(c + 1) * 128], identity=ident),
                 reads=[("ub", s2), "cbf"], writes=[("bank", bk)])
        if t % 2 == 0:
            P.op("act", lambda t=t, bk=bk: nc.scalar.copy(out=UT[:, :, t * 128:(t + 1) * 128],
                                                          in_=bank_bf(bk).rearrange("p (c n) -> p c n", c=KC)),
                 reads=[("bank", bk)], writes=[("UT", t)])
        else:
            P.op("dve", lambda t=t, bk=bk: nc.vector.tensor_copy(out=UT[:, :, t * 128:(t + 1) * 128],
                                                                  in_=bank_bf(bk).rearrange("p (c n) -> p c n", c=KC)),
                 reads=[("bank", bk)], writes=[("UT", t)])

    w_in_v = w_in.rearrange("(c p) n -> p c n", p=128)

    P.fence()
    A.reset()
    wda = [A.take([128, KC, 384], BF16) for i in range(2)]
    QT = A.take([128, T], BF16)
    KT = A.take([128, T], BF16)
    V = A.take([128, NT, 132], BF16)
    sqb = [A.take([128, 512], BF16) for i in range(2)]
    rsb = [A.take([128, 512], F32) for i in range(2)]
    pts = [[A.take([128, 512], BF16) for i in range(3)] for m in range(2)]
    rec = A.take([128, 2, 2], F32)
    t1 = A.take([128, 2, 128], F32)
    dd = A.take([128, 2, 128], F32)
    dsq = A.take([128, 2], F32)
    drs = A.take([128, 2], F32)
    ob = A.take([128, 2, 128], BF16)
    junk2 = A.take([128, 128], BF16)
    P.op("pool", lambda: nc.gpsimd.memset(V, 1.0), writes=["Vones"])

    n_heads = 4 if stage != "gla" else 0
    for h in range(n_heads):
        ws = h % 2
        for j, col0 in enumerate((h * 128, 512 + h * 128, 1024 + h * 128)):
            P.dma("pool", lambda ws=ws, j=j, col0=col0: nc.gpsimd.dma_start(out=wda[ws][:, :, j * 128:(j + 1) * 128],
                                                                             in_=w_in_v[:, :, col0:col0 + 128]),
                  writes=[("wda", ws)], slot=("wda", ws, j))
        for tc in range(8):
            for qk, (dst, gcol, dname) in enumerate(((QT, gq, "QT"), (KT, gk, "KT"))):
                ba = (2 * tc + qk) % 2
                bb = 2 + ba
                for c in range(KC):
                    P.op("pe", lambda c=c, ba=ba, ws=ws, qk=qk, tc=tc: nc.tensor.matmul(
                        banks[ba][:], wda[ws][:, c, qk * 128:(qk + 1) * 128], UT[:, c, tc * 512:(tc + 1) * 512],
                        start=(c == 0), stop=(c == KC - 1)),
                        reads=[("wda", ws)] + [("UT", 4 * tc + i) for i in range(4)], writes=[("bank", ba)])
                P.op("act", lambda ba=ba: nc.scalar.activation(out=sqb[ba], in_=banks[ba][:], func=AF.Square),
                     reads=[("bank", ba)], writes=[("sqb", ba)])
                P.op("pe", lambda ba=ba, bb=bb: nc.tensor.matmul(banks[bb][:], bones, sqb[ba], start=True, stop=True),
                     reads=["cbf", ("sqb", ba)], writes=[("bank", bb)])
                P.op("act", lambda ba=ba, bb=bb: nc.scalar.activation(out=rsb[ba], in_=banks[bb][:], func=AF.Ln, bias=EPS),
                     reads=[("bank", bb)], writes=[("rsb", ba)])
                P.op("act", lambda ba=ba: nc.scalar.activation(out=rsb[ba], in_=rsb[ba], func=AF.Exp, scale=-0.5),
                     reads=[("rsb", ba)], writes=[("rsb", ba)])
                P.op("dve", lambda ba=ba, dst=dst, gcol=gcol, tc=tc: nc.vector.scalar_tensor_tensor(
                    out=dst[:, tc * 512:(tc + 1) * 512], in0=banks[ba][:], scalar=gcol[:, 0:1], in1=rsb[ba],
                    op0=ALU.mult, op1=ALU.mult),
                    reads=[("bank", ba), ("rsb", ba), "gq" if qk == 0 else "gk"],
                    writes=[(dname, tc)])
        for g4 in range(8):
            bv = 4 + (g4 % 2)
            for i in range(4):
                t = g4 * 4 + i
                for c in range(KC):
                    P.op("pe", lambda c=c, t=t, i=i, bv=bv, ws=ws: nc.tensor.matmul(
                        banks[bv][:, i * 128:(i + 1) * 128], UT[:, c, t * 128:(t + 1) * 128], wda[ws][:, c, 256:384],
                        start=(c == 0), stop=(c == KC - 1)),
                        reads=[("wda", ws), ("UT", t)], writes=[("bank", bv)])
            P.op("act", lambda g4=g4, bv=bv: nc.scalar.copy(out=V[:, g4 * 4:(g4 + 1) * 4, 0:128],
                                                            in_=banks[bv][:].rearrange("p (a b) -> p a b", a=4)),
                 reads=[("bank", bv), "Vones"], writes=[("V", g4)])

        for qc in range(8):
            nkt = 4 * qc + 4

            def emit_S(kt, qc=qc):
                s = kt % 2
                r = kt - 4 * qc
                q0 = 128 * r if r > 0 else 0
                for m in range(2):
                    bk = 2 * s + m
                    P.op("pe", lambda m=m, bk=bk, kt=kt, q0=q0, qc=qc: nc.tensor.matmul(
                        banks[bk][:, q0:512], KT[m * 64:(m + 1) * 64, kt * 128:(kt + 1) * 128],
                        QT[m * 64:(m + 1) * 64, qc * 512 + q0:(qc + 1) * 512], start=True, stop=True),
                        reads=[("KT", kt // 4), ("QT", qc)], writes=[("bank", bk)])

            emit_S(0)
            for kt in range(nkt):
                s = kt % 2
                ps_ = kt % 3
                r = kt - 4 * qc
                q0 = 128 * r if r > 0 else 0
                for m in range(2):
                    bk = 2 * s + m
                    P.op("act", lambda m=m, bk=bk, ps_=ps_, q0=q0: nc.scalar.activation(
                        out=pts[m][ps_][:, q0:512], in_=banks[bk][:, q0:512], func=AF.Exp),
                        reads=[("bank", bk)], writes=[("pt", m, ps_)])
                    if r >= 0:
                        P.op("pool", lambda m=m, ps_=ps_, q0=q0, r=r: nc.gpsimd.memset(
                            pts[m][ps_][64:128, 128 * r:128 * r + 64], 0.0),
                            writes=[("pt", m, ps_)])
                if kt + 1 < nkt:
                    emit_S(kt + 1)
                qs0 = r if r > 0 else 0
                for qs in range(qs0, 4):
                    last = 4 * qc + qs
                    for m in range(2):
                        bo = 4 + 2 * m + qs // 2
                        P.op("pe", lambda m=m, bo=bo, qs=qs, kt=kt, ps_=ps_, last=last: nc.tensor.matmul(
                            banks[bo][:, (qs % 2) * 129:(qs % 2) * 129 + 129], pts[m][ps_][:, qs * 128:(qs + 1) * 128],
                            V[:, kt, 0:129], start=(kt == 0 and qs % 2 == 0), stop=(kt == last), skip_group_check=True),
                            reads=[("pt", m, ps_), ("V", kt // 4), "Vones"], writes=[("bank", bo)])
                for hf in range(2):
                    if kt == 4 * qc + 2 * hf + 1:
                        b1 = 4 + hf
                        b2 = 6 + hf
                        o1 = banks[b1][:, 0:258].rearrange("p (a b) -> p a b", a=2)
                        o2 = banks[b2][:, 0:258].rearrange("p (a b) -> p a b", a=2)
                        tok0 = qc * 512 + hf * 256
                        P.op("dve", lambda o1=o1: nc.vector.reciprocal(out=rec[:, 0, :], in_=o1[:, :, 128]),
                             reads=[("bank", b1)], writes=["rec0"])
                        P.op("dve", lambda o2=o2: nc.vector.reciprocal(out=rec[:, 1, :], in_=o2[:, :, 128]),
                             reads=[("bank", b2)], writes=["rec1"])
                        P.op("dve", lambda: nc.vector.tensor_scalar(out=rec[:, 1, :], in0=rec[:, 1, :], scalar1=nlam[:, 0:1],
                                                                    scalar2=None, op0=ALU.mult),
                             reads=["rec1", "nlam"], writes=["rec1"])
                        P.op("dve", lambda o1=o1: nc.vector.tensor_tensor(
                            out=t1, in0=o1[:, :, 0:128], in1=rec[:, 0, :].unsqueeze(2).to_broadcast([128, 2, 128]), op=ALU.mult),
                            reads=[("bank", b1), "rec0"], writes=["t1"])
                        P.op("dve", lambda o2=o2: nc.vector.tensor_tensor(
                            out=dd, in0=o2[:, :, 0:128], in1=rec[:, 1, :].unsqueeze(2).to_broadcast([128, 2, 128]), op=ALU.mult),
                            reads=[("bank", b2), "rec1"], writes=["dd"])
                        P.op("dve", lambda: nc.vector.tensor_tensor(out=dd, in0=dd, in1=t1, op=ALU.add),
                             reads=["dd", "t1"], writes=["dd"])
                        for a in range(2):
                            P.op("act", lambda a=a: nc.scalar.activation(out=junk2, in_=dd[:, a, :], func=AF.Square,
                                                                          accum_out=dsq[:, a:a + 1]),
                                 reads=["dd"], writes=["junk2", ("dsq", a)])
                        P.op("act", lambda: nc.scalar.activation(out=drs, in_=dsq, func=AF.Ln, bias=EPS, scale=1.0 / 128),
                             reads=[("dsq", 0), ("dsq", 1)], writes=["drs"])
                        P.op("act", lambda: nc.scalar.activation(out=drs, in_=drs, func=AF.Exp, scale=-0.5),
                             reads=["drs"], writes=["drs"])
                        for a in range(2):
                            P.op("dve", lambda a=a: nc.vector.scalar_tensor_tensor(
                                out=ob[:, a, :], in0=dd[:, a, :], scalar=drs[:, a:a + 1], in1=gon[:], op0=ALU.mult, op1=ALU.mult),
                                reads=["dd", "drs", "gon"], writes=[("ob", a)])
                        for a in range(2):
                            P.op("pe", lambda a=a, b1=b1: nc.tensor.transpose(out=bank_bf(b1)[:, a * 128:(a + 1) * 128],
                                                                              in_=ob[:, a, :], identity=ident),
                                 reads=[("ob", a), "cbf"], writes=[("bank", b1)])
                        P.op("act", lambda b1=b1, tok0=tok0, h=h: nc.scalar.copy(out=mixT[:, h, tok0:tok0 + 256],
                                                                                  in_=bank_bf(b1)[:, 0:256]),
                             reads=[("bank", b1)], writes=[("mixT", h, tok0 // 256)])

    if stage == "da":
        for h in range(n_heads):
            P.dma("sp", lambda h=h: nc.sync.dma_start(out=dbg[:, h, :], in_=mixT[:, h, :]),
                  reads=[("mixT", h, i) for i in range(16)], slot="out")
        P.emit()
        sem, v = P.final_slots["out"]
        nc.sync.wait_ge(sem, v)
        return nc, P

    P.fence()
    A.reset()
    wg = A.take([128, KC, 1552], BF16)
    for j in range(4):
        c0 = 1536 + j * 388
        P.dma("pool", lambda j=j, c0=c0: nc.gpsimd.dma_start(out=wg[:, :, j * 388:(j + 1) * 388], in_=w_in_v[:, :, c0:c0 + 388]),
              writes=["wg"], slot=("wg", j))
    gwa = A.take([32, 256], BF16)
    P.dma("pool", lambda: nc.gpsimd.dma_start(out=gwa[0:16, :], in_=gla_gate_w), writes=["gwa"], slot="gwa0")
    P.dma("pool", lambda: nc.gpsimd.dma_start(out=gwa[16:17, :], in_=gla_gate_b), writes=["gwa"], slot="gwa1")
    grT = A.take([32, 256], BF16)
    P.op("pool", lambda: nc.gpsimd.memset(grT, 1.0), writes=["grT1"])
    spe = A.take([128, 256], F32)
    spb = A.take([128, 256], BF16)
    EP = A.take([128, 2, 256], F32)
    EN = A.take([128, 2, 256], F32)
    ELa = A.take([128, 2, 256], F32)
    ELb = A.take([128, 2, 256], F32)
    P.op("pool", lambda: nc.gpsimd.memset(ELa, 0.0), writes=["ELa0"])
    P.op("pool", lambda: nc.gpsimd.memset(ELb, 0.0), writes=["ELb0"])
    QP = A.take([128, 2, 256], BF16)
    QN = A.take([128, 2, 256], BF16)
    KNh = [A.take([128, 2, 256], BF16) for i in range(2)]
    KPh = [A.take([128, 2, 256], BF16) for i in range(2)]
    QLah = [A.take([128, 2, 256], BF16) for i in range(2)]
    QLbh = [A.take([128, 2, 256], BF16) for i in range(2)]
    EE = [A.take([128, 256], F32) for i in range(2)]
    KE = [[A.take([128, 256], BF16) for ch in range(2)] for i in range(2)]
    Vg = [A.take([128, 512], BF16) for i in range(2)]
    sg = [A.take([128, 4, 128], F32) for i in range(2)]
    sge = A.take([128, 512], F32)
    at1 = A.take([128, 4, 128], F32)
    at2 = A.take([128, 4, 128], F32)
    ATb = A.take([128, 4, 128], BF16)
    Sst = [A.take([128, 128], F32) for p in range(2)]
    Sba2 = [[A.take([128, 128], BF16) for p in range(2)] for par in range(2)]
    Sbb2 = [[A.take([128, 128], BF16) for p in range(2)] for par in range(2)]
    osq = A.take([128, 4, 128], F32)
    oss = A.take([128, 4], F32)
    ors = A.take([128, 4], F32)
    otm = A.take([128, 4, 128], F32)
    ogb = A.take([128, 4, 128], BF16)
    for p in range(2):
        P.op("pool", lambda p=p: nc.gpsimd.memset(Sst[p], 0.0), writes=[("S", p)])
        P.op("pool", lambda p=p: nc.gpsimd.memset(Sba2[0][p], 0.0), writes=[("Sba", 0, p)])

    import os
    if stage == "gla":
        P.op("pool", lambda: nc.gpsimd.memset(mixT[:, 4:8, :], 0.0), writes=[("mixTg", i) for i in range(NT)])
    CUT = int(os.environ.get('GLA_CUT', '99'))
    NG = int(os.environ.get('GLA_NG', '16'))

    class _Cut(Exception):
        pass

    CUTT = int(os.environ.get('GLA_CUTT', '0'))
    cur = {"t": 0}

    def cut(k):
        if CUT == k and cur["t"] == CUTT:
            raise _Cut()

    def gla_all():
      for g in range(NG):
          P.op("pe", lambda: nc.tensor.matmul(banks[0][:, 0:128], ident, ident, start=True, stop=True, skip_group_check=True),
               reads=["cbf"], writes=[("bank", 0)])
          for c in range(KC):
              P.op("pe", lambda c=c, g=g: nc.tensor.matmul(banks[0][0:16, 0:256], wg[:, c, 1536:1552], UT[:, c, g * 256:(g + 1) * 256],
                                                            start=(c == 0), stop=(c == KC - 1)),
                   reads=["wg", ("UT", 2 * g), ("UT", 2 * g + 1)], writes=[("bank", 0)])
          P.op("act", lambda: nc.scalar.copy(out=grT[0:16, :], in_=banks[0][0:16, 0:256]),
               reads=[("bank", 0), "grT1"], writes=["grT"])
          for tt in range(2):
              t = 2 * g + tt
              cur["t"] = t
              cs = slice(tt * 128, (tt + 1) * 128)
              cut(1)
              P.op("pe", lambda cs=cs: nc.tensor.matmul(banks[0][:, 256:512], grT[0:17, cs], gwa[0:17, :], start=True, stop=True),
                   reads=["grT", "gwa"], writes=[("bank", 0)])
              P.op("act", lambda: nc.scalar.activation(out=spe, in_=banks[0][:, 256:512], func=AF.Exp, scale=-1.0),
                   reads=[("bank", 0)], writes=["spe"])
              P.op("act", lambda: nc.scalar.activation(out=spb, in_=spe, func=AF.Ln, bias=1.0),
                   reads=["spe"], writes=["spb"])
              cut(2)
              for p in range(2):
                  P.op("pe", lambda p=p: nc.tensor.matmul(banks[1][:, p * 128:(p + 1) * 128], spb[:, p * 128:(p + 1) * 128], TriCn,
                                                           start=True, stop=True),
                       reads=["spb", "cbf"], writes=[("bank", 1)])
              for p in range(2):
                  P.op("pe", lambda p=p: nc.tensor.matmul(banks[1][:, 256 + p * 128:256 + (p + 1) * 128],
                                                           spb[:, p * 128:(p + 1) * 128], Trin, start=True, stop=True),
                       reads=["spb", "cbf"], writes=[("bank", 1)])
              P.op("pe", lambda: nc.tensor.matmul(banks[2][:, 0:256], TriEn, spb, start=True, stop=True),
                   reads=["spb", "cbf"], writes=[("bank", 2)])
              lc = banks[1][:, 0:256].rearrange("p (a b) -> p a b", a=2)
              lt = banks[1][:, 256:512].rearrange("p (a b) -> p a b", a=2)
              P.op("act", lambda lc=lc, cs=cs: nc.scalar.activation(out=EP[:, :, cs], in_=lc, func=AF.Exp),
                   reads=[("bank", 1)], writes=["EP"])
              P.op("act", lambda lc=lc, cs=cs: nc.scalar.activation(out=EN[:, :, cs], in_=lc, func=AF.Exp, scale=-1.0),
                   reads=[("bank", 1)], writes=["EN"])
              P.op("act", lambda lt=lt, tt=tt: nc.scalar.activation(out=ELa[:, :, tt * 128:tt * 128 + 64], in_=lt[:, :, 0:64], func=AF.Exp),
                   reads=[("bank", 1), "ELa0"], writes=["ELa"])
              P.op("act", lambda lt=lt, tt=tt: nc.scalar.activation(out=ELb[:, :, tt * 128 + 64:tt * 128 + 128], in_=lt[:, :, 64:128], func=AF.Exp),
                   reads=[("bank", 1), "ELb0"], writes=["ELb"])
              P.op("act", lambda tt=tt: nc.scalar.activation(out=EE[tt], in_=banks[2][:, 0:256], func=AF.Exp),
                   reads=[("bank", 2)], writes=[("EE", tt)])
              cut(3)
              for c in range(KC):
                  P.op("pe", lambda c=c, t=t: nc.tensor.matmul(banks[2][:, 256:512], UT[:, c, t * 128:(t + 1) * 128], wg[:, c, 256:512],
                                                                start=(c == 0), stop=(c == KC - 1), skip_group_check=True),
                       reads=["wg", ("UT", t)], writes=[("bank", 2)])
              for c in range(KC):
                  P.op("pe", lambda c=c, t=t: nc.tensor.matmul(banks[5][:], UT[:, c, t * 128:(t + 1) * 128], wg[:, c, 512:1024],
                                                                start=(c == 0), stop=(c == KC - 1)),
                       reads=["wg", ("UT", t)], writes=[("bank", 5)])
              for c in range(KC):
                  P.op("pe", lambda c=c, t=t: nc.tensor.matmul(banks[6][:], UT[:, c, t * 128:(t + 1) * 128], wg[:, c, 1024:1536],
                                                                start=(c == 0), stop=(c == KC - 1)),
                       reads=["wg", ("UT", t)], writes=[("bank", 6)])
              for ch in range(2):
                  P.op("dve", lambda tt=tt, ch=ch: nc.vector.scalar_tensor_tensor(out=KE[tt][ch], in0=banks[2][:, 256:512],
                                                                                  scalar=cf32[:, 288 + ch:289 + ch], in1=EE[tt],
                                                                                  op0=ALU.mult, op1=ALU.mult),
                       reads=[("bank", 2), ("EE", tt), "cf32"], writes=[("KE", tt)])
              P.op("act", lambda tt=tt: nc.scalar.copy(out=Vg[tt], in_=banks[5][:]), reads=[("bank", 5)], writes=[("Vg", tt)])
              cut(4)
              P.op("act", lambda: nc.scalar.activation(out=sge, in_=banks[6][:], func=AF.Exp, scale=-1.0),
                   reads=[("bank", 6)], writes=["sge"])
              P.op("dve", lambda: nc.vector.tensor_scalar(out=sge, in0=sge, scalar1=1.0, scalar2=None, op0=ALU.add),
                   reads=["sge"], writes=["sge"])
              P.op("dve", lambda: nc.vector.reciprocal(out=sge, in_=sge), reads=["sge"], writes=["sge"])
              P.op("dve", lambda tt=tt: nc.vector.tensor_tensor(out=sg[tt].rearrange("p a b -> p (a b)"), in0=banks[6][:], in1=sge, op=ALU.mult),
                   reads=[("bank", 6), "sge"], writes=[("sg", tt)])
              P.op("dve", lambda tt=tt: nc.vector.tensor_tensor(out=sg[tt], in0=sg[tt],
                                                                  in1=ggl[:].unsqueeze(1).to_broadcast([128, 4, 128]), op=ALU.mult),
                   reads=[("sg", tt), "ggl"], writes=[("sg", tt)])
          cut(5)
          for qk, bk in ((0, 3), (1, 4)):
              for p in range(2):
                  for c in range(KC):
                      P.op("pe", lambda c=c, p=p, qk=qk, bk=bk, g=g: nc.tensor.matmul(
                          banks[bk][:, p * 256:(p + 1) * 256], wg[:, c, qk * 256 + p * 128:qk * 256 + (p + 1) * 128],
                          UT[:, c, g * 256:(g + 1) * 256], start=(c == 0), stop=(c == KC - 1), skip_group_check=True),
                          reads=["wg", ("UT", 2 * g), ("UT", 2 * g + 1)], writes=[("bank", bk)])
          qv = banks[3][:].rearrange("p (a b) -> p a b", a=2)
          kv = banks[4][:].rearrange("p (a b) -> p a b", a=2)
          for dst, src, ee, nm, sc in ((QP, qv, EP, "QP", 0.125), (QN, qv, EN, "QN", 0.125)):
              P.op("dve", lambda dst=dst, src=src, ee=ee, sc=sc: nc.vector.scalar_tensor_tensor(
                  out=dst, in0=src, scalar=sc, in1=ee, op0=ALU.mult, op1=ALU.mult),
                  reads=[("bank", 3), {"QP": "EP", "QN": "EN"}[nm]], writes=[nm])
          for hh in range(2):
              for dst, src, ee, nm, ekey, mcol, bk in ((QLah[hh], qv, ELa, "QLa", "ELa", 290 + hh, 3), (QLbh[hh], qv, ELb, "QLb", "ELb", 290 + hh, 3),
                                                       (KNh[hh], kv, EN, "KN", "EN", 288 + hh, 4), (KPh[hh], kv, EP, "KP", "EP", 288 + hh, 4)):
                  P.op("dve", lambda dst=dst, src=src, ee=ee, mcol=mcol: nc.vector.scalar_tensor_tensor(
                      out=dst, in0=src, scalar=cf32[:, mcol:mcol + 1], in1=ee, op0=ALU.mult, op1=ALU.mult),
                      reads=[("bank", bk), ekey, "cf32"], writes=[(nm, hh)])
          for tt in range(2):
              t = 2 * g + tt
              cur["t"] = t
              cs = slice(tt * 128, (tt + 1) * 128)
              cut(6)
              P.op("pe", lambda: nc.tensor.matmul(banks[5][:, 0:128], ident, ident, start=True, stop=True, skip_group_check=True),
                   reads=["cbf"], writes=[("bank", 5)])
              for h in range(4):
                  p, hh = h // 2, h % 2
                  rows = slice(hh * 64, (hh + 1) * 64)
                  P.op("pe", lambda h=h, p=p, hh=hh, cs=cs: nc.tensor.matmul(
                      banks[5][:, h * 128:(h + 1) * 128], KNh[hh][:, p, cs], QP[:, p, cs], start=True, stop=True, skip_group_check=True),
                      reads=[("KN", hh), "QP"], writes=[("bank", 5)])
                  P.op("pe", lambda h=h, p=p, hh=hh, cs=cs: nc.tensor.matmul(
                      banks[6][:, h * 128:(h + 1) * 128], KPh[hh][:, p, cs], QN[:, p, cs], start=True, stop=True, skip_group_check=True),
                      reads=[("KP", hh), "QN"], writes=[("bank", 6)])
              P.op("dve", lambda: nc.vector.tensor_tensor(out=at1, in0=banks[5][:].rearrange("p (a b) -> p a b", a=4),
                                                          in1=maskP.unsqueeze(1).to_broadcast([128, 4, 128]), op=ALU.mult),
                   reads=[("bank", 5), "cf32"], writes=["at1"])
              P.op("dve", lambda: nc.vector.tensor_tensor(out=at2, in0=banks[6][:].rearrange("p (a b) -> p a b", a=4),
                                                          in1=maskF.unsqueeze(1).to_broadcast([128, 4, 128]), op=ALU.mult),
                   reads=[("bank", 6), "cf32"], writes=["at2"])
              P.op("dve", lambda: nc.vector.tensor_tensor(out=ATb, in0=at1, in1=at2, op=ALU.add),
                   reads=["at1", "at2"], writes=["ATb"])
              cut(7)
              for ch, bu in ((0, 7), (1, 4)):
                  rows = slice(ch * 64, (ch + 1) * 64)
                  for p in range(2):
                      P.op("pe", lambda p=p, ch=ch, bu=bu, tt=tt: nc.tensor.matmul(
                          banks[bu][:, p * 256:(p + 1) * 256], KE[tt][ch][:, p * 128:(p + 1) * 128], Vg[tt][:, p * 256:(p + 1) * 256],
                          start=True, stop=True, skip_group_check=True),
                          reads=[("KE", tt), ("Vg", tt)], writes=[("bank", bu)])
              par = t % 2
              Sba, Sbb = Sba2[par], Sbb2[par]

              def upd(ch, bu, snap, sname, tt=tt):
                  dcol = tt * 128 + ch * 64 + 63
                  els = ELa if ch == 0 else ELb
                  for p in range(2):
                      for hh in range(2):
                          if ch == 1 and os.environ.get("GLA_VAR") == "B":
                              continue
                          rows = slice(hh * 64, (hh + 1) * 64)
                          P.op("dve", lambda p=p, rows=rows, bu=bu, els=els, dcol=dcol, hh=hh: nc.vector.scalar_tensor_tensor(
                              out=Sst[p][rows, :], in0=Sst[p][rows, :], scalar=els[rows, p, dcol:dcol + 1],
                              in1=banks[bu][rows, p * 256 + hh * 128:p * 256 + (hh + 1) * 128], op0=ALU.mult, op1=ALU.add),
                              reads=[("S", p), ("bank", bu), "ELa" if ch == 0 else "ELb"], writes=[("S", p)])
                      if ch == 1 and os.environ.get("GLA_VAR") == "A":
                          continue
                      P.op("act", lambda p=p, snap=snap: nc.scalar.copy(out=snap[p], in_=Sst[p]),
                           reads=[("S", p)], writes=[sname + (p,)])

              cut(8)
              upd(0, 7, Sbb, ("Sbb", par))
              cut(9)
              for h in range(4):
                  p, hh = h // 2, h % 2
                  rows = slice(hh * 64, (hh + 1) * 64)
                  oc = slice(h * 128, (h + 1) * 128)
                  P.op("pe", lambda p=p, hh=hh, oc=oc, cs=cs, Sba=Sba: nc.tensor.matmul(banks[1][:, oc], QLah[hh][:, p, cs], Sba[p],
                                                                                    start=True, stop=False, skip_group_check=True),
                       reads=[("QLa", hh), ("Sba", par, p)], writes=[("bank", 1)])
                  P.op("pe", lambda p=p, hh=hh, oc=oc, cs=cs, Sbb=Sbb: nc.tensor.matmul(banks[1][:, oc], QLbh[hh][:, p, cs], Sbb[p],
                                                                                    start=False, stop=False, skip_group_check=True),
                       reads=[("QLb", hh), ("Sbb", par, p)], writes=[("bank", 1)])
                  P.op("pe", lambda h=h, oc=oc, tt=tt: nc.tensor.matmul(banks[1][:, oc], ATb[:, h, :], Vg[tt][:, oc],
                                                                         start=False, stop=True, skip_group_check=True),
                       reads=["ATb", ("Vg", tt)], writes=[("bank", 1)])
              cut(11)
              upd(1, 4, Sba2[1 - par], ("Sba", 1 - par))
              cut(10)
              o4 = banks[1][:].rearrange("p (a b) -> p a b", a=4)
              P.op("act", lambda: nc.scalar.activation(out=osq.rearrange("p a b -> p (a b)"), in_=banks[1][:], func=AF.Square),
                   reads=[("bank", 1)], writes=["osq"])
              P.op("dve", lambda: nc.vector.reduce_sum(out=oss, in_=osq, axis=AX.X), reads=["osq"], writes=["oss"])
              P.op("act", lambda: nc.scalar.activation(out=ors, in_=oss, func=AF.Ln, bias=EPS, scale=1.0 / 128),
                   reads=["oss"], writes=["ors"])
              P.op("act", lambda: nc.scalar.activation(out=ors, in_=ors, func=AF.Exp, scale=-0.5), reads=["ors"], writes=["ors"])
              P.op("dve", lambda o4=o4: nc.vector.tensor_tensor(out=otm, in0=o4, in1=ors.unsqueeze(2).to_broadcast([128, 4, 128]),
                                                                 op=ALU.mult),
                   reads=[("bank", 1), "ors"], writes=["otm"])
              P.op("dve", lambda tt=tt: nc.vector.tensor_tensor(out=ogb, in0=otm, in1=sg[tt], op=ALU.mult),
                   reads=["otm", ("sg", tt)], writes=["ogb"])
              cut(121)
              for h in range(4):
                  P.op("pe", lambda h=h: nc.tensor.transpose(out=bank_bf(3)[:, h * 128:(h + 1) * 128], in_=ogb[:, h, :], identity=ident),
                       reads=["ogb", "cbf"], writes=[("bank", 3)])
              cut(122)
              P.op("act", lambda t=t: nc.scalar.copy(out=mixT[:, 4:8, t * 128:(t + 1) * 128],
                                                     in_=bank_bf(3)[:, 0:512].rearrange("p (a b) -> p a b", a=4)),
                   reads=[("bank", 3)], writes=[("mixTg", t)])


    try:
        gla_all()
    except _Cut:
        pass

    if stage == "gla":
        for h in range(4):
            P.dma("sp", lambda h=h: nc.sync.dma_start(out=dbg[:, h, :], in_=mixT[:, 4 + h, :]),
                  reads=[("mixTg", i) for i in range(NT)], slot="out")
        P.emit()
        sem, v = P.final_slots["out"]
        nc.sync.wait_ge(sem, v)
        return nc, P


    P.fence()
    A.reset()
    A2 = Arena(UT[:].rearrange("p c t -> p (c t)"), KC * T)
    w_o_v = w_o.rearrange("(c p) n -> p c n", p=128)
    w_cq_v = w_cq.rearrange("(c p) n -> p c n", p=128)
    w_co_v = w_co.rearrange("(c p) n -> p c n", p=128)
    w_ckv_v = w_ckv.rearrange("(c p) n -> p c n", p=128)
    wo = A.take([128, KC, D], BF16)
    wcq = A.take([128, KC, D], BF16)
    wco = A.take([128, KC, D], BF16)
    def load_w(nm_, dst_, src_):
        for hf_ in range(2):
            P.dma("pool", lambda dst_=dst_, src_=src_, hf_=hf_: nc.gpsimd.dma_start(out=dst_[:, :, hf_ * 512:(hf_ + 1) * 512],
                                                                                     in_=src_[:, :, hf_ * 512:(hf_ + 1) * 512]),
                  writes=[nm_], slot=(nm_, hf_))
    gcross = A.take([128, D], F32)
    gffn = A.take([128, D], F32)
    gcq = A.take([128, 4, 256], F32)
    gck = A.take([128, 4, 256], F32)
    P.dma("sp", lambda: nc.sync.dma_start(out=gcross, in_=norm_cross.partition_broadcast(128)), writes=["gcross"], slot="t0")
    P.dma("sp", lambda: nc.sync.dma_start(out=gffn, in_=norm_ffn.partition_broadcast(128)), writes=["gffn"], slot="t1")
    for hq in range(4):
        P.dma("sp", lambda hq=hq: nc.sync.dma_start(out=gcq[:, hq, :], in_=cross_q_norm.partition_broadcast(128)), writes=["gcq"], slot="t2")
        P.dma("sp", lambda hq=hq: nc.sync.dma_start(out=gck[:, hq, :], in_=cross_k_norm.partition_broadcast(128)), writes=["gck"], slot="t3")
    P.op("dve", lambda: nc.vector.tensor_scalar(out=gcq, in0=gcq, scalar1=1.0 / 16, scalar2=None, op0=ALU.mult), reads=["gcq"], writes=["gcq"])
    wr = A.take([128, KC, 36], BF16)
    P.dma("pool", lambda: nc.gpsimd.dma_start(out=wr[:, :, 0:4], in_=w_group.rearrange("(c p) n -> p c n", p=128)), writes=["wr"], slot="t4")
    P.dma("pool", lambda: nc.gpsimd.dma_start(out=wr[:, :, 4:36], in_=w_expert.rearrange("(c p) n -> p c n", p=128)), writes=["wr"], slot="t5")
    rbias = A.take([128, 36], F32)
    P.dma("sp", lambda: nc.sync.dma_start(out=rbias[:, 0:4], in_=b_group.partition_broadcast(128)), writes=["rbias"], slot="t6")
    P.dma("sp", lambda: nc.sync.dma_start(out=rbias[:, 4:36], in_=b_expert.partition_broadcast(128)), writes=["rbias"], slot="t7")
    _bc = {}

    def get_bc():
        if "r" not in _bc:
            _bc["r"] = nc.gpsimd.to_reg(NSLOT - 1)
        return _bc["r"]

    destAll = P.sb("destAll", [128, NT, 2], I32)
    gateAll = P.sb("gateAll", [128, NT, 2], F32)
    base = A.take([128, 32], F32)
    P.op("dve", lambda: nc.vector.memset(base, 0.0), writes=["base"])

    zt = A.take([128, D], BF16)
    P.op("dve", lambda: nc.vector.memset(zt, 0.0), writes=["zt"])
    xp_z = x_pad.rearrange("(n p) d -> n p d", p=128)
    for zi in range(NSLOT // 128):
        P.dma("sp", lambda zi=zi: nc.sync.dma_start(out=xp_z[zi], in_=zt), reads=["zt"], writes=["x_pad"], slot="xz")

    KcT = A2.take([128, KC, 256], BF16)
    Vc = A2.take([128, 2, D], BF16)
    a2_mark = A2.off
    gmem = A2.take([128, D], F32)
    P.dma("sp", lambda: nc.sync.dma_start(out=gmem, in_=norm_mem.partition_broadcast(128)), writes=["gmem"], slot="t8")
    wkv = A2.take([128, KC, D], BF16)
    MT = A2.take([128, KC, 256], BF16)
    xtm = [A2.take([128, D], F32) for i in range(2)]
    scr0 = (A2.take([128, D], BF16), A2.take([128, 4], F32), A2.take([128, 4], F32), A2.take([128, D], F32), "m")
    nb16 = A2.take([128, D], BF16)

    def transpose8(src_bf, src_key, dst3, dst_key, copy_eng):
        for c in range(KC):
            P.op("pe", lambda c=c, src_bf=src_bf: nc.tensor.transpose(out=bank_bf(4)[:, c * 128:(c + 1) * 128],
                                                                      in_=src_bf[:, c * 128:(c + 1) * 128], identity=ident),
                 reads=[src_key, "cbf"], writes=[("bank", 4)])
        if copy_eng == "act":
            P.op("act", lambda dst3=dst3: nc.scalar.copy(out=dst3, in_=bank_bf(4).rearrange("p (c n) -> p c n", c=KC)),
                 reads=[("bank", 4)], writes=[dst_key])
        else:
            P.op("dve", lambda dst3=dst3: nc.vector.tensor_copy(out=dst3, in_=bank_bf(4).rearrange("p (c n) -> p c n", c=KC)),
                 reads=[("bank", 4)], writes=[dst_key])

    def rms_rows(srcap, src_keys, ngrp, gain3, gain_key, dst_bf, dst_key, scr):
        sj, stss, strs, snf, tag = scr
        kj, kt_, kr, kn = ("junk3", tag), ("tss", tag), ("trs", tag), ("nf32", tag)
        w = D // ngrp
        for gi in range(ngrp):
            P.op("act", lambda gi=gi, sj=sj, stss=stss, srcap=srcap, w=w: nc.scalar.activation(
                out=sj[:, 0:w], in_=srcap[:, gi * w:(gi + 1) * w], func=AF.Square, accum_out=stss[:, gi:gi + 1]),
                reads=list(src_keys), writes=[kj, kt_])
        P.op("act", lambda stss=stss, strs=strs, ngrp=ngrp, w=w: nc.scalar.activation(out=strs[:, 0:ngrp], in_=stss[:, 0:ngrp], func=AF.Ln,
                                                                                      bias=EPS, scale=1.0 / w),
             reads=[kt_], writes=[kr])
        P.op("act", lambda strs=strs, ngrp=ngrp: nc.scalar.activation(out=strs[:, 0:ngrp], in_=strs[:, 0:ngrp], func=AF.Exp, scale=-0.5),
             reads=[kr], writes=[kr])
        P.op("dve", lambda snf=snf, srcap=srcap, strs=strs, ngrp=ngrp, w=w: nc.vector.tensor_tensor(
            out=snf.rearrange("p (a b) -> p a b", a=ngrp), in0=srcap.rearrange("p (a b) -> p a b", a=ngrp),
            in1=strs[:, 0:ngrp].unsqueeze(2).to_broadcast([128, ngrp, w]), op=ALU.mult),
            reads=list(src_keys) + [kr], writes=[kn])
        P.op("dve", lambda snf=snf, dst_bf=dst_bf, gain3=gain3: nc.vector.tensor_tensor(out=dst_bf, in0=snf, in1=gain3, op=ALU.mult),
             reads=[kn, gain_key], writes=[dst_key])

    for mt in range(2):
        P.dma("sp", lambda mt=mt: nc.sync.dma_start(out=xtm[mt], in_=mem[mt * 128:(mt + 1) * 128, :]), writes=[("xtm", mt)], slot=("xtm", mt))
        rms_rows(xtm[mt], [("xtm", mt)], 1, gmem, "gmem", nb16, "nb16m", scr0)
        transpose8(nb16, "nb16m", MT[:, :, mt * 128:(mt + 1) * 128], ("MT", mt), "act")
    for part in range(2):
        for hf_ in range(2):
            P.dma("pool", lambda part=part, hf_=hf_: nc.gpsimd.dma_start(
                out=wkv[:, :, hf_ * 512:(hf_ + 1) * 512], in_=w_ckv_v[:, :, part * D + hf_ * 512:part * D + (hf_ + 1) * 512]),
                writes=["wkv"], slot=("wkv", hf_))
        if part == 0:
            load_w("wo", wo, w_o_v)
        else:
            load_w("wcq", wcq, w_cq_v)
            load_w("wco", wco, w_co_v)
        for mt in range(2):
            for hf_ in range(2):
                for c in range(KC):
                    P.op("pe", lambda c=c, mt=mt, hf_=hf_: nc.tensor.matmul(banks[hf_], MT[:, c, mt * 128:(mt + 1) * 128],
                                                                             wkv[:, c, hf_ * 512:(hf_ + 1) * 512],
                                                                             start=(c == 0), stop=(c == KC - 1)),
                         reads=[("MT", 0), ("MT", 1), "wkv"], writes=[("bank", hf_)])
            if part == 0:
                rms_rows(bigv[0], [("bank", 0), ("bank", 1)], 4, gck.rearrange("p a b -> p (a b)"), "gck", nb16, "nb16m", scr0)
                transpose8(nb16, "nb16m", KcT[:, :, mt * 128:(mt + 1) * 128], ("KcT", mt), "act")
            else:
                P.op("act", lambda mt=mt: nc.scalar.copy(out=Vc[:, mt, :], in_=bigv[0]), reads=[("bank", 0), ("bank", 1)], writes=[("Vc", mt)])

    P.fence()
    A2.off = a2_mark
    xtt = [A2.take([128, D], F32) for i in range(2)]
    scr1 = (A2.take([128, D], BF16), A2.take([128, 4], F32), A2.take([128, 4], F32), A2.take([128, D], F32), "t")
    h1 = A2.take([128, D], F32)
    u2 = A2.take([128, D], BF16)
    U2T = A2.take([128, KC, 128], BF16)
    qnb = A2.take([128, D], BF16)
    QcT = A2.take([128, KC, 128], BF16)
    PT = A2.take([128, 8, 128], BF16)
    ocb = A2.take([128, 4, 256], BF16)
    OcT = A2.take([128, KC, 128], BF16)
    h2 = [A2.take([128, D], F32) for i in range(2)]
    xfb = [A2.take([128, D], BF16) for i in range(2)]
    XfT = A2.take([128, KC, 128], BF16)
    csum = A2.take([128, 4], F32)
    L = A2.take([128, 36], F32)
    rt = A2.take([128, 64], F32)
    tmp48 = A2.take([128, 4, 8], F32)
    OH1 = A2.take([128, 4, 8], F32)
    OH2 = A2.take([128, 4, 8], F32)
    OHs = A2.take([128, 32], BF16)
    posE = A2.take([128, 32], F32)
    t32 = A2.take([128, 32], F32)
    dstf = A2.take([128, 2], F32)
    NT_RUN = int(os.environ.get("TAIL_NT", str(NT)))

    def tail_a(t):
        tk = slice(t * 128, (t + 1) * 128)
        xs = t % 2
        P.dma("sp", lambda t=t, xs=xs: nc.sync.dma_start(out=xtt[xs], in_=x[t * 128:(t + 1) * 128, :]), writes=[("xtt", xs)], slot=("xtt", xs))
        for hf_ in range(2):
            for c in range(KC):
                P.op("pe", lambda c=c, hf_=hf_, tk=tk: nc.tensor.matmul(banks[hf_], mixT[:, c, tk], wo[:, c, hf_ * 512:(hf_ + 1) * 512],
                                                                         start=(c == 0), stop=(c == KC - 1)),
                     reads=["wo", ("mixT", c, t // 2) if c < 4 else ("mixTg", t)], writes=[("bank", hf_)])
        P.op("dve", lambda xs=xs: nc.vector.tensor_tensor(out=h1, in0=bigv[0], in1=xtt[xs], op=ALU.add),
             reads=[("bank", 0), ("bank", 1), ("xtt", xs)], writes=["h1"])
        rms_rows(h1, ["h1"], 1, gcross, "gcross", u2, "u2", scr1)
        transpose8(u2, "u2", U2T, "U2T", "act")
        for hf_ in range(2):
            for c in range(KC):
                P.op("pe", lambda c=c, hf_=hf_: nc.tensor.matmul(banks[2 + hf_], U2T[:, c, :], wcq[:, c, hf_ * 512:(hf_ + 1) * 512],
                                                                  start=(c == 0), stop=(c == KC - 1)),
                     reads=["wcq", "U2T"], writes=[("bank", 2 + hf_)])
        rms_rows(bigv[1], [("bank", 2), ("bank", 3)], 4, gcq.rearrange("p a b -> p (a b)"), "gcq", qnb, "qnb", scr1)
        transpose8(qnb, "qnb", QcT, "QcT", "dve")
        for hq in range(4):
            for mt in range(2):
                col = (hq * 2 + mt) * 128
                bk = 6 + col // 512
                for dc in range(2):
                    P.op("pe", lambda hq=hq, mt=mt, dc=dc, col=col, bk=bk: nc.tensor.matmul(
                        banks[bk][:, col % 512:col % 512 + 128], KcT[:, 2 * hq + dc, mt * 128:(mt + 1) * 128], QcT[:, 2 * hq + dc, :],
                        start=(dc == 0), stop=(dc == 1), skip_group_check=True),
                        reads=[("KcT", 0), ("KcT", 1), "QcT"], writes=[("bank", bk)])
        P.op("act", lambda: nc.scalar.activation(out=PT.rearrange("p a b -> p (a b)"), in_=bigv[3], func=AF.Exp),
             reads=[("bank", 6), ("bank", 7)], writes=["PT"])
        for hq in range(4):
            for mt in range(2):
                P.op("pe", lambda hq=hq, mt=mt: nc.tensor.matmul(banks[2 + hq // 2][:, (hq % 2) * 256:(hq % 2) * 256 + 256], PT[:, hq * 2 + mt, :],
                                                                  Vc[:, mt, hq * 256:(hq + 1) * 256], start=(mt == 0), stop=(mt == 1),
                                                                  skip_group_check=True),
                     reads=["PT", ("Vc", 0), ("Vc", 1)], writes=[("bank", 2 + hq // 2)])
            for mt in range(2):
                P.op("pe", lambda hq=hq, mt=mt: nc.tensor.matmul(banks[5][:, hq:hq + 1], PT[:, hq * 2 + mt, :], ones_bf[:, 0:1],
                                                                  start=(mt == 0), stop=(mt == 1), skip_group_check=True),
                     reads=["PT", "cbf"], writes=[("bank", 5)])
        P.op("dve", lambda: nc.vector.reciprocal(out=csum, in_=banks[5][:, 0:4]), reads=[("bank", 5)], writes=["csum"])
        P.op("dve", lambda: nc.vector.tensor_tensor(out=ocb, in0=bigv[1].rearrange("p (a b) -> p a b", a=4),
                                                    in1=csum.unsqueeze(2).to_broadcast([128, 4, 256]), op=ALU.mult),
             reads=[("bank", 2), ("bank", 3), "csum"], writes=["ocb"])
        transpose8(ocb.rearrange("p a b -> p (a b)"), "ocb", OcT, "OcT", "act")
        for hf_ in range(2):
            for c in range(KC):
                P.op("pe", lambda c=c, hf_=hf_: nc.tensor.matmul(banks[hf_], OcT[:, c, :], wco[:, c, hf_ * 512:(hf_ + 1) * 512],
                                                                  start=(c == 0), stop=(c == KC - 1)),
                     reads=["wco", "OcT"], writes=[("bank", hf_)])
        P.op("dve", lambda xs=xs: nc.vector.tensor_tensor(out=h2[xs], in0=bigv[0], in1=h1, op=ALU.add),
             reads=[("bank", 0), ("bank", 1), "h1"], writes=[("h2", xs)])
        P.dma("sp", lambda t=t, xs=xs: nc.sync.dma_start(out=out[t * 128:(t + 1) * 128, :], in_=h2[xs]),
              reads=[("h2", xs)], writes=[("h2d", t)], slot=("h2d", xs))

    def tail_b(t):
        tk = slice(t * 128, (t + 1) * 128)
        xs = t % 2
        rms_rows(h2[xs], [("h2", xs)], 1, gffn, "gffn", xfb[xs], ("xfb", xs), scr1)
        transpose8(xfb[xs], ("xfb", xs), XfT, "XfT", "dve")
        for c in range(KC):
            P.op("pe", lambda c=c: nc.tensor.matmul(banks[5][:, 8:44], XfT[:, c, :], wr[:, c, :], start=(c == 0), stop=(c == KC - 1),
                                                     skip_group_check=True),
                 reads=["XfT", "wr"], writes=[("bank", 5)])
        P.op("dve", lambda: nc.vector.tensor_tensor(out=L, in0=banks[5][:, 8:44], in1=rbias, op=ALU.add),
             reads=[("bank", 5), "rbias"], writes=["L"])
        gl = L[:, 0:4]
        el = L[:, 4:36].rearrange("p (a b) -> p a b", a=4)
        R = "rt"
        P.op("dve", lambda: nc.vector.reduce_max(out=rt[:, 0:1], in_=gl, axis=AX.X), reads=["L"], writes=[R])
        P.op("dve", lambda: nc.vector.tensor_scalar(out=rt[:, 1:2], in0=rt[:, 0:1], scalar1=-1.0, scalar2=None, op0=ALU.mult), reads=[R], writes=[R])
        P.op("dve", lambda: nc.vector.tensor_scalar(out=rt[:, 16:20], in0=gl, scalar1=rt[:, 0:1], scalar2=None, op0=ALU.is_ge), reads=["L", R], writes=[R])
        P.op("act", lambda: nc.scalar.activation(out=rt[:, 20:24], in_=gl, func=AF.Exp, bias=rt[:, 1:2], accum_out=rt[:, 2:3]), reads=["L", R], writes=[R])
        P.op("dve", lambda: nc.vector.reciprocal(out=rt[:, 3:4], in_=rt[:, 2:3]), reads=[R], writes=[R])
        P.op("dve", lambda: nc.vector.tensor_tensor(out=tmp48, in0=el, in1=rt[:, 16:20].unsqueeze(2).to_broadcast([128, 4, 8]), op=ALU.mult),
             reads=["L", R], writes=["tmp48"])
        P.op("dve", lambda: nc.vector.reduce_sum(out=rt[:, 24:32], in_=tmp48.rearrange("p a b -> p b a"), axis=AX.X), reads=["tmp48"], writes=[R])
        P.op("dve", lambda: nc.vector.reduce_max(out=rt[:, 4:5], in_=rt[:, 24:32], axis=AX.X), reads=[R], writes=[R])
        P.op("dve", lambda: nc.vector.tensor_scalar(out=rt[:, 32:40], in0=rt[:, 24:32], scalar1=rt[:, 4:5], scalar2=None, op0=ALU.is_ge), reads=[R], writes=[R])
        P.op("dve", lambda: nc.vector.scalar_tensor_tensor(out=rt[:, 40:48], in0=rt[:, 32:40], scalar=-1.0e9, in1=rt[:, 24:32],
                                                            op0=ALU.mult, op1=ALU.add), reads=[R], writes=[R])
        P.op("dve", lambda: nc.vector.reduce_max(out=rt[:, 5:6], in_=rt[:, 40:48], axis=AX.X), reads=[R], writes=[R])
        P.op("dve", lambda: nc.vector.tensor_scalar(out=rt[:, 48:56], in0=rt[:, 40:48], scalar1=rt[:, 5:6], scalar2=None, op0=ALU.is_ge), reads=[R], writes=[R])
        P.op("dve", lambda: nc.vector.tensor_tensor(out=rt[:, 6:7], in0=rt[:, 5:6], in1=rt[:, 4:5], op=ALU.subtract), reads=[R], writes=[R])
        P.op("act", lambda: nc.scalar.activation(out=rt[:, 7:8], in_=rt[:, 6:7], func=AF.Exp), reads=[R], writes=[R])
        P.op("dve", lambda: nc.vector.tensor_scalar(out=rt[:, 7:8], in0=rt[:, 7:8], scalar1=1.0, scalar2=None, op0=ALU.add), reads=[R], writes=[R])
        P.op("dve", lambda: nc.vector.reciprocal(out=rt[:, 7:8], in_=rt[:, 7:8]), reads=[R], writes=[R])
        P.op("dve", lambda: nc.vector.tensor_tensor(out=rt[:, 8:9], in0=rt[:, 7:8], in1=rt[:, 3:4], op=ALU.mult), reads=[R], writes=[R])
        P.op("dve", lambda: nc.vector.tensor_tensor(out=rt[:, 9:10], in0=rt[:, 3:4], in1=rt[:, 8:9], op=ALU.subtract), reads=[R], writes=[R])
        P.op("dve", lambda t=t: nc.vector.tensor_copy(out=gateAll[:, t, :], in_=rt[:, 8:10]), reads=[R], writes=[("gate", t)])
        for ohx, c0_, nm2 in ((OH1, 32, "OH1"), (OH2, 48, "OH2")):
            P.op("dve", lambda ohx=ohx, c0_=c0_: nc.vector.tensor_tensor(
                out=ohx, in0=rt[:, 16:20].unsqueeze(2).to_broadcast([128, 4, 8]),
                in1=rt[:, c0_:c0_ + 8].unsqueeze(1).to_broadcast([128, 4, 8]), op=ALU.mult), reads=[R], writes=[nm2])
        P.op("dve", lambda: nc.vector.tensor_tensor(out=OHs, in0=OH1.rearrange("p a b -> p (a b)"), in1=OH2.rearrange("p a b -> p (a b)"), op=ALU.add),
             reads=["OH1", "OH2"], writes=["OHs"])
        P.op("pe", lambda: nc.tensor.matmul(banks[5][:, 64:96], TriS, OHs, start=True, stop=True, skip_group_check=True),
             reads=["OHs", "cbf"], writes=[("bank", 5)])
        P.op("pe", lambda: nc.tensor.matmul(banks[5][:, 96:128], ones_bf, OHs, start=True, stop=True, skip_group_check=True),
             reads=["OHs", "cbf"], writes=[("bank", 5)])
        P.op("dve", lambda: nc.vector.tensor_tensor(out=posE, in0=banks[5][:, 64:96], in1=base, op=ALU.add),
             reads=[("bank", 5), "base"], writes=["posE"])
        P.op("dve", lambda: nc.vector.tensor_tensor(out=base, in0=banks[5][:, 96:128], in1=base, op=ALU.add),
             reads=[("bank", 5), "base"], writes=["base"])
        for kx, ohx, nm2 in ((0, OH1, "OH1"), (1, OH2, "OH2")):
            P.op("dve", lambda ohx=ohx: nc.vector.tensor_tensor(out=t32, in0=ohx.rearrange("p a b -> p (a b)"), in1=posE, op=ALU.mult),
                 reads=[nm2, "posE"], writes=["t32"])
            P.op("dve", lambda kx=kx: nc.vector.reduce_sum(out=rt[:, 10 + kx:11 + kx], in_=t32, axis=AX.X), reads=["t32"], writes=[R])
            P.op("dve", lambda ohx=ohx: nc.vector.tensor_tensor(out=t32, in0=ohx.rearrange("p a b -> p (a b)"), in1=iota32, op=ALU.mult),
                 reads=[nm2, "cf32"], writes=["t32"])
            P.op("dve", lambda kx=kx: nc.vector.reduce_sum(out=rt[:, 12 + kx:13 + kx], in_=t32, axis=AX.X), reads=["t32"], writes=[R])
            P.op("dve", lambda kx=kx: nc.vector.tensor_scalar(out=rt[:, 14 + kx:15 + kx], in0=rt[:, 10 + kx:11 + kx], scalar1=float(CAP),
                                                              scalar2=1.0e6, op0=ALU.is_ge, op1=ALU.mult), reads=[R], writes=[R])
            P.op("dve", lambda kx=kx: nc.vector.scalar_tensor_tensor(out=dstf[:, kx:kx + 1], in0=rt[:, 12 + kx:13 + kx], scalar=float(CAP),
                                                                      in1=rt[:, 10 + kx:11 + kx], op0=ALU.mult, op1=ALU.add),
                 reads=[R], writes=["dstf"])
            P.op("dve", lambda kx=kx: nc.vector.tensor_tensor(out=dstf[:, kx:kx + 1], in0=dstf[:, kx:kx + 1], in1=rt[:, 14 + kx:15 + kx], op=ALU.add),
                 reads=[R, "dstf"], writes=["dstf"])
        P.op("dve", lambda t=t: nc.vector.tensor_copy(out=destAll[:, t, :], in_=dstf), reads=["dstf"], writes=[("dest", t)])
        for kx in range(2):
            P.dma("pool", lambda t=t, kx=kx, xs=xs: nc.gpsimd.indirect_dma_start(
                out=x_pad[:, :], out_offset=bass.IndirectOffsetOnAxis(ap=destAll[:, t, kx:kx + 1], axis=0),
                in_=xfb[xs], in_offset=None, bounds_check=get_bc(), oob_is_err=False),
                reads=[("dest", t), ("xfb", xs)], writes=["x_pad"], slot="scat")


    for t in range(NT_RUN + 1):
        if t < NT_RUN:
            tail_a(t)
        if t >= 1 and stage != "tail":
            tail_b(t - 1)

    if stage == "tail":
        P.emit()
        return nc, P


    P.fence()
    A.reset()
    A3 = Arena(mixT[:].rearrange("p c t -> p (c t)"), KC * T)
    wv_g = w_e_gate.rearrange("e (c p) n -> e p c n", p=128)
    wv_u = w_e_up.rearrange("e (c p) n -> e p c n", p=128)
    wv_d = w_e_down.rearrange("e (c p) n -> e p c n", p=128)
    xp_v = x_pad.rearrange("(e j p) d -> e p j d", j=CAP // 128, p=128)
    NB = CAP // 128
    wgs = [A3.take([128, KC, 512], BF16) for i in range(2)]
    wus = [A3.take([128, KC, 512], BF16) for i in range(2)]
    wds = [A3.take([128, 4, D], BF16) for i in range(2)]
    xblk = [A.take([128, NB, D], BF16) for i in range(2)]
    XeT = [A.take([128, KC, CAP], BF16) for i in range(2)]
    hidT = A.take([128, 4, CAP], BF16)
    sil = [A.take([128, CAP], F32) for i in range(2)]
    oblk = [A.take([128, D], F32) for i in range(2)]
    NE = int(os.environ.get("MOE_NE", "32"))
    for e in range(NE):
        wsl = e % 2
        for hf_ in range(2):
            P.dma("pool", lambda e=e, wsl=wsl, hf_=hf_: nc.gpsimd.dma_start(out=wgs[wsl][:, :, hf_ * 256:(hf_ + 1) * 256],
                                                                            in_=wv_g[e, :, :, hf_ * 256:(hf_ + 1) * 256]),
                  writes=[("wgs", wsl)], slot=("wgs", wsl, hf_))
            P.dma("pool", lambda e=e, wsl=wsl, hf_=hf_: nc.gpsimd.dma_start(out=wus[wsl][:, :, hf_ * 256:(hf_ + 1) * 256],
                                                                            in_=wv_u[e, :, :, hf_ * 256:(hf_ + 1) * 256]),
                  writes=[("wus", wsl)], slot=("wus", wsl, hf_))
            P.dma("pool", lambda e=e, wsl=wsl, hf_=hf_: nc.gpsimd.dma_start(out=wds[wsl][:, :, hf_ * 512:(hf_ + 1) * 512],
                                                                            in_=wv_d[e, :, :, hf_ * 512:(hf_ + 1) * 512]),
                  writes=[("wds", wsl)], slot=("wds", wsl, hf_))
        P.dma("sp", lambda e=e, wsl=wsl: nc.sync.dma_start(out=xblk[wsl], in_=xp_v[e]), reads=["x_pad"], writes=[("xblk", wsl)],
              slot=("xblk", wsl))
        for j in range(NB):
            for c in range(KC):
                P.op("pe", lambda c=c, j=j, wsl=wsl: nc.tensor.transpose(out=bank_bf(4)[:, c * 128:(c + 1) * 128],
                                                                         in_=xblk[wsl][:, j, c * 128:(c + 1) * 128], identity=ident),
                     reads=[("xblk", wsl), "cbf"], writes=[("bank", 4)])
            if j % 2 == 0:
                P.op("act", lambda j=j, wsl=wsl: nc.scalar.copy(out=XeT[wsl][:, :, j * 128:(j + 1) * 128],
                                                                in_=bank_bf(4).rearrange("p (c n) -> p c n", c=KC)),
                     reads=[("bank", 4)], writes=[("XeT", wsl)])
            else:
                P.op("dve", lambda j=j, wsl=wsl: nc.vector.tensor_copy(out=XeT[wsl][:, :, j * 128:(j + 1) * 128],
                                                                       in_=bank_bf(4).rearrange("p (c n) -> p c n", c=KC)),
                     reads=[("bank", 4)], writes=[("XeT", wsl)])
        for hc in range(4):
            bg = 0 if hc % 2 == 0 else 2
            for wsrc, bk, wkey in ((wgs, bg, "wgs"), (wus, bg + 1, "wus")):
                for c in range(KC):
                    P.op("pe", lambda c=c, hc=hc, wsl=wsl, wsrc=wsrc, bk=bk: nc.tensor.matmul(
                        banks[bk], wsrc[wsl][:, c, hc * 128:(hc + 1) * 128], XeT[wsl][:, c, :], start=(c == 0), stop=(c == KC - 1)),
                        reads=[(wkey, wsl), ("XeT", wsl)], writes=[("bank", bk)])
            P.op("act", lambda hc=hc, bg=bg: nc.scalar.activation(out=sil[hc % 2], in_=banks[bg], func=AF.Silu),
                 reads=[("bank", bg)], writes=[("sil", hc % 2)])
            P.op("dve", lambda hc=hc, bg=bg: nc.vector.tensor_tensor(out=hidT[:, hc, :], in0=banks[bg + 1], in1=sil[hc % 2], op=ALU.mult),
                 reads=[("bank", bg + 1), ("sil", hc % 2)], writes=[("hidT", hc)])
        for j in range(NB):
            for half in range(2):
                for hc in range(4):
                    P.op("pe", lambda hc=hc, j=j, half=half, wsl=wsl: nc.tensor.matmul(
                        banks[6 + half], hidT[:, hc, j * 128:(j + 1) * 128], wds[wsl][:, hc, half * 512:(half + 1) * 512],
                        start=(hc == 0), stop=(hc == 3)),
                        reads=[("hidT", hc), ("wds", wsl)], writes=[("bank", 6 + half)])
            if j % 2 == 0:
                P.op("act", lambda j=j: nc.scalar.copy(out=oblk[j % 2], in_=bigv[3]), reads=[("bank", 6), ("bank", 7)], writes=[("oblk", j % 2)])
            else:
                P.op("dve", lambda j=j: nc.vector.tensor_copy(out=oblk[j % 2], in_=bigv[3]), reads=[("bank", 6), ("bank", 7)], writes=[("oblk", j % 2)])
            r0 = e * CAP + j * 128
            P.dma("sp", lambda j=j, r0=r0: nc.sync.dma_start(out=o_pad[r0:r0 + 128, :], in_=oblk[j % 2]),
                  reads=[("oblk", j % 2)], writes=["o_pad"], slot="opad")

    P.fence()
    A2.off = 0
    g1s = [A2.take([128, D], F32) for i in range(2)]
    g2s = [A2.take([128, D], F32) for i in range(2)]
    hhs = [A2.take([128, D], F32) for i in range(2)]
    fins = [A2.take([128, D], F32) for i in range(2)]
    for t in range(NT_RUN):
        s2 = t % 2
        for kx, gs, gname in ((0, g1s, "g1s"), (1, g2s, "g2s")):
            P.op("dve", lambda gs=gs, s2=s2: nc.vector.memset(gs[s2], 0.0), writes=[(gname, s2)])
            P.dma("pool", lambda gs=gs, s2=s2, t=t, kx=kx: nc.gpsimd.indirect_dma_start(
                out=gs[s2], out_offset=None, in_=o_pad[:, :], in_offset=bass.IndirectOffsetOnAxis(ap=destAll[:, t, kx:kx + 1], axis=0),
                bounds_check=get_bc(), oob_is_err=False),
                reads=["o_pad", ("dest", t)], writes=[(gname, s2)], slot=(gname, s2))
        P.dma("sp", lambda t=t, s2=s2: nc.sync.dma_start(out=hhs[s2], in_=out[t * 128:(t + 1) * 128, :]),
              reads=[("h2d", t)], writes=[("hhs", s2)], slot=("hhs", s2))
        P.op("dve", lambda t=t, s2=s2: nc.vector.scalar_tensor_tensor(out=fins[s2], in0=g1s[s2], scalar=gateAll[:, t, 0:1], in1=hhs[s2],
                                                                       op0=ALU.mult, op1=ALU.add),
             reads=[("g1s", s2), ("hhs", s2), ("gate", t)], writes=[("fins", s2)])
        P.op("dve", lambda t=t, s2=s2: nc.vector.scalar_tensor_tensor(out=fins[s2], in0=g2s[s2], scalar=gateAll[:, t, 1:2], in1=fins[s2],
                                                                       op0=ALU.mult, op1=ALU.add),
             reads=[("g2s", s2), ("fins", s2), ("gate", t)], writes=[("fins", s2)])
        P.dma("sp", lambda t=t, s2=s2: nc.sync.dma_start(out=out[t * 128:(t + 1) * 128, :], in_=fins[s2]),
              reads=[("fins", s2), ("hhs", s2)], writes=[("h2d", t)], slot=("fin", s2))
    P.emit()
    return nc, P


_CACHE = {}


def kernel(**inputs):
    nb = inputs["x"].shape[0]
    if "nc" not in _CACHE:
        _CACHE["nc"] = build("full")[0]
    nc = _CACHE["nc"]
    consts = make_consts()
    shared = {}
    for k, v in inputs.items():
        if k in ("x", "mem"):
            continue
        v = np.ascontiguousarray(np.asarray(v, dtype=np.float32))
        shared[k] = v[0] if v.ndim >= 3 else v
    in_maps = []
    for b in range(nb):
        m = dict(shared)
        m["x"] = np.ascontiguousarray(np.asarray(inputs["x"][b], dtype=np.float32))
        m["mem"] = np.ascontiguousarray(np.asarray(inputs["mem"][b], dtype=np.float32))
        m["consts"] = consts
        in_maps.append(m)
    res = run_bass_kernel_spmd(nc, in_maps, core_ids=list(range(nb)))
    return np.stack([np.asarray(r["out"], dtype=np.float32) for r in res.results], axis=0)


def make_consts():
    c = np.zeros((128, NCONST), np.float32)
    c[:, 0:128] = np.eye(128, dtype=np.float32)
    bo = np.zeros((128, 128), np.float32)
    bo[0:64, 0:64] = 1.0 / 64
    bo[64:128, 64:128] = 1.0 / 64
    c[:, 128:256] = bo
    j = np.arange(128)[:, None]
    i = np.arange(128)[None, :]
    same = (j // 64 == i // 64).astype(np.float32)
    mid = (i // 64) * 64 + 31
    c[:, 2304:2432] = -(1.0 / 16) * same * (j <= i)
    c[:, 2432:2560] = -(1.0 / 16) * same * ((j <= i).astype(np.float32) - (j <= mid).astype(np.float32))
    c[:, 2560:2688] = -(1.0 / 16) * same * (j > i)
    c[:, 2688:2816] = same * (j <= i)
    c[:, 2816:2944] = same * (j > i)
    c[:, 2944:3072] = (j < i)
    c[:, 3072:3200] = 1.0
    c[:, 3200:3232] = np.arange(32, dtype=np.float32)[None, :]
    c[0:64, 3232] = 1.0
    c[64:128, 3233] = 1.0
    c[0:64, 3234] = 0.125
    c[64:128, 3235] = 0.125
    return c
```
